# Optimizing a Trainium2 kernel written in Bass

```python
import jax, jax.numpy as jnp
from jax import lax
import numpy as np

D_MODEL = 1024
BATCH = 32
SEQ = 2048
DEPTH = 2

GRID_W = 64
CTX_LEN = 256
N_MOD = 6
EPS = 1e-6
ROPE_BASE = 10000.0
Q_BLOCK = 128
HEAD_DIM = 64
GQA_Q_HEADS = 6
GQA_KV_HEADS = 2
GQA_GROUP = GQA_Q_HEADS // GQA_KV_HEADS
GQA_WIDTH = GQA_Q_HEADS * HEAD_DIM
POOL_GROUPS = 4
POOL_WINDOWS = (2, 4, 8, 16)
POOL_WIDTH = D_MODEL // 4
POOL_GROUP_DIM = POOL_WIDTH // POOL_GROUPS
MLA_HEADS = 6
MLA_NOPE_DIM = 64
MLA_ROPE_DIM = 32
MLA_V_DIM = 64
MLA_Q_RANK = 384
MLA_KV_RANK = 256
MLA_WIDTH = MLA_HEADS * MLA_V_DIM
MIX_WIDTH = GQA_WIDTH + POOL_WIDTH + MLA_WIDTH
PROJ_SIZES = (GQA_WIDTH, GQA_KV_HEADS * HEAD_DIM, GQA_KV_HEADS * HEAD_DIM, POOL_WIDTH, MLA_Q_RANK, MLA_KV_RANK, MLA_ROPE_DIM)
IN_COLS = GQA_WIDTH + 4 * GQA_KV_HEADS * HEAD_DIM // 2 + POOL_WIDTH + MLA_Q_RANK + MLA_KV_RANK + MLA_ROPE_DIM
PEER_HEADS = 8
PEER_N_KEYS = 128
PEER_N_EXPERTS = PEER_N_KEYS * PEER_N_KEYS
PEER_TOPK = 16
PEER_QUERY_DIM = 256
PEER_HALF = PEER_QUERY_DIM // 2
PEER_CHUNK = 128

kernel_name = "hybrid_prefix_dit_gqa_pool_mla_peer"


def rms_norm(x, g):
    xf = x.astype(jnp.float32)
    y = xf * lax.rsqrt(jnp.mean(xf * xf, axis=-1, keepdims=True) + EPS)
    return (y * g.astype(jnp.float32)).astype(x.dtype)


def modulate(x, g, shift, scale):
    return rms_norm(x, g) * (1 + scale) + shift


def axial_rope(rows, rot_dim):
    row = jnp.repeat(jnp.arange(rows), GRID_W).astype(jnp.float32)
    col = jnp.tile(jnp.arange(GRID_W), rows).astype(jnp.float32)
    n = rot_dim // 4
    inv = ROPE_BASE ** (-jnp.arange(n, dtype=jnp.float32) / n)
    ang = jnp.concatenate([row[:, None] * inv, col[:, None] * inv], axis=-1)
    return jnp.cos(ang), jnp.sin(ang)


def apply_rope(x, cos, sin):
    L = x.shape[1]
    shape = (1, L) + (1,) * (x.ndim - 3) + (cos.shape[-1],)
    cos = cos.reshape(shape).astype(x.dtype)
    sin = sin.reshape(shape).astype(x.dtype)
    x1, x2 = jnp.split(x, 2, axis=-1)
    return jnp.concatenate([x1 * cos - x2 * sin, x1 * sin + x2 * cos], axis=-1)


def split_cols(p):
    outs = []
    o = 0
    for s in PROJ_SIZES:
        outs.append(p[..., o:o + s])
        o += s
    return outs


def blocked_attention(q, k, v, scale):
    B, Lq = q.shape[0], q.shape[1]
    nb = Lq // Q_BLOCK
    qb = q.reshape((B, nb, Q_BLOCK) + q.shape[2:]).swapaxes(0, 1)

    def one_block(qblk):
        s = jnp.einsum('bqhgd,bkhd->bhgqk', qblk, k, preferred_element_type=jnp.float32) * scale
        p = jax.nn.softmax(s, axis=-1).astype(v.dtype)
        return jnp.einsum('bhgqk,bkhd->bqhgd', p, v)

    o = lax.map(one_block, qb)
    return o.swapaxes(0, 1).reshape(B, Lq, -1)


def gqa_heads(q, k, v, qn_g, kn_g, rope):
    B, L, _ = q.shape
    q = rms_norm(q.reshape(B, L, GQA_KV_HEADS, GQA_GROUP, HEAD_DIM), qn_g)
    k = rms_norm(k.reshape(B, L, GQA_KV_HEADS, HEAD_DIM), kn_g)
    v = v.reshape(B, L, GQA_KV_HEADS, HEAD_DIM)
    if rope is not None:
        q = apply_rope(q, rope[0], rope[1])
        k = apply_rope(k, rope[0], rope[1])
    return q, k, v


def mla_heads(cq, ckv, kr, qn_g, kvn_g, w_uq, w_ukv, rope):
    B, L, _ = cq.shape
    q = (rms_norm(cq, qn_g) @ w_uq).reshape(B, L, MLA_HEADS, 1, MLA_NOPE_DIM + MLA_ROPE_DIM)
    kv = (rms_norm(ckv, kvn_g) @ w_ukv).reshape(B, L, MLA_HEADS, MLA_NOPE_DIM + MLA_V_DIM)
    q_nope, q_rope = q[..., :MLA_NOPE_DIM], q[..., MLA_NOPE_DIM:]
    k_nope, v = kv[..., :MLA_NOPE_DIM], kv[..., MLA_NOPE_DIM:]
    kr = kr[:, :, None, :]
    if rope is not None:
        q_rope = apply_rope(q_rope, rope[0], rope[1])
        kr = apply_rope(kr, rope[0], rope[1])
    q = jnp.concatenate([q_nope, q_rope], axis=-1)
    k = jnp.concatenate([k_nope, jnp.broadcast_to(kr, (B, L, MLA_HEADS, MLA_ROPE_DIM))], axis=-1)
    return q, k, v


def centred_mean_minus_self(x, w):
    B, L, C = x.shape
    xf = x.astype(jnp.float32)
    cs = jnp.concatenate([jnp.zeros((B, 1, C), jnp.float32), jnp.cumsum(xf, axis=1)], axis=1)
    t = jnp.arange(L)
    lo = jnp.clip(t - w // 2, 0, L)
    hi = jnp.clip(t - w // 2 + w, 0, L)
    cnt = (hi - lo).astype(jnp.float32)
    return ((cs[:, hi] - cs[:, lo]) / cnt[None, :, None] - xf).astype(x.dtype)


def pool_mixer(x, pool_w, pool_scale):
    B, L, _ = x.shape
    xg = x.reshape(B, L, POOL_GROUPS, POOL_GROUP_DIM)
    pooled = jnp.stack([centred_mean_minus_self(xg[:, :, i], POOL_WINDOWS[i]) for i in range(POOL_GROUPS)], axis=2)
    y = jnp.einsum('blgc,gcd->blgd', pooled, pool_w).reshape(B, L, POOL_WIDTH)
    return y * pool_scale


def peer_ffn(h, wq, subkeys, u_tab, v_tab):
    B, L, D = h.shape
    hc = h.reshape(B * L // PEER_CHUNK, PEER_CHUNK, D)

    def one_chunk(xc):
        C = xc.shape[0]
        q = (xc @ wq).reshape(C, PEER_HEADS, 2, PEER_HALF)
        s = jnp.einsum('chpd,hpnd->chpn', q, subkeys, preferred_element_type=jnp.float32)
        s1, i1 = lax.top_k(s[:, :, 0], PEER_TOPK)
        s2, i2 = lax.top_k(s[:, :, 1], PEER_TOPK)
        cand_s = (s1[..., :, None] + s2[..., None, :]).reshape(C, PEER_HEADS, PEER_TOPK * PEER_TOPK)
        cand_i = (i1[..., :, None] * PEER_N_KEYS + i2[..., None, :]).reshape(C, PEER_HEADS, PEER_TOPK * PEER_TOPK)
        top_s, pos = lax.top_k(cand_s, PEER_TOPK)
        eid = jnp.take_along_axis(cand_i, pos, axis=-1).reshape(C, PEER_HEADS * PEER_TOPK)
        gate = jax.nn.softmax(top_s, axis=-1).reshape(C, PEER_HEADS * PEER_TOPK)
        u = u_tab[eid]
        v = v_tab[eid]
        a = jnp.einsum('cd,ced->ce', xc, u, preferred_element_type=jnp.float32)
        wgt = (gate * jax.nn.gelu(a)).astype(xc.dtype)
        return jnp.einsum('ce,ced->cd', wgt, v)

    return lax.map(one_chunk, hc).reshape(B, L, D)


def mix_out(oa, ob, oc, w_out):
    return jnp.concatenate([oa, ob, oc], axis=-1) @ w_out


def trunk_layer(xl, xc, mod_l, mod_c, n1_g, n2_g, w_in, gqa_qn_g, gqa_kn_g, pool_w, pool_scale,
                mla_qn_g, mla_kvn_g, mla_w_uq, mla_w_ukv, w_out, peer_wq, peer_subkeys, peer_u, peer_v,
                rope_a, rope_c, update_ctx):
    shl, scl, gtl, shl2, scl2, gtl2 = [mod_l[:, i][:, None, :] for i in range(N_MOD)]
    shc, scc, gtc, shc2, scc2, gtc2 = [mod_c[i] for i in range(N_MOD)]
    hl = modulate(xl, n1_g, shl, scl)
    hc = modulate(xc, n1_g, shc, scc)
    aq_l, ak_l, av_l, b_l, cq_l, ckv_l, kr_l = split_cols(hl @ w_in)
    aq_c, ak_c, av_c, b_c, cq_c, ckv_c, kr_c = split_cols(hc @ w_in)
    qa_l, ka_l, va_l = gqa_heads(aq_l, ak_l, av_l, gqa_qn_g, gqa_kn_g, rope_a)
    qa_c, ka_c, va_c = gqa_heads(aq_c, ak_c, av_c, gqa_qn_g, gqa_kn_g, None)
    sa = HEAD_DIM ** -0.5
    oa_l = blocked_attention(qa_l, jnp.concatenate([ka_c, ka_l], axis=1), jnp.concatenate([va_c, va_l], axis=1), sa)
    qc_l, kc_l, vc_l = mla_heads(cq_l, ckv_l, kr_l, mla_qn_g, mla_kvn_g, mla_w_uq, mla_w_ukv, rope_c)
    qc_c, kc_c, vc_c = mla_heads(cq_c, ckv_c, kr_c, mla_qn_g, mla_kvn_g, mla_w_uq, mla_w_ukv, None)
    sc = (MLA_NOPE_DIM + MLA_ROPE_DIM) ** -0.5
    oc_l = blocked_attention(qc_l, jnp.concatenate([kc_c, kc_l], axis=1), jnp.concatenate([vc_c, vc_l], axis=1), sc)
    ob_l = pool_mixer(b_l, pool_w, pool_scale)
    xl_new = xl + gtl * mix_out(oa_l, ob_l, oc_l, w_out)
    xl_new = xl_new + gtl2 * peer_ffn(modulate(xl_new, n2_g, shl2, scl2), peer_wq, peer_subkeys, peer_u, peer_v)
    if update_ctx:
        oa_c = blocked_attention(qa_c, ka_c, va_c, sa)
        oc_c = blocked_attention(qc_c, kc_c, vc_c, sc)
        ob_c = pool_mixer(b_c, pool_w, pool_scale)
        xc_new = xc + gtc * mix_out(oa_c, ob_c, oc_c, w_out)
        xc_new = xc_new + gtc2 * peer_ffn(modulate(xc_new, n2_g, shc2, scc2), peer_wq, peer_subkeys, peer_u, peer_v)
    else:
        xc_new = xc
    return xl_new, xc_new


def setup_inputs(seed: int = 0) -> dict:
    key = jax.random.key(seed)
    ks = jax.random.split(key, 32)
    D = D_MODEL

    def nrm(k, shape, scale):
        return jax.random.normal(k, shape, jnp.float32) * scale

    def gain(k, shape):
        return 1.0 + 0.1 * jax.random.normal(k, shape, jnp.float32)

    return {
        "x": nrm(ks[0], (BATCH, SEQ, D), 1.0),
        "c": nrm(ks[1], (BATCH, D), 1.0),
        "ctx": nrm(ks[2], (BATCH, CTX_LEN, D), 1.0),
        "c_ctx": nrm(ks[3], (D,), 1.0),
        "ada_w": nrm(ks[4], (DEPTH, D, N_MOD * D), 0.5 * D ** -0.5),
        "ada_b": nrm(ks[5], (DEPTH, N_MOD * D), 0.02),
        "norm1_g": gain(ks[6], (DEPTH, D)),
        "norm2_g": gain(ks[7], (DEPTH, D)),
        "w_in": nrm(ks[8], (DEPTH, D, IN_COLS), D ** -0.5),
        "gqa_qn_g": gain(ks[9], (DEPTH, HEAD_DIM)),
        "gqa_kn_g": gain(ks[10], (DEPTH, HEAD_DIM)),
        "pool_w": nrm(ks[11], (DEPTH, POOL_GROUPS, POOL_GROUP_DIM, POOL_GROUP_DIM), POOL_GROUP_DIM ** -0.5),
        "pool_scale": gain(ks[12], (DEPTH, POOL_WIDTH)),
        "mla_qn_g": gain(ks[13], (DEPTH, MLA_Q_RANK)),
        "mla_kvn_g": gain(ks[14], (DEPTH, MLA_KV_RANK)),
        "mla_w_uq": nrm(ks[15], (DEPTH, MLA_Q_RANK, MLA_HEADS * (MLA_NOPE_DIM + MLA_ROPE_DIM)), MLA_Q_RANK ** -0.5),
        "mla_w_ukv": nrm(ks[16], (DEPTH, MLA_KV_RANK, MLA_HEADS * (MLA_NOPE_DIM + MLA_V_DIM)), MLA_KV_RANK ** -0.5),
        "w_out": nrm(ks[17], (DEPTH, MIX_WIDTH, D), MIX_WIDTH ** -0.5),
        "peer_wq": nrm(ks[18], (DEPTH, D, PEER_HEADS * PEER_QUERY_DIM), D ** -0.5),
        "peer_subkeys": nrm(ks[19], (DEPTH, PEER_HEADS, 2, PEER_N_KEYS, PEER_HALF), PEER_HALF ** -0.5),
        "peer_u": nrm(ks[20], (DEPTH, PEER_N_EXPERTS, D), D ** -0.5),
        "peer_v": nrm(ks[21], (DEPTH, PEER_N_EXPERTS, D), PEER_HEADS ** -0.5),
        "final_g": gain(ks[22], (D,)),
    }


def reference(x, c, ctx, c_ctx, ada_w, ada_b, norm1_g, norm2_g, w_in, gqa_qn_g, gqa_kn_g, pool_w, pool_scale,
              mla_qn_g, mla_kvn_g, mla_w_uq, mla_w_ukv, w_out, peer_wq, peer_subkeys, peer_u, peer_v, final_g):
    B, L, D = x.shape
    rows = L // GRID_W
    rope_a = axial_rope(rows, HEAD_DIM)
    rope_c = axial_rope(rows, MLA_ROPE_DIM)
    sc_lat = jax.nn.silu(c)
    sc_ctx = jax.nn.silu(c_ctx)
    xl, xc = x, ctx
    for layer in range(DEPTH):
        mod_l = (sc_lat @ ada_w[layer] + ada_b[layer]).reshape(B, N_MOD, D)
        mod_c = (sc_ctx @ ada_w[layer] + ada_b[layer]).reshape(N_MOD, D)
        xl, xc = trunk_layer(xl, xc, mod_l, mod_c, norm1_g[layer], norm2_g[layer], w_in[layer],
                             gqa_qn_g[layer], gqa_kn_g[layer], pool_w[layer], pool_scale[layer],
                             mla_qn_g[layer], mla_kvn_g[layer], mla_w_uq[layer], mla_w_ukv[layer], w_out[layer],
                             peer_wq[layer], peer_subkeys[layer], peer_u[layer], peer_v[layer],
                             rope_a, rope_c, layer < DEPTH - 1)
    return rms_norm(xl, final_g)
```

```python
import contextlib
import numpy as np
import concourse.bass as bass
import concourse.mybir as mybir
from concourse.bass_utils import run_bass_kernel_spmd

F32 = mybir.dt.float32
I32 = mybir.dt.int32
U32 = mybir.dt.uint32
ALU = mybir.AluOpType
AF = mybir.ActivationFunctionType
AX = mybir.AxisListType

D = 1024
KD = 8
NMOD = 6
EPS = 1e-6
GRID_W = 64
HD = 64
INC = 1568
NEXP = 16384
N_CORES = 8


class Buf:
    def __init__(self, ap, dsem=None, psum=False):
        self.ap = ap
        self.psum = psum
        self.ws = {}
        self.rs = {}
        self.dsem = dsem

    def __getitem__(self, k):
        return self.ap[k]


class DSem:
    def __init__(self, sem):
        self.sem = sem
        self.cnt = 0


class Eng:
    def __init__(self, name, e, sem, is_pe=False):
        self.name = name
        self.e = e
        self.sem = sem
        self.cnt = 0
        self.seen = {}
        self.is_pe = is_pe
        self.n = 0

    def wait(self, sem, val):
        if val <= 0 or self.seen.get(sem, 0) >= val:
            return
        self.e.wait_ge(sem, val)
        self.seen[sem] = val
        self.n += 1


class KB:
    def __init__(self, nc, es, n_dsem=64):
        self.nc = nc
        self.es = es
        mk = lambda n: es.enter_context(nc.semaphore(n))
        self.pe = Eng("pe", nc.tensor, mk("s_pe"), is_pe=True)
        self.act = Eng("act", nc.scalar, mk("s_act"))
        self.dve = Eng("dve", nc.vector, mk("s_dve"))
        self.pool = Eng("pool", nc.gpsimd, mk("s_pool"))
        self.sp = Eng("sp", nc.sync, None)
        self.engs = [self.pe, self.act, self.dve, self.pool, self.sp]
        self.dsems = [DSem(mk("s_d%d" % i)) for i in range(n_dsem)]
        self.dfree = 0
        self.budget = None
        self.nops = 0
        self.gsems = [DSem(mk("s_g%d" % i)) for i in range(16)]

    def new_phase(self):
        self.barrier()
        self.dfree = 0

    def dsem(self):
        d = self.dsems[self.dfree]
        self.dfree += 1
        return d

    def _deps(self, eng, r, w, aw):
        deps = {}
        def need(dd):
            for s, v in dd.items():
                if deps.get(s, 0) < v:
                    deps[s] = v
        for b in r:
            need(b.ws)
            if b.psum:
                need(b.rs)
        for b in w:
            need(b.ws)
            need(b.rs)
        for b in aw:
            need(b.rs)
            need(b.ws)
        for s, v in deps.items():
            if eng.is_pe and s is eng.sem:
                continue
            eng.wait(s, v)

    def op(self, eng, fn, r=(), w=(), aw=()):
        self.nops += 1
        if self.budget is not None and self.nops > self.budget:
            return None
        self._deps(eng, r, w, aw)
        inst = fn(eng.e)
        eng.cnt += 1
        eng.n += 1
        inst.then_inc(eng.sem, 1)
        s, c = eng.sem, eng.cnt
        for b in r:
            b.rs[s] = c
        for b in w:
            b.ws = {s: c}
            b.rs = {}
        for b in aw:
            b.ws[s] = c
        return inst

    def dma(self, q, out, in_, slot, r=(), w=(), aw=(), fn=None):
        self.nops += 1
        if self.budget is not None and self.nops > self.budget:
            return None
        self._deps(q, r, w, aw)
        if fn is None:
            inst = q.e.dma_start(out=out, in_=in_)
        else:
            inst = fn(q.e)
        q.n += 1
        d = slot.dsem
        d.cnt += 16
        inst.then_inc(d.sem, 16)
        s, c = d.sem, d.cnt
        for b in r:
            b.rs[s] = c
        for b in w:
            b.ws = {s: c}
            b.rs = {}
        for b in aw:
            b.ws[s] = c
        return inst

    def barrier(self):
        allv = {}
        for e in self.engs:
            if e.sem is not None and e.cnt > 0:
                allv[e.sem] = e.cnt
        for d in self.dsems + self.gsems:
            if d.cnt > 0:
                allv[d.sem] = d.cnt
        for e in self.engs:
            for s, v in allv.items():
                e.wait(s, v)


def v3(ap, a, b):
    return ap.rearrange("p (a b) -> p a b", a=a, b=b)


def v4(ap, a, b, c):
    return ap.rearrange("p (a b c) -> p a b c", a=a, b=b, c=c)


class Arena:
    def __init__(self, t, ncols):
        self.t = t
        self.n = ncols
        self.off = 0

    def reset(self):
        self.off = 0

    def take(self, ncols, parts=128):
        assert self.off + ncols <= self.n, ("arena overflow", self.off, ncols, self.n)
        ap = self.t[0:parts, self.off:self.off + ncols]
        self.off += ncols
        return ap


def peer_routing(kb, ar, sS, iota16, eid_i, gate, eoff=0, cache=None):
    dve, act, pool = kb.dve, kb.act, kb.pool
    if cache is None:
        cache = {}
    names = iter(range(1000))
    def B(n, parts=128):
        k = next(names)
        if k not in cache:
            cache[k] = Buf(ar.take(n, parts))
        return cache[k]
    sv = B(256)
    si = B(256)
    sif = B(256)
    wk = B(128)
    cs = B(2048)
    wk2 = B(256)
    ts = B(128)
    pos = B(128)
    posf = B(128)
    pbf = B(128)
    paf = B(128)
    oh = B(2048)
    i1s = B(128)
    i2s = B(128)
    eidf = B(128)
    ex = B(128)
    sm = B(8)
    s3 = v3(sS.ap, 16, 128)
    sv3 = v3(sv.ap, 16, 16)
    siu = si.ap.bitcast(U32)
    si3 = v3(siu, 16, 16)
    for hp in range(16):
        kb.op(dve, lambda e: e.max(out=sv3[:, hp, 0:8], in_=s3[:, hp, :]), r=[sS], aw=[sv])
        kb.op(dve, lambda e: e.max_index(out=si3[:, hp, 0:8], in_max=sv3[:, hp, 0:8], in_values=s3[:, hp, :]), r=[sS, sv], aw=[si])
        kb.op(dve, lambda e: e.match_replace(out=wk.ap, in_to_replace=sv3[:, hp, 0:8], in_values=s3[:, hp, :], imm_value=-1e30), r=[sS, sv], w=[wk])
        kb.op(dve, lambda e: e.max(out=sv3[:, hp, 8:16], in_=wk.ap), r=[wk], aw=[sv])
        kb.op(dve, lambda e: e.max_index(out=si3[:, hp, 8:16], in_max=sv3[:, hp, 8:16], in_values=wk.ap), r=[wk, sv], aw=[si])
    kb.op(dve, lambda e: e.tensor_copy(out=sif.ap, in_=siu), r=[si], w=[sif])
    sv4 = v4(sv.ap, 8, 2, 16)
    sif4 = v4(sif.ap, 8, 2, 16)
    cs4 = v4(cs.ap, 8, 16, 16)
    shp = [128, 8, 16, 16]
    kb.op(dve, lambda e: e.tensor_tensor(out=cs4, in0=sv4[:, :, 0, :].unsqueeze(3).to_broadcast(shp),
                                         in1=sv4[:, :, 1, :].unsqueeze(2).to_broadcast(shp), op=ALU.add), r=[sv], w=[cs])
    cs3 = v3(cs.ap, 8, 256)
    ts3 = v3(ts.ap, 8, 16)
    posu = pos.ap.bitcast(U32)
    pos3 = v3(posu, 8, 16)
    for h in range(8):
        kb.op(dve, lambda e: e.max(out=ts3[:, h, 0:8], in_=cs3[:, h, :]), r=[cs], aw=[ts])
        kb.op(dve, lambda e: e.max_index(out=pos3[:, h, 0:8], in_max=ts3[:, h, 0:8], in_values=cs3[:, h, :]), r=[cs, ts], aw=[pos])
        kb.op(dve, lambda e: e.match_replace(out=wk2.ap, in_to_replace=ts3[:, h, 0:8], in_values=cs3[:, h, :], imm_value=-1e30), r=[cs, ts], w=[wk2])
        kb.op(dve, lambda e: e.max(out=ts3[:, h, 8:16], in_=wk2.ap), r=[wk2], aw=[ts])
        kb.op(dve, lambda e: e.max_index(out=pos3[:, h, 8:16], in_max=ts3[:, h, 8:16], in_values=wk2.ap), r=[wk2, ts], aw=[pos])
    kb.op(dve, lambda e: e.tensor_copy(out=posf.ap, in_=posu), r=[pos], w=[posf])
    oh4 = v4(oh.ap, 8, 16, 16)
    oh2 = cs
    oh2_4 = v4(oh2.ap, 8, 16, 16)
    io_i = iota16.ap[:, 0:16].unsqueeze(1).unsqueeze(1).to_broadcast(shp)
    io_lo = iota16.ap[:, 16:32].unsqueeze(1).unsqueeze(1).to_broadcast(shp)
    io_hi = iota16.ap[:, 32:48].unsqueeze(1).unsqueeze(1).to_broadcast(shp)
    posb = v3(posf.ap, 8, 16).unsqueeze(3).to_broadcast(shp)
    kb.op(dve, lambda e: e.tensor_tensor(out=oh4, in0=posb, in1=io_lo, op=ALU.is_ge), r=[posf, iota16], w=[oh])
    kb.op(dve, lambda e: e.tensor_tensor(out=oh2_4, in0=posb, in1=io_hi, op=ALU.is_ge), r=[posf, iota16], w=[oh2])
    kb.op(dve, lambda e: e.tensor_tensor(out=oh4, in0=oh4, in1=oh2_4, op=ALU.subtract), r=[oh, oh2], w=[oh])
    kb.op(dve, lambda e: e.tensor_tensor(out=oh2_4, in0=oh4, in1=io_i, op=ALU.mult), r=[oh, iota16], w=[oh2])
    kb.op(dve, lambda e: e.reduce_sum(out=paf.ap, in_=v3(oh2.ap, 128, 16), axis=AX.X), r=[oh2], w=[paf])
    kb.op(dve, lambda e: e.tensor_tensor(out=oh2_4, in0=oh4, in1=sif4[:, :, 0, :].unsqueeze(2).to_broadcast(shp), op=ALU.mult), r=[oh, sif], w=[oh2])
    kb.op(dve, lambda e: e.reduce_sum(out=i1s.ap, in_=v3(oh2.ap, 128, 16), axis=AX.X), r=[oh2], w=[i1s])
    kb.op(dve, lambda e: e.scalar_tensor_tensor(out=pbf.ap, in0=paf.ap, scalar=-16.0, in1=posf.ap, op0=ALU.mult, op1=ALU.add), r=[paf, posf], w=[pbf])
    kb.op(dve, lambda e: e.tensor_tensor(out=oh4, in0=v3(pbf.ap, 8, 16).unsqueeze(3).to_broadcast(shp), in1=io_i, op=ALU.is_equal), r=[pbf, iota16], w=[oh])
    kb.op(dve, lambda e: e.tensor_tensor(out=oh4, in0=oh4, in1=sif4[:, :, 1, :].unsqueeze(2).to_broadcast(shp), op=ALU.mult), r=[oh, sif], w=[oh])
    kb.op(dve, lambda e: e.reduce_sum(out=i2s.ap, in_=v3(oh.ap, 128, 16), axis=AX.X), r=[oh], w=[i2s])
    kb.op(dve, lambda e: e.scalar_tensor_tensor(out=eidf.ap, in0=i1s.ap, scalar=128.0, in1=i2s.ap, op0=ALU.mult, op1=ALU.add), r=[i1s, i2s], w=[eidf])
    if eoff:
        kb.op(dve, lambda e: e.tensor_scalar_add(out=eidf.ap, in0=eidf.ap, scalar1=float(eoff)), r=[eidf], w=[eidf])
    kb.op(dve, lambda e: e.tensor_copy(out=eid_i.ap.bitcast(I32), in_=eidf.ap), r=[eidf], w=[eid_i])
    ex3 = v3(ex.ap, 8, 16)
    kb.op(dve, lambda e: e.tensor_tensor(out=ex3, in0=ts3, in1=ts3[:, :, 0:1].to_broadcast([128, 8, 16]), op=ALU.subtract), r=[ts], w=[ex])
    kb.op(act, lambda e: e.activation(out=ex.ap, in_=ex.ap, func=AF.Exp), r=[ex], w=[ex])
    kb.op(dve, lambda e: e.reduce_sum(out=sm.ap, in_=ex3, axis=AX.X), r=[ex], w=[sm])
    kb.op(dve, lambda e: e.reciprocal(out=sm.ap, in_=sm.ap), r=[sm], w=[sm])
    kb.op(dve, lambda e: e.tensor_tensor(out=v3(gate.ap, 8, 16), in0=ex3, in1=sm.ap.unsqueeze(2).to_broadcast([128, 8, 16]), op=ALU.mult), r=[ex, sm], w=[gate])


WNAMES = [("ada_w", None), ("ada_b", None), ("norm1_g", None), ("norm2_g", None), ("w_in", None),
          ("gqa_qn_g", None), ("gqa_kn_g", None), ("pool_w", None), ("pool_scale", None), ("mla_qn_g", None),
          ("mla_kvn_g", None), ("mla_w_uq", None), ("mla_w_ukv", None), ("w_out", None), ("peer_wq", None),
          ("peer_subkeys", None), ("final_g", None)]


def build(NB, S, CTX, DEPTH, stop=None, budget=None):
    NT_L = S // 128
    NT_C = CTX // 128
    NK = CTX + S
    NKC = NK // 128
    nc = bass.Bass("TRN2", target_bir_lowering=False)

    def din(name, shape, dtype=F32):
        return nc.dram_tensor(name, list(shape), dtype, kind="ExternalInput").ap()

    def dscr(name, shape, dtype=F32):
        return nc.dram_tensor(name, list(shape), dtype).ap()

    x_d = din("x", [NB, S, D])
    ctx_d = din("ctx", [NB, CTX, D])
    cT_d = din("cT", [D, NB + 1])
    ada_w = din("ada_w", [DEPTH, D, NMOD * D])
    ada_b = din("ada_b", [DEPTH, NMOD * D])
    norm1_g = din("norm1_g", [DEPTH, D])
    norm2_g = din("norm2_g", [DEPTH, D])
    w_in = din("w_in", [DEPTH, D, INC])
    gqa_qn_g = din("gqa_qn_g", [DEPTH, 64])
    gqa_kn_g = din("gqa_kn_g", [DEPTH, 64])
    pool_w = din("pool_w", [DEPTH, 4, 64, 64])
    pool_scale = din("pool_scale", [DEPTH, 256])
    mla_qn_g = din("mla_qn_g", [DEPTH, 384])
    mla_kvn_g = din("mla_kvn_g", [DEPTH, 256])
    mla_w_uq = din("mla_w_uq", [DEPTH, 384, 576])
    mla_w_ukv = din("mla_w_ukv", [DEPTH, 256, 768])
    w_out = din("w_out", [DEPTH, D, D])
    peer_wq = din("peer_wq", [DEPTH, D, 2048])
    peer_subkeys = din("peer_subkeys", [DEPTH, 8, 2, 128, 128])
    peer_u = din("peer_u", [DEPTH * NEXP, D])
    peer_v = din("peer_v", [DEPTH * NEXP, D])
    final_g = din("final_g", [D])
    ident_d = din("c_ident", [128, 128])
    iota_d = din("c_iota", [128, 48])
    ropeA_d = din("c_ropeA", [S, 64])
    ropeC_d = din("c_ropeC", [S, 32])
    rcL_d = din("c_rcL", [4, S])
    rcC_d = din("c_rcC", [4, CTX])
    out_d = nc.dram_tensor("out", [NB, S, D], F32, kind="ExternalOutput").ap()

    mod_d = dscr("mod_d", [DEPTH, NB + 1, NMOD * D])
    QaT_d = dscr("QaT_d", [3, 128, NK])
    KaT_d = dscr("KaT_d", [128, NK])
    Va_d = dscr("Va_d", [NK, 128])
    bT_d = dscr("bT_d", [2, 128, NK])
    obT_d = dscr("obT_d", [2, 128, NK])
    QcT_d = dscr("QcT_d", [6, 96, NK])
    KcT_d = dscr("KcT_d", [6, 96, NK])
    Vc_d = dscr("Vc_d", [NK, 384])
    mix_d = dscr("mix_d", [NK, 12, 65])
    xmid_d = dscr("xmid_d", [NK, D])
    h2_d = dscr("h2_d", [NK, D])
    eid_d = dscr("eid_d", [NK, 128], I32)
    gate_d = dscr("gate_d", [NK, 128])
    xl1_d = dscr("xl1_d", [NB, S, D])
    xc1_d = dscr("xc1_d", [NB, CTX, D])

    GC, R1C, R2C = 3840, 24704, 24616
    with contextlib.ExitStack() as es:
        kb = KB(nc, es, n_dsem=44)
        kb.budget = budget
        pe, act, dve, pool, sp = kb.pe, kb.act, kb.dve, kb.pool, kb.sp
        GT = es.enter_context(nc.sbuf_tensor("GT", [128, GC], F32))
        R1T = es.enter_context(nc.sbuf_tensor("R1T", [128, R1C], F32))
        R2T = es.enter_context(nc.sbuf_tensor("R2T", [128, R2C], F32))
        PSALL = es.enter_context(nc.psum_tensor("PSALL", [128, 4096], F32))
        G = Arena(GT, GC)
        R1 = Arena(R1T, R1C)
        R2 = Arena(R2T, R2C)

        def PSB(c0, c1, parts=128):
            return Buf(PSALL[0:parts, c0:c1], psum=True)

        def rms_a(src_ap, src_bufs, junk, ss, n):
            kb.op(act, lambda e: e.activation(out=junk.ap[:, 0:n], in_=src_ap, func=AF.Square, accum_out=ss.ap), r=src_bufs, w=[junk, ss])
            kb.op(act, lambda e: e.activation(out=ss.ap, in_=ss.ap, func=AF.Sqrt, scale=1.0 / n, bias=EPS), r=[ss], w=[ss])

        def rms_b(ss):
            kb.op(dve, lambda e: e.reciprocal(out=ss.ap, in_=ss.ap), r=[ss], w=[ss])

        def rms_rstd(src_ap, src_bufs, junk, ss, n):
            kb.op(act, lambda e: e.activation(out=junk.ap[:, 0:n], in_=src_ap, func=AF.Square, accum_out=ss.ap), r=src_bufs, w=[junk, ss])
            kb.op(act, lambda e: e.activation(out=ss.ap, in_=ss.ap, func=AF.Sqrt, scale=1.0 / n, bias=EPS), r=[ss], w=[ss])
            kb.op(dve, lambda e: e.reciprocal(out=ss.ap, in_=ss.ap), r=[ss], w=[ss])

        def transposes(src_ap_fn, n, src_bufs, ident, PT, pt_ap_fn, dst, dst_ap, cols, parts=128):
            for k in range(n):
                kb.op(pe, lambda e: e.transpose(out=pt_ap_fn(k), in_=src_ap_fn(k), identity=ident.ap), r=src_bufs + [ident],
                      w=[PT] if k == 0 else (), aw=() if k == 0 else [PT])
            kb.op(act, lambda e: e.copy(out=dst_ap, in_=PT.ap[0:parts, 0:cols]), r=[PT], w=[dst])

        ident = Buf(G.take(128), kb.dsem())
        iota = Buf(G.take(48), kb.dsem())
        ropeA = Buf(G.take(NT_L * 64), kb.dsem())
        ropeC = Buf(G.take(NT_L * 32), kb.dsem())
        FG = Buf(G.take(1024), kb.dsem())
        gq = Buf(G.take(64))
        gk = Buf(G.take(64))
        gcq = Buf(G.take(384))
        gckv = Buf(G.take(256))
        psc = Buf(G.take(2))
        PWbd = Buf(G.take(256))
        kb.dma(sp, ident.ap, ident_d, ident, w=[ident])
        kb.dma(sp, iota.ap, iota_d, iota, w=[iota])
        kb.dma(sp, v3(ropeA.ap, NT_L, 64), ropeA_d.rearrange("(n p) c -> p n c", p=128), ropeA, w=[ropeA])
        kb.dma(sp, v3(ropeC.ap, NT_L, 32), ropeC_d.rearrange("(n p) c -> p n c", p=128), ropeC, w=[ropeC])
        kb.dma(sp, FG.ap, final_g.partition_broadcast(128), FG, w=[FG])
        NB1 = NB + 1
        cT = Buf(R2.take(KD * NB1), kb.dsem())
        cT3 = v3(cT.ap, KD, NB1)
        kb.dma(sp, cT3, cT_d.rearrange("(k p) n -> p k n", p=128), cT, w=[cT])
        kb.op(act, lambda e: e.activation(out=cT.ap, in_=cT.ap, func=AF.Silu), r=[cT], w=[cT])
        adab = Buf(R2.take(NMOD * D, NB1), kb.dsem())
        g1b = Buf(R2.take(D, NB1), kb.dsem())
        g2b = Buf(R2.take(D, NB1), kb.dsem())
        modsb = Buf(R2.take(NMOD * D, NB1), kb.dsem())
        wsl = [Buf(R1.take(KD * 512), kb.dsem()) for _ in range(2)]
        pps = [PSB(0, 512, NB1), PSB(512, 1024, NB1)]
        for l in range(DEPTH):
            kb.dma(sp, adab.ap, ada_b[l].partition_broadcast(NB1), adab, w=[adab])
            kb.dma(sp, g1b.ap, norm1_g[l].partition_broadcast(NB1), g1b, w=[g1b])
            kb.dma(sp, g2b.ap, norm2_g[l].partition_broadcast(NB1), g2b, w=[g2b])
            for pc in range(12):
                ws_ = wsl[pc % 2]
                pp = pps[pc % 2]
                kb.dma(sp, v3(ws_.ap, KD, 512), ada_w[l][:, pc * 512:(pc + 1) * 512].rearrange("(k p) n -> p k n", p=128), ws_, w=[ws_])
                for k in range(KD):
                    kb.op(pe, lambda e: e.matmul(pp.ap, lhsT=cT3[:, k, :], rhs=v3(ws_.ap, KD, 512)[:, k, :], start=(k == 0), stop=(k == KD - 1)),
                          r=[cT, ws_], w=[pp] if k == 0 else (), aw=() if k == 0 else [pp])
                kb.op(dve, lambda e: e.tensor_tensor(out=modsb.ap[:, pc * 512:(pc + 1) * 512], in0=pp.ap, in1=adab.ap[:, pc * 512:(pc + 1) * 512], op=ALU.add),
                      r=[pp, adab], w=[modsb] if pc == 0 else (), aw=() if pc == 0 else [modsb])
            kb.op(dve, lambda e: e.scalar_tensor_tensor(out=modsb.ap[:, D:2 * D], in0=modsb.ap[:, D:2 * D], scalar=1.0, in1=g1b.ap, op0=ALU.add, op1=ALU.mult),
                  r=[g1b], w=[modsb])
            kb.op(dve, lambda e: e.scalar_tensor_tensor(out=modsb.ap[:, 4 * D:5 * D], in0=modsb.ap[:, 4 * D:5 * D], scalar=1.0, in1=g2b.ap, op0=ALU.add, op1=ALU.mult),
                  r=[g2b], w=[modsb])
            kb.dma(sp, mod_d[l], modsb.ap, modsb, r=[modsb])

        def bc_load(dst, l, row, i):
            kb.dma(sp, dst.ap, mod_d[l, row, i * D:(i + 1) * D].partition_broadcast(128), dst, w=[dst])

        def tok_tiles(with_ctx):
            tl = []
            if with_ctx:
                tl += [(True, i, i * 128) for i in range(NT_C)]
            tl += [(False, i, CTX + i * 128) for i in range(NT_L)]
            return tl

        def x_src(l, b, is_ctx, ti):
            if l == 0:
                return (ctx_d if is_ctx else x_d)[b, ti * 128:(ti + 1) * 128, :]
            return (xc1_d if is_ctx else xl1_d)[b, ti * 128:(ti + 1) * 128, :]

        def gen_A(l, b):
            A = lambda n, parts=128, d=False: Buf(R2.take(n, parts), kb.dsem() if d else None)
            w_in_sb = Buf(R1.take(KD * INC), kb.dsem())
            w_uq_sb = Buf(R1.take(3 * 576), kb.dsem())
            w_ukv_sb = Buf(R1.take(2 * 768), kb.dsem())
            wi3 = v3(w_in_sb.ap, KD, INC)
            wq3 = v3(w_uq_sb.ap, 3, 576)
            wkv3 = v3(w_ukv_sb.ap, 2, 768)
            kb.dma(sp, wi3, w_in[l].rearrange("(k p) n -> p k n", p=128), w_in_sb, w=[w_in_sb])
            kb.dma(sp, wq3, mla_w_uq[l].rearrange("(k p) n -> p k n", p=128), w_uq_sb, w=[w_uq_sb])
            kb.dma(sp, wkv3, mla_w_ukv[l].rearrange("(k p) n -> p k n", p=128), w_ukv_sb, w=[w_ukv_sb])
            bcs = {}
            for is_ctx in (True, False):
                row = NB if is_ctx else b
                a1 = A(D, d=True)
                sh1 = A(D, d=True)
                bc_load(a1, l, row, 1)
                bc_load(sh1, l, row, 0)
                bcs[is_ctx] = (a1, sh1)
            xts = [A(D, d=True) for _ in range(2)]
            h = A(D)
            hT = A(D)
            pj = A(INC, d=True)
            sq = A(512)
            ss = A(1); ss8 = A(8); ssq = A(1); ssk = A(1)
            qkn = A(512)
            qkr = A(512)
            t1 = A(256); t2 = A(256); t3 = A(256); t4 = A(256)
            qkT = A(512, d=True)
            bTs = A(256, d=True)
            cqn = A(384)
            cqnT = A(384)
            qm = A(576)
            u1 = A(96); u2 = A(96); u3 = A(96); u4 = A(96)
            qcT = A(768, 96, d=True)
            ckvn = A(256)
            ckvnT = A(256)
            kfull = A(576)
            vc = A(384, d=True)
            kcT = A(768, 96, d=True)
            krr = A(32)
            k1 = A(16); k2 = A(16); k3 = A(16); k4 = A(16)
            PT = PSB(0, 1024)
            PJ = PSB(1024, 3072)
            PX = PSB(3072, 4096)
            tiles = tok_tiles(True)

            def load_x(i):
                is_ctx, ti, tk0 = tiles[i]
                xt = xts[i % 2]
                kb.dma(sp, xt.ap, x_src(l, b, is_ctx, ti), xt, w=[xt])

            load_x(0)
            for i, (is_ctx, ti, tk0) in enumerate(tiles):
                if i + 1 < len(tiles):
                    load_x(i + 1)
                xt = xts[i % 2]
                a1, sh1 = bcs[is_ctx]
                rms_a(xt.ap, [xt], h, ss, D)
                yield
                rms_b(ss)
                kb.op(dve, lambda e: e.scalar_tensor_tensor(out=h.ap, in0=xt.ap, scalar=ss.ap[:, 0:1], in1=a1.ap, op0=ALU.mult, op1=ALU.mult), r=[xt, ss, a1], w=[h])
                kb.op(dve, lambda e: e.tensor_tensor(out=h.ap, in0=h.ap, in1=sh1.ap, op=ALU.add), r=[sh1], w=[h])
                transposes(lambda k: h.ap[:, k * 128:(k + 1) * 128], KD, [h], ident, PT, lambda k: PT.ap[:, k * 128:(k + 1) * 128], hT, hT.ap, 1024)
                hT3 = v3(hT.ap, KD, 128)
                for pc, (c0, c1) in enumerate(((0, 512), (512, 1024), (1024, 1536), (1536, INC))):
                    for k in range(KD):
                        kb.op(pe, lambda e: e.matmul(PJ.ap[:, c0:c1], lhsT=hT3[:, k, :], rhs=wi3[:, k, c0:c1], start=(k == 0), stop=(k == KD - 1)),
                              r=[hT, w_in_sb], w=[PJ] if (pc == 0 and k == 0) else (), aw=() if (pc == 0 and k == 0) else [PJ])
                kb.op(act, lambda e: e.copy(out=pj.ap, in_=PJ.ap[:, 0:INC]), r=[PJ], w=[pj])
                yield
                kb.op(dve, lambda e: e.tensor_tensor(out=sq.ap, in0=pj.ap[:, 0:512], in1=pj.ap[:, 0:512], op=ALU.mult), r=[pj], w=[sq])
                kb.op(dve, lambda e: e.reduce_sum(out=ss8.ap, in_=v3(sq.ap, 8, 64), axis=AX.X), r=[sq], w=[ss8])
                kb.op(act, lambda e: e.activation(out=ss8.ap, in_=ss8.ap, func=AF.Sqrt, scale=1.0 / 64, bias=EPS), r=[ss8], w=[ss8])
                yield
                kb.op(dve, lambda e: e.reciprocal(out=ss8.ap, in_=ss8.ap), r=[ss8], w=[ss8])
                qkn3 = v3(qkn.ap, 8, 64)
                kb.op(dve, lambda e: e.tensor_tensor(out=qkn3, in0=v3(pj.ap[:, 0:512], 8, 64), in1=ss8.ap.unsqueeze(2).to_broadcast([128, 8, 64]), op=ALU.mult), r=[pj, ss8], w=[qkn])
                kb.op(dve, lambda e: e.tensor_tensor(out=qkn3[:, 0:6, :], in0=qkn3[:, 0:6, :], in1=gq.ap.unsqueeze(1).to_broadcast([128, 6, 64]), op=ALU.mult), r=[gq], w=[qkn])
                kb.op(dve, lambda e: e.tensor_tensor(out=qkn3[:, 6:8, :], in0=qkn3[:, 6:8, :], in1=gk.ap.unsqueeze(1).to_broadcast([128, 2, 64]), op=ALU.mult), r=[gk], w=[qkn])
                if is_ctx:
                    qsrc = qkn
                else:
                    qsrc = qkr
                    q4 = v4(qkn.ap, 8, 2, 32)
                    r4 = v4(qkr.ap, 8, 2, 32)
                    rA = v3(ropeA.ap, NT_L, 64)
                    cosb = rA[:, ti, 0:32].unsqueeze(1).to_broadcast([128, 8, 32])
                    sinb = rA[:, ti, 32:64].unsqueeze(1).to_broadcast([128, 8, 32])
                    T = lambda t: v3(t.ap, 8, 32)
                    kb.op(dve, lambda e: e.tensor_tensor(out=T(t1), in0=q4[:, :, 0, :], in1=cosb, op=ALU.mult), r=[qkn, ropeA], w=[t1])
                    kb.op(dve, lambda e: e.tensor_tensor(out=T(t2), in0=q4[:, :, 1, :], in1=sinb, op=ALU.mult), r=[qkn, ropeA], w=[t2])
                    kb.op(dve, lambda e: e.tensor_tensor(out=r4[:, :, 0, :], in0=T(t1), in1=T(t2), op=ALU.subtract), r=[t1, t2], w=[qkr])
                    kb.op(dve, lambda e: e.tensor_tensor(out=T(t3), in0=q4[:, :, 0, :], in1=sinb, op=ALU.mult), r=[qkn, ropeA], w=[t3])
                    kb.op(dve, lambda e: e.tensor_tensor(out=T(t4), in0=q4[:, :, 1, :], in1=cosb, op=ALU.mult), r=[qkn, ropeA], w=[t4])
                    kb.op(dve, lambda e: e.tensor_tensor(out=r4[:, :, 1, :], in0=T(t3), in1=T(t4), op=ALU.add), r=[t3, t4], aw=[qkr])
                transposes(lambda k: qsrc.ap[:, k * 128:(k + 1) * 128], 4, [qsrc], ident, PJ, lambda k: PJ.ap[:, k * 128:(k + 1) * 128], qkT, qkT.ap, 512)
                qkT3 = v3(qkT.ap, 4, 128)
                kb.dma(sp, QaT_d[:, :, tk0:tk0 + 128].rearrange("j p t -> p j t"), qkT3[:, 0:3, :], qkT, r=[qkT])
                kb.dma(sp, KaT_d[:, tk0:tk0 + 128], qkT3[:, 3, :], qkT, r=[qkT])
                kb.dma(sp, Va_d[tk0:tk0 + 128, :], pj.ap[:, 512:640], pj, r=[pj])
                transposes(lambda k: pj.ap[:, 640 + k * 128:640 + (k + 1) * 128], 2, [pj], ident, PX, lambda k: PX.ap[:, k * 128:(k + 1) * 128], bTs, bTs.ap, 256)
                kb.dma(sp, bT_d[:, :, tk0:tk0 + 128].rearrange("j p t -> p j t"), v3(bTs.ap, 2, 128), bTs, r=[bTs])
                rms_a(pj.ap[:, 896:1280], [pj], cqn, ssq, 384)
                yield
                rms_b(ssq)
                kb.op(dve, lambda e: e.scalar_tensor_tensor(out=cqn.ap, in0=pj.ap[:, 896:1280], scalar=ssq.ap[:, 0:1], in1=gcq.ap, op0=ALU.mult, op1=ALU.mult), r=[pj, ssq, gcq], w=[cqn])
                transposes(lambda k: cqn.ap[:, k * 128:(k + 1) * 128], 3, [cqn], ident, PT, lambda k: PT.ap[:, k * 128:(k + 1) * 128], cqnT, cqnT.ap, 384)
                cq3 = v3(cqnT.ap, 3, 128)
                for pc, (c0, c1) in enumerate(((0, 512), (512, 576))):
                    for k in range(3):
                        kb.op(pe, lambda e: e.matmul(PX.ap[:, c0:c1], lhsT=cq3[:, k, :], rhs=wq3[:, k, c0:c1], start=(k == 0), stop=(k == 2)),
                              r=[cqnT, w_uq_sb], w=[PX] if (pc == 0 and k == 0) else (), aw=() if (pc == 0 and k == 0) else [PX])
                kb.op(act, lambda e: e.copy(out=qm.ap, in_=PX.ap[:, 0:576]), r=[PX], w=[qm])
                qm3 = v3(qm.ap, 6, 96)
                if not is_ctx:
                    yield
                    rC = v3(ropeC.ap, NT_L, 32)
                    cosc = rC[:, ti, 0:16].unsqueeze(1).to_broadcast([128, 6, 16])
                    sinc = rC[:, ti, 16:32].unsqueeze(1).to_broadcast([128, 6, 16])
                    U = lambda t: v3(t.ap, 6, 16)
                    x1 = qm3[:, :, 64:80]
                    x2 = qm3[:, :, 80:96]
                    kb.op(dve, lambda e: e.tensor_tensor(out=U(u1), in0=x1, in1=cosc, op=ALU.mult), r=[qm, ropeC], w=[u1])
                    kb.op(dve, lambda e: e.tensor_tensor(out=U(u2), in0=x2, in1=sinc, op=ALU.mult), r=[qm, ropeC], w=[u2])
                    kb.op(dve, lambda e: e.tensor_tensor(out=U(u3), in0=x1, in1=sinc, op=ALU.mult), r=[qm, ropeC], w=[u3])
                    kb.op(dve, lambda e: e.tensor_tensor(out=U(u4), in0=x2, in1=cosc, op=ALU.mult), r=[qm, ropeC], w=[u4])
                    kb.op(dve, lambda e: e.tensor_tensor(out=x1, in0=U(u1), in1=U(u2), op=ALU.subtract), r=[u1, u2, u3, u4], w=[qm])
                    kb.op(dve, lambda e: e.tensor_tensor(out=x2, in0=U(u3), in1=U(u4), op=ALU.add), r=[u3, u4], w=[qm])
                transposes(lambda hh: qm3[:, hh, :], 6, [qm], ident, PT, lambda hh: PSALL[0:96, hh * 128:(hh + 1) * 128], qcT, qcT.ap, 768, parts=96)
                kb.dma(sp, QcT_d[:, :, tk0:tk0 + 128].rearrange("h p t -> p h t"), v3(qcT.ap, 6, 128), qcT, r=[qcT])
                rms_a(pj.ap[:, 1280:1536], [pj], ckvn, ssk, 256)
                yield
                rms_b(ssk)
                kb.op(dve, lambda e: e.scalar_tensor_tensor(out=ckvn.ap, in0=pj.ap[:, 1280:1536], scalar=ssk.ap[:, 0:1], in1=gckv.ap, op0=ALU.mult, op1=ALU.mult), r=[pj, ssk, gckv], w=[ckvn])
                transposes(lambda k: ckvn.ap[:, k * 128:(k + 1) * 128], 2, [ckvn], ident, PJ, lambda k: PJ.ap[:, k * 128:(k + 1) * 128], ckvnT, ckvnT.ap, 256)
                ck3 = v3(ckvnT.ap, 2, 128)
                for pc, (c0, c1) in enumerate(((0, 512), (512, 768))):
                    for k in range(2):
                        kb.op(pe, lambda e: e.matmul(PX.ap[:, c0:c1], lhsT=ck3[:, k, :], rhs=wkv3[:, k, c0:c1], start=(k == 0), stop=(k == 1)),
                              r=[ckvnT, w_ukv_sb], w=[PX] if (pc == 0 and k == 0) else (), aw=() if (pc == 0 and k == 0) else [PX])
                kf3 = v3(kfull.ap, 6, 96)
                px3 = v3(PX.ap[:, 0:768], 6, 128)
                yield
                kb.op(dve, lambda e: e.tensor_copy(out=kf3[:, :, 0:64], in_=px3[:, :, 0:64]), r=[PX], w=[kfull])
                kb.op(act, lambda e: e.copy(out=v3(vc.ap, 6, 64), in_=px3[:, :, 64:128]), r=[PX], w=[vc])
                kb.dma(sp, Vc_d[tk0:tk0 + 128, :], vc.ap, vc, r=[vc])
                if is_ctx:
                    krsrc_ap, krsrc = pj.ap[:, 1536:1568], pj
                else:
                    rC = v3(ropeC.ap, NT_L, 32)
                    cos1 = rC[:, ti, 0:16]
                    sin1 = rC[:, ti, 16:32]
                    y1 = pj.ap[:, 1536:1552]
                    y2 = pj.ap[:, 1552:1568]
                    kb.op(dve, lambda e: e.tensor_tensor(out=k1.ap, in0=y1, in1=cos1, op=ALU.mult), r=[pj, ropeC], w=[k1])
                    kb.op(dve, lambda e: e.tensor_tensor(out=k2.ap, in0=y2, in1=sin1, op=ALU.mult), r=[pj, ropeC], w=[k2])
                    kb.op(dve, lambda e: e.tensor_tensor(out=k3.ap, in0=y1, in1=sin1, op=ALU.mult), r=[pj, ropeC], w=[k3])
                    kb.op(dve, lambda e: e.tensor_tensor(out=k4.ap, in0=y2, in1=cos1, op=ALU.mult), r=[pj, ropeC], w=[k4])
                    kb.op(dve, lambda e: e.tensor_tensor(out=krr.ap[:, 0:16], in0=k1.ap, in1=k2.ap, op=ALU.subtract), r=[k1, k2], w=[krr])
                    kb.op(dve, lambda e: e.tensor_tensor(out=krr.ap[:, 16:32], in0=k3.ap, in1=k4.ap, op=ALU.add), r=[k3, k4], aw=[krr])
                    krsrc_ap, krsrc = krr.ap, krr
                kb.op(dve, lambda e: e.tensor_copy(out=kf3[:, :, 64:96], in_=krsrc_ap.unsqueeze(1).to_broadcast([128, 6, 32])), r=[krsrc], aw=[kfull])
                transposes(lambda hh: kf3[:, hh, :], 6, [kfull], ident, PT, lambda hh: PSALL[0:96, hh * 128:(hh + 1) * 128], kcT, kcT.ap, 768, parts=96)
                kb.dma(sp, KcT_d[:, :, tk0:tk0 + 128].rearrange("h p t -> p h t"), v3(kcT.ap, 6, 128), kcT, r=[kcT])
                yield

        def phase_P(l, b, with_ctx):
            kb.new_phase()
            R1.reset()
            R2.reset()
            Lmax = S
            Lp = Lmax + 16
            xb = Buf(R1.take(2 * Lp), kb.dsem())
            Wa = Buf(R1.take(2 * Lp))
            Wb = Buf(R1.take(2 * Lp))
            PTb = Buf(R1.take(2 * Lmax))
            rc = Buf(R1.take(4 * Lmax), kb.dsem())
            obs = [Buf(R2.take(512), kb.dsem()) for _ in range(2)]
            pbs = [PSB(i * 512, (i + 1) * 512) for i in range(4)]
            streams = ([(CTX, 0, rcC_d)] if with_ctx else []) + [(S, CTX, rcL_d)]
            nmm = 0
            for (L, tk0, rc_d) in streams:
                Lq = L + 16
                x3 = v3(xb.ap[:, 0:2 * Lq], 2, Lq)
                a3 = v3(Wa.ap[:, 0:2 * Lq], 2, Lq)
                b3 = v3(Wb.ap[:, 0:2 * Lq], 2, Lq)
                p3 = v3(PTb.ap[:, 0:2 * L], 2, L)
                r3 = v3(rc.ap[:, 0:4 * L], 4, L)
                kb.op(pool, lambda e: e.memset(xb.ap[:, 0:2 * Lq], 0.0), w=[xb])
                kb.dma(sp, x3[:, :, 8:8 + L], bT_d[:, :, tk0:tk0 + L].rearrange("j p t -> p j t"), xb, w=[xb])
                kb.dma(sp, rc.ap[:, 0:4 * L], rc_d.rearrange("w s -> (w s)").partition_broadcast(128), rc, w=[rc])
                kb.op(dve, lambda e: e.tensor_tensor(out=a3[:, :, 1:Lq - 1], in0=x3[:, :, 0:Lq - 2], in1=x3[:, :, 1:Lq - 1], op=ALU.add), r=[xb], w=[Wa])

                def grp(g, Wsrc, w3):
                    j, p0 = g // 2, (g % 2) * 64
                    kb.op(dve, lambda e: e.tensor_tensor(out=p3[p0:p0 + 64, j, :], in0=w3[p0:p0 + 64, j, 8:8 + L], in1=r3[p0:p0 + 64, g, :], op=ALU.mult), r=[Wsrc, rc], aw=[PTb])
                    kb.op(dve, lambda e: e.tensor_tensor(out=p3[p0:p0 + 64, j, :], in0=p3[p0:p0 + 64, j, :], in1=x3[p0:p0 + 64, j, 8:8 + L], op=ALU.subtract), r=[xb], aw=[PTb])

                grp(0, Wa, a3)
                kb.op(dve, lambda e: e.tensor_tensor(out=b3[:, :, 2:Lq - 2], in0=a3[:, :, 1:Lq - 3], in1=a3[:, :, 3:Lq - 1], op=ALU.add), r=[Wa], w=[Wb])
                grp(1, Wb, b3)
                kb.op(dve, lambda e: e.tensor_tensor(out=a3[:, :, 4:Lq - 4], in0=b3[:, :, 2:Lq - 6], in1=b3[:, :, 6:Lq - 2], op=ALU.add), r=[Wb], w=[Wa])
                grp(2, Wa, a3)
                kb.op(dve, lambda e: e.tensor_tensor(out=b3[:, :, 8:Lq - 8], in0=a3[:, :, 4:Lq - 12], in1=a3[:, :, 12:Lq - 4], op=ALU.add), r=[Wa], w=[Wb])
                grp(3, Wb, b3)
                for j in range(2):
                    for c0 in range(0, L, 512):
                        cn = min(512, L - c0)
                        pb_ = pbs[nmm % 4]
                        ob = obs[nmm % 2]
                        nmm += 1
                        kb.op(pe, lambda e: e.matmul(pb_.ap[:, 0:cn], lhsT=v3(PWbd.ap, 2, 128)[:, j, :], rhs=p3[:, j, c0:c0 + cn], start=True, stop=True), r=[PWbd, PTb], w=[pb_])
                        kb.op(act, lambda e: e.activation(out=ob.ap[:, 0:cn], in_=pb_.ap[:, 0:cn], func=AF.Identity, scale=psc.ap[:, j:j + 1]), r=[pb_, psc], w=[ob])
                        kb.dma(sp, obT_d[j, :, tk0 + c0:tk0 + c0 + cn], ob.ap[:, 0:cn], ob, r=[ob])

        def gen_B(l, b, with_ctx):
            A = lambda n, parts=128, d=False: Buf(R2.take(n, parts), kb.dsem() if d else None)
            kTs = [A(NK, 96, d=True) for _ in range(2)]
            qTs = [A(NK, 96, d=True) for _ in range(2)]
            vSs = [A(NKC * 65, d=True) for _ in range(2)]
            PTs = [A(512) for _ in range(3)]
            osts = [A(4 * 65, d=True) for _ in range(2)]
            for vS in vSs:
                kb.op(pool, lambda e: e.memset(vS.ap, 1.0), w=[vS])
            PS = [PSB(i * 512, (i + 1) * 512) for i in range(3)]
            PO = [PSB((3 + i) * 512, (3 + i) * 512 + 65) for i in range(4)]
            jobs = []
            for hh in range(6):
                g = hh // 3
                jobs.append((64, KaT_d[g * 64:(g + 1) * 64, :], Va_d[:, g * 64:(g + 1) * 64], QaT_d[hh // 2, (hh % 2) * 64:(hh % 2) * 64 + 64, :], 64 ** -0.5, hh))
            for hh in range(6):
                jobs.append((96, KcT_d[hh], Vc_d[:, hh * 64:(hh + 1) * 64], QcT_d[hh], 96 ** -0.5, 6 + hh))

            def load_job(j):
                dk, ksrc, vsrc, qsrc, sc, col = jobs[j]
                kT, qT, vS = kTs[j % 2], qTs[j % 2], vSs[j % 2]
                kb.dma(sp, kT.ap[0:dk, :], ksrc, kT, w=[kT])
                kb.dma(sp, qT.ap[0:dk, :], qsrc, qT, w=[qT])
                kb.dma(sp, v3(vS.ap, NKC, 65)[:, :, 0:64], vsrc.rearrange("(c p) d -> p c d", p=128), vS, w=[vS])

            blocks = []
            if with_ctx:
                blocks.append((0, CTX, list(range(NT_C))))
            for q0 in range(CTX, NK, 512):
                blocks.append((q0, min(512, NK - q0), list(range(NKC))))
            load_job(0)
            nblk = 0
            for j in range(len(jobs)):
                if j + 1 < len(jobs):
                    load_job(j + 1)
                dk, ksrc, vsrc, qsrc, sc, col = jobs[j]
                kT, qT, vS = kTs[j % 2], qTs[j % 2], vSs[j % 2]
                vS3 = v3(vS.ap, NKC, 65)
                for (q0, qn, kcs) in blocks:
                    nsub = qn // 128
                    ost = osts[nblk % 2]
                    nblk += 1

                    def emit_S(idx):
                        kc = kcs[idx]
                        ps_ = PS[idx % 3]
                        kb.op(pe, lambda e: e.matmul(ps_.ap[:, 0:qn], lhsT=kT.ap[0:dk, kc * 128:(kc + 1) * 128], rhs=qT.ap[0:dk, q0:q0 + qn], start=True, stop=True), r=[kT, qT], w=[ps_])
                        pt_ = PTs[idx % 3]
                        kb.op(act, lambda e: e.activation(out=pt_.ap[:, 0:qn], in_=ps_.ap[:, 0:qn], func=AF.Exp, scale=float(sc)), r=[ps_], w=[pt_])

                    def emit_PV(idx):
                        kc = kcs[idx]
                        pt_ = PTs[idx % 3]
                        for s_ in range(nsub):
                            kb.op(pe, lambda e: e.matmul(PO[s_].ap, lhsT=pt_.ap[:, s_ * 128:(s_ + 1) * 128], rhs=vS3[:, kc, :], start=(idx == 0), stop=(idx == len(kcs) - 1)),
                                  r=[pt_, vS], w=[PO[s_]] if idx == 0 else (), aw=() if idx == 0 else [PO[s_]])

                    emit_S(0)
                    for idx in range(len(kcs)):
                        if idx + 1 < len(kcs):
                            emit_S(idx + 1)
                        emit_PV(idx)
                    o3 = v3(ost.ap, 4, 65)
                    for s_ in range(nsub):
                        kb.op(act, lambda e: e.copy(out=o3[:, s_, :], in_=PO[s_].ap), r=[PO[s_]], w=[ost] if s_ == 0 else (), aw=() if s_ == 0 else [ost])
                    kb.dma(sp, mix_d[q0:q0 + qn, col, :].rearrange("(s p) d -> p s d", p=128), o3[:, 0:nsub, :], ost, r=[ost])
                    yield

        def layer_setup(l):
            kb.new_phase()
            for bf in (gq, gk, gcq, gckv, psc, PWbd):
                bf.dsem = kb.dsem()
            kb.dma(sp, gq.ap, gqa_qn_g[l].partition_broadcast(128), gq, w=[gq])
            kb.dma(sp, gk.ap, gqa_kn_g[l].partition_broadcast(128), gk, w=[gk])
            kb.dma(sp, gcq.ap, mla_qn_g[l].partition_broadcast(128), gcq, w=[gcq])
            kb.dma(sp, gckv.ap, mla_kvn_g[l].partition_broadcast(128), gckv, w=[gckv])
            for j in range(2):
                kb.dma(sp, psc.ap[:, j:j + 1], pool_scale[l, j * 128:(j + 1) * 128].rearrange("(p o) -> p o", o=1), psc, w=[psc] if j == 0 else (), aw=() if j == 0 else [psc])
            kb.op(pool, lambda e: e.memset(PWbd.ap, 0.0), w=[PWbd])
            pw3 = v3(PWbd.ap, 2, 128)
            for g in range(4):
                p0 = (g % 2) * 64
                kb.dma(sp, pw3[p0:p0 + 64, g // 2, p0:p0 + 64], pool_w[l, g], PWbd, aw=[PWbd])

        def phase_C1(l, b, with_ctx):
            kb.new_phase()
            R1.reset()
            R2.reset()
            A = lambda n, parts=128, d=False: Buf(R2.take(n, parts), kb.dsem() if d else None)
            wq_sb = Buf(R1.take(KD * 2048), kb.dsem())
            wo_sb = Buf(R1.take(KD * 1024), kb.dsem())
            wq3 = v3(wq_sb.ap, KD, 2048)
            wo3 = v3(wo_sb.ap, KD, 1024)
            kb.dma(sp, wo3, w_out[l].rearrange("(k p) n -> p k n", p=128), wo_sb, w=[wo_sb])
            kb.dma(sp, wq3, peer_wq[l].rearrange("(k p) n -> p k n", p=128), wq_sb, w=[wq_sb])
            skT = A(2048)
            sSs = [A(2048, d=True) for _ in range(2)]
            sS = sSs[0]
            G1 = A(D, d=True); A2 = A(D, d=True); SH2 = A(D, d=True)
            xts = [A(D, d=True) for _ in range(1)]
            mixins = [A(780, d=True) for _ in range(2)]
            obts = [A(256, d=True) for _ in range(1)]
            mixT = A(768)
            xn = A(D, d=True)
            h2 = A(D, d=True)
            h2T = A(D)
            qpT = A(2048)
            eid = A(128, d=True)
            gate = A(128, d=True)
            ss = A(1)
            PT = PSB(0, 1024)
            PW = PSB(1024, 2048)
            PQ = PSB(2048, 4096)
            s3 = v3(sS.ap, 16, 128)
            kb.dma(sp, s3, peer_subkeys[l].rearrange("h p n d -> n (h p) d"), sS, w=[sS])
            transposes(lambda k: s3[:, k, :], 16, [sS], ident, PQ, lambda k: PQ.ap[:, k * 128:(k + 1) * 128], skT, skT.ap, 2048)
            sk3 = v3(skT.ap, 16, 128)
            tiles = tok_tiles(with_ctx)
            rcache = {}
            rar = Arena(R2T, R2C)
            rar.off = R2.off

            def loads(i):
                is_ctx, ti, tk0 = tiles[i]
                xt = xts[i % len(xts)]
                kb.dma(sp, xt.ap, x_src(l, b, is_ctx, ti), xt, w=[xt])
                ob_ = obts[i % len(obts)]
                kb.dma(sp, v3(ob_.ap, 2, 128), obT_d[:, :, tk0:tk0 + 128].rearrange("j p t -> p j t"), ob_, w=[ob_])
                mi_ = mixins[i % 2]
                kb.dma(sp, v3(mi_.ap[:, 0:768], 12, 64), mix_d[tk0:tk0 + 128, :, 0:64], mi_, w=[mi_])
                kb.dma(sp, None, None, mi_, aw=[mi_], fn=lambda q: q.dma_start(out=mi_.ap[:, 768:780].unsqueeze(2), in_=mix_d[tk0:tk0 + 128, :, 64:65], allow_slow_non_contiguous=True))

            def do_routing(p):
                sS_, tk_ = p
                peer_routing(kb, rar, sS_, iota, eid, gate, eoff=l * NEXP, cache=rcache)
                kb.dma(sp, eid_d[tk_:tk_ + 128, :], eid.ap.bitcast(I32), eid, r=[eid])
                kb.dma(sp, gate_d[tk_:tk_ + 128, :], gate.ap, gate, r=[gate])

            loads(0)
            cur_kind = None
            pending = None
            for i, (is_ctx, ti, tk0) in enumerate(tiles):
                if cur_kind != is_ctx:
                    cur_kind = is_ctx
                    row = NB if is_ctx else b
                    bc_load(G1, l, row, 2)
                    bc_load(A2, l, row, 4)
                    bc_load(SH2, l, row, 3)
                xt = xts[i % len(xts)]
                obt = obts[i % len(obts)]
                mixin = mixins[i % 2]
                sS = sSs[i % 2]
                kb.op(dve, lambda e: e.reciprocal(out=mixin.ap[:, 768:780], in_=mixin.ap[:, 768:780]), r=[mixin], w=[mixin])
                kb.op(dve, lambda e: e.tensor_tensor(out=v3(mixin.ap[:, 0:768], 12, 64), in0=v3(mixin.ap[:, 0:768], 12, 64), in1=mixin.ap[:, 768:780].unsqueeze(2).to_broadcast([128, 12, 64]), op=ALU.mult), r=[mixin], w=[mixin])
                transposes(lambda k: mixin.ap[:, k * 128:(k + 1) * 128], 6, [mixin], ident, PT, lambda k: PT.ap[:, k * 128:(k + 1) * 128], mixT, mixT.ap, 768)
                mT3 = v3(mixT.ap, 6, 128)
                ob3 = v3(obt.ap, 2, 128)
                for half in range(2):
                    for kk in range(KD):
                        if kk < 3:
                            lt, lb = mT3[:, kk, :], mixT
                        elif kk < 5:
                            lt, lb = ob3[:, kk - 3, :], obt
                        else:
                            lt, lb = mT3[:, kk - 2, :], mixT
                        first = (half == 0 and kk == 0)
                        kb.op(pe, lambda e: e.matmul(PW.ap[:, half * 512:(half + 1) * 512], lhsT=lt, rhs=wo3[:, kk, half * 512:(half + 1) * 512], start=(kk == 0), stop=(kk == KD - 1)),
                              r=[lb, wo_sb], w=[PW] if first else (), aw=() if first else [PW])
                kb.op(dve, lambda e: e.tensor_tensor(out=xn.ap, in0=PW.ap, in1=G1.ap, op=ALU.mult), r=[PW, G1], w=[xn])
                kb.op(pool, lambda e: e.tensor_tensor(out=xn.ap, in0=xn.ap, in1=xt.ap, op=ALU.add), r=[xt], w=[xn])
                kb.dma(sp, xmid_d[tk0:tk0 + 128, :], xn.ap, xn, r=[xn])
                rms_rstd(xn.ap, [xn], h2, ss, D)
                kb.op(dve, lambda e: e.scalar_tensor_tensor(out=h2.ap, in0=xn.ap, scalar=ss.ap[:, 0:1], in1=A2.ap, op0=ALU.mult, op1=ALU.mult), r=[xn, ss, A2], w=[h2])
                kb.op(pool, lambda e: e.tensor_tensor(out=h2.ap, in0=h2.ap, in1=SH2.ap, op=ALU.add), r=[SH2], w=[h2])
                kb.dma(sp, h2_d[tk0:tk0 + 128, :], h2.ap, h2, r=[h2])
                if pending is not None:
                    do_routing(pending)
                    pending = None
                if i + 1 < len(tiles):
                    loads(i + 1)
                transposes(lambda k: h2.ap[:, k * 128:(k + 1) * 128], KD, [h2], ident, PT, lambda k: PT.ap[:, k * 128:(k + 1) * 128], h2T, h2T.ap, 1024)
                hT3 = v3(h2T.ap, KD, 128)
                for hp in range(16):
                    for k in range(KD):
                        first = (hp == 0 and k == 0)
                        kb.op(pe, lambda e: e.matmul(PQ.ap[:, hp * 128:(hp + 1) * 128], lhsT=wq3[:, k, hp * 128:(hp + 1) * 128], rhs=hT3[:, k, :], start=(k == 0), stop=(k == KD - 1)),
                              r=[wq_sb, h2T], w=[PQ] if first else (), aw=() if first else [PQ])
                kb.op(act, lambda e: e.copy(out=qpT.ap, in_=PQ.ap), r=[PQ], w=[qpT])
                qp3 = v3(qpT.ap, 16, 128)
                for hp in range(16):
                    kb.op(pe, lambda e: e.matmul(PSALL[:, hp * 128:(hp + 1) * 128], lhsT=qp3[:, hp, :], rhs=sk3[:, hp, :], start=True, stop=True),
                          r=[qpT, skT], w=[PT, PW] if hp == 0 else (), aw=() if hp == 0 else [PT, PW])
                kb.op(act, lambda e: e.copy(out=sS.ap[:, 0:1024], in_=PT.ap), r=[PT], w=[sS])
                kb.op(act, lambda e: e.copy(out=sS.ap[:, 1024:2048], in_=PW.ap), r=[PW], aw=[sS])
                pending = (sS, tk0)
            do_routing(pending)

        NS = 16

        def gen_C2(l, b, with_ctx, last, sub=None, nslots=NS, nacc=4):
            A = lambda n, parts=128, d=False: Buf(R2.take(n, parts), kb.dsem() if d else None)
            A1_ = lambda n, d=False: Buf(R1.take(n), kb.dsem() if d else None)
            G2 = A(D, d=True)
            h2ts = [A(D, d=True) for _ in range(2)]
            xnts = [A(D, d=True) for _ in range(2)]
            eids = [A(128, d=True) for _ in range(2)]
            gates = [A(128, d=True) for _ in range(2)]
            a = A(128); wgt = A(128); g1 = A(128); g2 = A(128)
            ss = A(1)
            accs = [A1_(D, d=True) for _ in range(nacc)]
            xo = A1_(D, d=True)
            junk = A1_(D)
            slots = [Buf(R1.take(D), kb.gsems[k_]) for k_ in range(nslots)]
            tiles = tok_tiles(with_ctx)
            if sub is not None:
                tiles = [tiles[k_] for k_ in sub]
            if not tiles:
                return

            def loads(i):
                is_ctx, ti, tk0 = tiles[i]
                kb.dma(sp, h2ts[i % 2].ap, h2_d[tk0:tk0 + 128, :], h2ts[i % 2], w=[h2ts[i % 2]])
                kb.dma(sp, xnts[i % 2].ap, xmid_d[tk0:tk0 + 128, :], xnts[i % 2], w=[xnts[i % 2]])
                kb.dma(sp, eids[i % 2].ap.bitcast(I32), eid_d[tk0:tk0 + 128, :], eids[i % 2], w=[eids[i % 2]])
                kb.dma(sp, gates[i % 2].ap, gate_d[tk0:tk0 + 128, :], gates[i % 2], w=[gates[i % 2]])

            def gather(slot, tab, eidb, e_):
                kb.dma(pool, None, None, slot, r=[eidb], w=[slot],
                       fn=lambda q: q.indirect_dma_start(out=slot.ap, out_offset=None, in_=tab,
                                                         in_offset=bass.IndirectOffsetOnAxis(ap=eidb.ap.bitcast(I32)[:, e_:e_ + 1], axis=0)))

            loads(0)
            cur_kind = None
            ng = 0
            for i, (is_ctx, ti, tk0) in enumerate(tiles):
                if cur_kind != is_ctx:
                    cur_kind = is_ctx
                    bc_load(G2, l, NB if is_ctx else b, 5)
                if i + 1 < len(tiles):
                    loads(i + 1)
                h2t, xnt, eidb, gateb = h2ts[i % 2], xnts[i % 2], eids[i % 2], gates[i % 2]
                kb.op(dve, lambda e: e.memset(a.ap, 0.0), w=[a])
                for e_ in range(128):
                    slot = slots[ng % nslots]
                    ng += 1
                    gather(slot, peer_u, eidb, e_)
                    kb.op(dve, lambda e: e.scalar_tensor_tensor(out=junk.ap, in0=slot.ap, scalar=1.0, in1=h2t.ap, op0=ALU.mult, op1=ALU.mult, accum_out=a.ap[:, e_:e_ + 1]), r=[slot, h2t], w=[junk], aw=[a])
                    if e_ % 8 == 7:
                        yield
                kb.op(dve, lambda e: e.tensor_tensor(out=g1.ap, in0=a.ap, in1=a.ap, op=ALU.mult), r=[a], w=[g1])
                kb.op(dve, lambda e: e.tensor_scalar(out=g1.ap, in0=g1.ap, scalar1=0.044715, scalar2=1.0, op0=ALU.mult, op1=ALU.add), r=[g1], w=[g1])
                kb.op(dve, lambda e: e.tensor_tensor(out=g1.ap, in0=g1.ap, in1=a.ap, op=ALU.mult), r=[a], w=[g1])
                kb.op(act, lambda e: e.activation(out=g2.ap, in_=g1.ap, func=AF.Tanh, scale=0.7978845608028654), r=[g1], w=[g2])
                kb.op(dve, lambda e: e.tensor_scalar(out=g2.ap, in0=g2.ap, scalar1=1.0, scalar2=0.5, op0=ALU.add, op1=ALU.mult), r=[g2], w=[g2])
                kb.op(dve, lambda e: e.tensor_tensor(out=g2.ap, in0=g2.ap, in1=a.ap, op=ALU.mult), r=[a], w=[g2])
                kb.op(dve, lambda e: e.tensor_tensor(out=wgt.ap, in0=g2.ap, in1=gateb.ap, op=ALU.mult), r=[g2, gateb], w=[wgt])
                for e_ in range(128):
                    slot = slots[ng % nslots]
                    ng += 1
                    gather(slot, peer_v, eidb, e_)
                    acc = accs[e_ % nacc]
                    if e_ < nacc:
                        kb.op(dve, lambda e: e.tensor_scalar_mul(out=acc.ap, in0=slot.ap, scalar1=wgt.ap[:, e_:e_ + 1]), r=[slot, wgt], w=[acc])
                    else:
                        kb.op(dve, lambda e: e.scalar_tensor_tensor(out=acc.ap, in0=slot.ap, scalar=wgt.ap[:, e_:e_ + 1], in1=acc.ap, op0=ALU.mult, op1=ALU.add), r=[slot, wgt], w=[acc])
                    if e_ % 8 == 7:
                        yield
                kb.op(dve, lambda e: e.tensor_tensor(out=accs[0].ap, in0=accs[0].ap, in1=accs[1].ap, op=ALU.add), r=[accs[1]], w=[accs[0]])
                if nacc == 4:
                    kb.op(dve, lambda e: e.tensor_tensor(out=accs[2].ap, in0=accs[2].ap, in1=accs[3].ap, op=ALU.add), r=[accs[3]], w=[accs[2]])
                    kb.op(dve, lambda e: e.tensor_tensor(out=accs[0].ap, in0=accs[0].ap, in1=accs[2].ap, op=ALU.add), r=[accs[2]], w=[accs[0]])
                kb.op(dve, lambda e: e.tensor_tensor(out=xo.ap, in0=accs[0].ap, in1=G2.ap, op=ALU.mult), r=[accs[0], G2], w=[xo])
                kb.op(dve, lambda e: e.tensor_tensor(out=xo.ap, in0=xo.ap, in1=xnt.ap, op=ALU.add), r=[xnt], w=[xo])
                if last:
                    if not is_ctx:
                        rms_rstd(xo.ap, [xo], accs[0], ss, D)
                        kb.op(dve, lambda e: e.scalar_tensor_tensor(out=accs[1].ap, in0=xo.ap, scalar=ss.ap[:, 0:1], in1=FG.ap, op0=ALU.mult, op1=ALU.mult), r=[xo, ss, FG], w=[accs[1]])
                        kb.dma(sp, out_d[b, ti * 128:(ti + 1) * 128, :], accs[1].ap, accs[1], r=[accs[1]])
                else:
                    dst = (xc1_d if is_ctx else xl1_d)[b, ti * 128:(ti + 1) * 128, :]
                    kb.dma(sp, dst, xo.ap, xo, r=[xo])

        def run_phase(gens, weights):
            kb.new_phase()
            R1.reset()
            R2.reset()
            done = [0] * len(gens)
            alive = [True] * len(gens)
            while any(alive):
                best = None
                for k_, g_ in enumerate(gens):
                    if alive[k_]:
                        fr = done[k_] / float(max(1, weights[k_]))
                        if best is None or fr < best[0]:
                            best = (fr, k_)
                k_ = best[1]
                try:
                    next(gens[k_])
                    done[k_] += 1
                except StopIteration:
                    alive[k_] = False

        def n_yields_B(with_ctx):
            return 12 * (len(range(CTX, NK, 512)) + (1 if with_ctx else 0))

        def n_yields_C2(with_ctx):
            return 32 * (NT_L + (NT_C if with_ctx else 0))

        def schedule():
            if stop == "pro":
                return
            seqs = [(l, b) for l in range(DEPTH) for b in range(NB)]
            n = len(seqs)
            upd_of = lambda q: q[0] < DEPTH - 1
            last_of = lambda q: q[0] == DEPTH - 1
            ntile = lambda q: NT_L + (NT_C if upd_of(q) else 0)
            setup_done = set()

            def ensure_setup(l):
                if l not in setup_done:
                    layer_setup(l)
                    setup_done.add(l)

            def C2g(q, sub=None, nslots=NS, nacc=4):
                return gen_C2(q[0], q[1], upd_of(q), last_of(q), sub=sub, nslots=nslots, nacc=nacc)

            if NB >= 3 and stop is None:
                TAIL = 8
                ensure_setup(seqs[0][0])
                run_phase([gen_A(*seqs[0])], [1])
                phase_P(seqs[0][0], seqs[0][1], upd_of(seqs[0]))
                run_phase([gen_B(seqs[0][0], seqs[0][1], upd_of(seqs[0]))], [1])
                phase_C1(seqs[0][0], seqs[0][1], upd_of(seqs[0]))
                ensure_setup(seqs[1][0])
                run_phase([gen_A(*seqs[1])], [1])
                phase_P(seqs[1][0], seqs[1][1], upd_of(seqs[1]))
                for k in range(n - 1):
                    q0, q1 = seqs[k], seqs[k + 1]
                    nt = ntile(q0)
                    split = max(0, nt - TAIL)
                    run_phase([C2g(q0, range(0, split)), gen_B(q1[0], q1[1], upd_of(q1))], [32 * split, n_yields_B(upd_of(q1))])
                    if k + 2 < n:
                        q2 = seqs[k + 2]
                        ensure_setup(q2[0])
                        run_phase([C2g(q0, range(split, nt), nslots=4, nacc=2), gen_A(*q2)], [32 * TAIL, 8 * (NT_C + NT_L)])
                    else:
                        run_phase([C2g(q0, range(split, nt))], [1])
                    phase_C1(q1[0], q1[1], upd_of(q1))
                    if k + 2 < n:
                        phase_P(q2[0], q2[1], upd_of(q2))
                run_phase([C2g(seqs[n - 1])], [1])
                return

            fuse = NB >= 2 and stop is None
            pend = None
            for (l, b) in seqs:
                ensure_setup(l)
                upd = l < DEPTH - 1
                if pend is not None and not fuse:
                    run_phase([C2g(pend)], [1])
                    pend = None
                    if stop == "C2":
                        return
                run_phase([gen_A(l, b)], [1])
                if stop == "A":
                    return
                phase_P(l, b, upd)
                if stop == "P":
                    return
                if pend is not None:
                    run_phase([C2g(pend), gen_B(l, b, upd)], [n_yields_C2(upd_of(pend)), n_yields_B(upd)])
                else:
                    run_phase([gen_B(l, b, upd)], [1])
                if stop == "B":
                    return
                phase_C1(l, b, upd)
                if stop == "C1":
                    return
                pend = (l, b)
            run_phase([C2g(pend)], [1])

        schedule()
        kb.barrier()
        build.stats = {e.name: e.n for e in kb.engs}
        build.stats['nops'] = kb.nops
    return nc


def _consts(S, CTX):
    f = np.float32
    c = {}
    c["c_ident"] = np.eye(128, dtype=f)
    i16 = np.arange(16, dtype=f)
    c["c_iota"] = np.tile(np.concatenate([i16, 16 * i16, 16 * i16 + 16]).astype(f), (128, 1))
    rows = S // GRID_W
    row = np.repeat(np.arange(rows), GRID_W).astype(f)
    col = np.tile(np.arange(GRID_W), rows).astype(f)

    def rope(rot_dim):
        n = rot_dim // 4
        inv = (f(10000.0) ** (-np.arange(n, dtype=f) / f(n))).astype(f)
        ang = np.concatenate([row[:, None] * inv, col[:, None] * inv], axis=-1).astype(f)
        return np.concatenate([np.cos(ang), np.sin(ang)], axis=-1).astype(f)

    c["c_ropeA"] = rope(64)
    c["c_ropeC"] = rope(32)

    def rc(L):
        out = np.zeros((4, L), f)
        t = np.arange(L)
        for i, w in enumerate((2, 4, 8, 16)):
            lo = np.clip(t - w // 2, 0, L)
            hi = np.clip(t - w // 2 + w, 0, L)
            out[i] = (1.0 / (hi - lo).astype(f)).astype(f)
        return out

    c["c_rcL"] = rc(S)
    c["c_rcC"] = rc(CTX)
    return c


def make_in_maps(inputs, n_cores, NB, S, CTX, DEPTH):
    f = np.float32
    shared = {}
    for k in ("ada_w", "ada_b", "norm1_g", "norm2_g", "w_in", "gqa_qn_g", "gqa_kn_g", "pool_w", "pool_scale", "mla_qn_g",
              "mla_kvn_g", "mla_w_uq", "mla_w_ukv", "w_out", "peer_wq", "peer_subkeys", "final_g"):
        shared[k] = np.ascontiguousarray(np.asarray(inputs[k], dtype=f))
    shared["peer_u"] = np.ascontiguousarray(np.asarray(inputs["peer_u"], dtype=f).reshape(DEPTH * NEXP, D))
    shared["peer_v"] = np.ascontiguousarray(np.asarray(inputs["peer_v"], dtype=f).reshape(DEPTH * NEXP, D))
    shared.update(_consts(S, CTX))
    x = np.asarray(inputs["x"], dtype=f)
    ctx = np.asarray(inputs["ctx"], dtype=f)
    c = np.asarray(inputs["c"], dtype=f)
    c_ctx = np.asarray(inputs["c_ctx"], dtype=f)
    maps = []
    for i in range(n_cores):
        m = dict(shared)
        m["x"] = np.ascontiguousarray(x[i * NB:(i + 1) * NB])
        m["ctx"] = np.ascontiguousarray(ctx[i * NB:(i + 1) * NB])
        m["cT"] = np.ascontiguousarray(np.concatenate([c[i * NB:(i + 1) * NB], c_ctx[None, :]], axis=0).T)
        maps.append(m)
    return maps


_NC_CACHE = {}


def kernel(**inputs):
    B, S, _ = inputs["x"].shape
    CTX = inputs["ctx"].shape[1]
    DEPTH = inputs["ada_w"].shape[0]
    NB = B // N_CORES
    key = (NB, S, CTX, DEPTH)
    if key not in _NC_CACHE:
        _NC_CACHE[key] = build(NB, S, CTX, DEPTH)
    nc = _NC_CACHE[key]
    maps = make_in_maps(inputs, N_CORES, NB, S, CTX, DEPTH)
    res = run_bass_kernel_spmd(nc, maps, core_ids=list(range(N_CORES)))
    return np.concatenate([np.asarray(r["out"], dtype=np.float32) for r in res.results], axis=0)
```

```python
import contextlib
import numpy as np
import concourse.bass as bass
import concourse.mybir as mybir
from concourse.bass_utils import run_bass_kernel_spmd

F32 = mybir.dt.float32
I32 = mybir.dt.int32
U32 = mybir.dt.uint32
ALU = mybir.AluOpType
AF = mybir.ActivationFunctionType
AX = mybir.AxisListType

D = 1024
KD = 8
NMOD = 6
EPS = 1e-6
GRID_W = 64
HD = 64
INC = 1568
NEXP = 16384
N_CORES = 8


class Buf:
    def __init__(self, ap, dsem=None, psum=False):
        self.ap = ap
        self.psum = psum
        self.ws = {}
        self.rs = {}
        self.dsem = dsem

    def __getitem__(self, k):
        return self.ap[k]


class DSem:
    def __init__(self, sem):
        self.sem = sem
        self.cnt = 0


class Eng:
    def __init__(self, name, e, sem, is_pe=False):
        self.name = name
        self.e = e
        self.sem = sem
        self.cnt = 0
        self.seen = {}
        self.is_pe = is_pe
        self.n = 0

    def wait(self, sem, val):
        if val <= 0 or self.seen.get(sem, 0) >= val:
            return
        self.e.wait_ge(sem, val)
        self.seen[sem] = val
        self.n += 1


class KB:
    def __init__(self, nc, es, n_dsem=64):
        self.nc = nc
        self.es = es
        mk = lambda n: es.enter_context(nc.semaphore(n))
        self.pe = Eng("pe", nc.tensor, mk("s_pe"), is_pe=True)
        self.act = Eng("act", nc.scalar, mk("s_act"))
        self.dve = Eng("dve", nc.vector, mk("s_dve"))
        self.pool = Eng("pool", nc.gpsimd, mk("s_pool"))
        self.sp = Eng("sp", nc.sync, None)
        self.engs = [self.pe, self.act, self.dve, self.pool, self.sp]
        self.dsems = [DSem(mk("s_d%d" % i)) for i in range(n_dsem)]
        self.dfree = 0
        self.budget = None
        self.nops = 0
        self.gsems = [DSem(mk("s_g%d" % i)) for i in range(16)]

    def new_phase(self):
        self.barrier()
        self.dfree = 0

    def dsem(self):
        d = self.dsems[self.dfree]
        self.dfree += 1
        return d

    def _deps(self, eng, r, w, aw):
        deps = {}
        def need(dd):
            for s, v in dd.items():
                if deps.get(s, 0) < v:
                    deps[s] = v
        for b in r:
            need(b.ws)
            if b.psum:
                need(b.rs)
        for b in w:
            need(b.ws)
            need(b.rs)
        for b in aw:
            need(b.rs)
            need(b.ws)
        for s, v in deps.items():
            if eng.is_pe and s is eng.sem:
                continue
            eng.wait(s, v)

    def op(self, eng, fn, r=(), w=(), aw=()):
        self.nops += 1
        if self.budget is not None and self.nops > self.budget:
            return None
        self._deps(eng, r, w, aw)
        inst = fn(eng.e)
        eng.cnt += 1
        eng.n += 1
        inst.then_inc(eng.sem, 1)
        s, c = eng.sem, eng.cnt
        for b in r:
            b.rs[s] = c
        for b in w:
            b.ws = {s: c}
            b.rs = {}
        for b in aw:
            b.ws[s] = c
        return inst

    def dma(self, q, out, in_, slot, r=(), w=(), aw=(), fn=None):
        self.nops += 1
        if self.budget is not None and self.nops > self.budget:
            return None
        self._deps(q, r, w, aw)
        if fn is None:
            inst = q.e.dma_start(out=out, in_=in_)
        else:
            inst = fn(q.e)
        q.n += 1
        d = slot.dsem
        d.cnt += 16
        inst.then_inc(d.sem, 16)
        s, c = d.sem, d.cnt
        for b in r:
            b.rs[s] = c
        for b in w:
            b.ws = {s: c}
            b.rs = {}
        for b in aw:
            b.ws[s] = c
        return inst

    def barrier(self):
        allv = {}
        for e in self.engs:
            if e.sem is not None and e.cnt > 0:
                allv[e.sem] = e.cnt
        for d in self.dsems + self.gsems:
            if d.cnt > 0:
                allv[d.sem] = d.cnt
        for e in self.engs:
            for s, v in allv.items():
                e.wait(s, v)


def v3(ap, a, b):
    return ap.rearrange("p (a b) -> p a b", a=a, b=b)


def v4(ap, a, b, c):
    return ap.rearrange("p (a b c) -> p a b c", a=a, b=b, c=c)


class Arena:
    def __init__(self, t, ncols):
        self.t = t
        self.n = ncols
        self.off = 0

    def reset(self):
        self.off = 0

    def take(self, ncols, parts=128):
        assert self.off + ncols <= self.n, ("arena overflow", self.off, ncols, self.n)
        ap = self.t[0:parts, self.off:self.off + ncols]
        self.off += ncols
        return ap


def peer_routing(kb, ar, sS, iota16, eid_i, gate, eoff=0, cache=None):
    dve, act, pool = kb.dve, kb.act, kb.pool
    if cache is None:
        cache = {}
    names = iter(range(1000))
    def B(n, parts=128):
        k = next(names)
        if k not in cache:
            cache[k] = Buf(ar.take(n, parts))
        return cache[k]
    sv = B(256)
    si = B(256)
    sif = B(256)
    wk = B(128)
    cs = B(2048)
    wk2 = B(256)
    ts = B(128)
    pos = B(128)
    posf = B(128)
    pbf = B(128)
    paf = B(128)
    oh = B(2048)
    i1s = B(128)
    i2s = B(128)
    eidf = B(128)
    ex = B(128)
    sm = B(8)
    s3 = v3(sS.ap, 16, 128)
    sv3 = v3(sv.ap, 16, 16)
    siu = si.ap.bitcast(U32)
    si3 = v3(siu, 16, 16)
    for hp in range(16):
        kb.op(dve, lambda e: e.max(out=sv3[:, hp, 0:8], in_=s3[:, hp, :]), r=[sS], aw=[sv])
        kb.op(dve, lambda e: e.max_index(out=si3[:, hp, 0:8], in_max=sv3[:, hp, 0:8], in_values=s3[:, hp, :]), r=[sS, sv], aw=[si])
        kb.op(dve, lambda e: e.match_replace(out=wk.ap, in_to_replace=sv3[:, hp, 0:8], in_values=s3[:, hp, :], imm_value=-1e30), r=[sS, sv], w=[wk])
        kb.op(dve, lambda e: e.max(out=sv3[:, hp, 8:16], in_=wk.ap), r=[wk], aw=[sv])
        kb.op(dve, lambda e: e.max_index(out=si3[:, hp, 8:16], in_max=sv3[:, hp, 8:16], in_values=wk.ap), r=[wk, sv], aw=[si])
    kb.op(dve, lambda e: e.tensor_copy(out=sif.ap, in_=siu), r=[si], w=[sif])
    sv4 = v4(sv.ap, 8, 2, 16)
    sif4 = v4(sif.ap, 8, 2, 16)
    cs4 = v4(cs.ap, 8, 16, 16)
    shp = [128, 8, 16, 16]
    kb.op(dve, lambda e: e.tensor_tensor(out=cs4, in0=sv4[:, :, 0, :].unsqueeze(3).to_broadcast(shp),
                                         in1=sv4[:, :, 1, :].unsqueeze(2).to_broadcast(shp), op=ALU.add), r=[sv], w=[cs])
    cs3 = v3(cs.ap, 8, 256)
    ts3 = v3(ts.ap, 8, 16)
    posu = pos.ap.bitcast(U32)
    pos3 = v3(posu, 8, 16)
    for h in range(8):
        kb.op(dve, lambda e: e.max(out=ts3[:, h, 0:8], in_=cs3[:, h, :]), r=[cs], aw=[ts])
        kb.op(dve, lambda e: e.max_index(out=pos3[:, h, 0:8], in_max=ts3[:, h, 0:8], in_values=cs3[:, h, :]), r=[cs, ts], aw=[pos])
        kb.op(dve, lambda e: e.match_replace(out=wk2.ap, in_to_replace=ts3[:, h, 0:8], in_values=cs3[:, h, :], imm_value=-1e30), r=[cs, ts], w=[wk2])
        kb.op(dve, lambda e: e.max(out=ts3[:, h, 8:16], in_=wk2.ap), r=[wk2], aw=[ts])
        kb.op(dve, lambda e: e.max_index(out=pos3[:, h, 8:16], in_max=ts3[:, h, 8:16], in_values=wk2.ap), r=[wk2, ts], aw=[pos])
    kb.op(dve, lambda e: e.tensor_copy(out=posf.ap, in_=posu), r=[pos], w=[posf])
    oh4 = v4(oh.ap, 8, 16, 16)
    oh2 = cs
    oh2_4 = v4(oh2.ap, 8, 16, 16)
    io_i = iota16.ap[:, 0:16].unsqueeze(1).unsqueeze(1).to_broadcast(shp)
    io_lo = iota16.ap[:, 16:32].unsqueeze(1).unsqueeze(1).to_broadcast(shp)
    io_hi = iota16.ap[:, 32:48].unsqueeze(1).unsqueeze(1).to_broadcast(shp)
    posb = v3(posf.ap, 8, 16).unsqueeze(3).to_broadcast(shp)
    kb.op(dve, lambda e: e.tensor_tensor(out=oh4, in0=posb, in1=io_lo, op=ALU.is_ge), r=[posf, iota16], w=[oh])
    kb.op(dve, lambda e: e.tensor_tensor(out=oh2_4, in0=posb, in1=io_hi, op=ALU.is_ge), r=[posf, iota16], w=[oh2])
    kb.op(dve, lambda e: e.tensor_tensor(out=oh4, in0=oh4, in1=oh2_4, op=ALU.subtract), r=[oh, oh2], w=[oh])
    kb.op(dve, lambda e: e.tensor_tensor(out=oh2_4, in0=oh4, in1=io_i, op=ALU.mult), r=[oh, iota16], w=[oh2])
    kb.op(dve, lambda e: e.reduce_sum(out=paf.ap, in_=v3(oh2.ap, 128, 16), axis=AX.X), r=[oh2], w=[paf])
    kb.op(dve, lambda e: e.tensor_tensor(out=oh2_4, in0=oh4, in1=sif4[:, :, 0, :].unsqueeze(2).to_broadcast(shp), op=ALU.mult), r=[oh, sif], w=[oh2])
    kb.op(dve, lambda e: e.reduce_sum(out=i1s.ap, in_=v3(oh2.ap, 128, 16), axis=AX.X), r=[oh2], w=[i1s])
    kb.op(dve, lambda e: e.scalar_tensor_tensor(out=pbf.ap, in0=paf.ap, scalar=-16.0, in1=posf.ap, op0=ALU.mult, op1=ALU.add), r=[paf, posf], w=[pbf])
    kb.op(dve, lambda e: e.tensor_tensor(out=oh4, in0=v3(pbf.ap, 8, 16).unsqueeze(3).to_broadcast(shp), in1=io_i, op=ALU.is_equal), r=[pbf, iota16], w=[oh])
    kb.op(dve, lambda e: e.tensor_tensor(out=oh4, in0=oh4, in1=sif4[:, :, 1, :].unsqueeze(2).to_broadcast(shp), op=ALU.mult), r=[oh, sif], w=[oh])
    kb.op(dve, lambda e: e.reduce_sum(out=i2s.ap, in_=v3(oh.ap, 128, 16), axis=AX.X), r=[oh], w=[i2s])
    kb.op(dve, lambda e: e.scalar_tensor_tensor(out=eidf.ap, in0=i1s.ap, scalar=128.0, in1=i2s.ap, op0=ALU.mult, op1=ALU.add), r=[i1s, i2s], w=[eidf])
    if eoff:
        kb.op(dve, lambda e: e.tensor_scalar_add(out=eidf.ap, in0=eidf.ap, scalar1=float(eoff)), r=[eidf], w=[eidf])
    kb.op(dve, lambda e: e.tensor_copy(out=eid_i.ap.bitcast(I32), in_=eidf.ap), r=[eidf], w=[eid_i])
    ex3 = v3(ex.ap, 8, 16)
    kb.op(dve, lambda e: e.tensor_tensor(out=ex3, in0=ts3, in1=ts3[:, :, 0:1].to_broadcast([128, 8, 16]), op=ALU.subtract), r=[ts], w=[ex])
    kb.op(act, lambda e: e.activation(out=ex.ap, in_=ex.ap, func=AF.Exp), r=[ex], w=[ex])
    kb.op(dve, lambda e: e.reduce_sum(out=sm.ap, in_=ex3, axis=AX.X), r=[ex], w=[sm])
    kb.op(dve, lambda e: e.reciprocal(out=sm.ap, in_=sm.ap), r=[sm], w=[sm])
    kb.op(dve, lambda e: e.tensor_tensor(out=v3(gate.ap, 8, 16), in0=ex3, in1=sm.ap.unsqueeze(2).to_broadcast([128, 8, 16]), op=ALU.mult), r=[ex, sm], w=[gate])


WNAMES = [("ada_w", None), ("ada_b", None), ("norm1_g", None), ("norm2_g", None), ("w_in", None),
          ("gqa_qn_g", None), ("gqa_kn_g", None), ("pool_w", None), ("pool_scale", None), ("mla_qn_g", None),
          ("mla_kvn_g", None), ("mla_w_uq", None), ("mla_w_ukv", None), ("w_out", None), ("peer_wq", None),
          ("peer_subkeys", None), ("final_g", None)]


def build(NB, S, CTX, DEPTH, stop=None, budget=None):
    NT_L = S // 128
    NT_C = CTX // 128
    NK = CTX + S
    NKC = NK // 128
    nc = bass.Bass("TRN2", target_bir_lowering=False)

    def din(name, shape, dtype=F32):
        return nc.dram_tensor(name, list(shape), dtype, kind="ExternalInput").ap()

    def dscr(name, shape, dtype=F32):
        return nc.dram_tensor(name, list(shape), dtype).ap()

    x_d = din("x", [NB, S, D])
    ctx_d = din("ctx", [NB, CTX, D])
    cT_d = din("cT", [D, NB + 1])
    ada_w = din("ada_w", [DEPTH, D, NMOD * D])
    ada_b = din("ada_b", [DEPTH, NMOD * D])
    norm1_g = din("norm1_g", [DEPTH, D])
    norm2_g = din("norm2_g", [DEPTH, D])
    w_in = din("w_in", [DEPTH, D, INC])
    gqa_qn_g = din("gqa_qn_g", [DEPTH, 64])
    gqa_kn_g = din("gqa_kn_g", [DEPTH, 64])
    pool_w = din("pool_w", [DEPTH, 4, 64, 64])
    pool_scale = din("pool_scale", [DEPTH, 256])
    mla_qn_g = din("mla_qn_g", [DEPTH, 384])
    mla_kvn_g = din("mla_kvn_g", [DEPTH, 256])
    mla_w_uq = din("mla_w_uq", [DEPTH, 384, 576])
    mla_w_ukv = din("mla_w_ukv", [DEPTH, 256, 768])
    w_out = din("w_out", [DEPTH, D, D])
    peer_wq = din("peer_wq", [DEPTH, D, 2048])
    peer_subkeys = din("peer_subkeys", [DEPTH, 8, 2, 128, 128])
    peer_u = din("peer_u", [DEPTH * NEXP, D])
    peer_v = din("peer_v", [DEPTH * NEXP, D])
    final_g = din("final_g", [D])
    ident_d = din("c_ident", [128, 128])
    iota_d = din("c_iota", [128, 48])
    ropeA_d = din("c_ropeA", [S, 64])
    ropeC_d = din("c_ropeC", [S, 32])
    rcL_d = din("c_rcL", [4, S])
    rcC_d = din("c_rcC", [4, CTX])
    out_d = nc.dram_tensor("out", [NB, S, D], F32, kind="ExternalOutput").ap()

    mod_d = dscr("mod_d", [DEPTH, NB + 1, NMOD * D])
    QaT_d = dscr("QaT_d", [3, 128, NK])
    KaT_d = dscr("KaT_d", [128, NK])
    Va_d = dscr("Va_d", [NK, 128])
    bT_d = dscr("bT_d", [2, 128, NK])
    obT_d = dscr("obT_d", [2, 128, NK])
    QcT_d = dscr("QcT_d", [6, 96, NK])
    KcT_d = dscr("KcT_d", [6, 96, NK])
    Vc_d = dscr("Vc_d", [NK, 384])
    mix_d = dscr("mix_d", [NK, 12, 65])
    xmid_d = dscr("xmid_d", [NK, D])
    h2_d = dscr("h2_d", [NK, D])
    eid_d = dscr("eid_d", [NK, 128], I32)
    gate_d = dscr("gate_d", [NK, 128])
    xl1_d = dscr("xl1_d", [NB, S, D])
    xc1_d = dscr("xc1_d", [NB, CTX, D])

    GC, R1C, R2C = 3840, 24704, 24616
    with contextlib.ExitStack() as es:
        kb = KB(nc, es, n_dsem=44)
        kb.budget = budget
        pe, act, dve, pool, sp = kb.pe, kb.act, kb.dve, kb.pool, kb.sp
        GT = es.enter_context(nc.sbuf_tensor("GT", [128, GC], F32))
        R1T = es.enter_context(nc.sbuf_tensor("R1T", [128, R1C], F32))
        R2T = es.enter_context(nc.sbuf_tensor("R2T", [128, R2C], F32))
        PSALL = es.enter_context(nc.psum_tensor("PSALL", [128, 4096], F32))
        G = Arena(GT, GC)
        R1 = Arena(R1T, R1C)
        R2 = Arena(R2T, R2C)

        def PSB(c0, c1, parts=128):
            return Buf(PSALL[0:parts, c0:c1], psum=True)

        def rms_a(src_ap, src_bufs, junk, ss, n):
            kb.op(act, lambda e: e.activation(out=junk.ap[:, 0:n], in_=src_ap, func=AF.Square, accum_out=ss.ap), r=src_bufs, w=[junk, ss])
            kb.op(act, lambda e: e.activation(out=ss.ap, in_=ss.ap, func=AF.Sqrt, scale=1.0 / n, bias=EPS), r=[ss], w=[ss])

        def rms_b(ss):
            kb.op(dve, lambda e: e.reciprocal(out=ss.ap, in_=ss.ap), r=[ss], w=[ss])

        def rms_rstd(src_ap, src_bufs, junk, ss, n):
            kb.op(act, lambda e: e.activation(out=junk.ap[:, 0:n], in_=src_ap, func=AF.Square, accum_out=ss.ap), r=src_bufs, w=[junk, ss])
            kb.op(act, lambda e: e.activation(out=ss.ap, in_=ss.ap, func=AF.Sqrt, scale=1.0 / n, bias=EPS), r=[ss], w=[ss])
            kb.op(dve, lambda e: e.reciprocal(out=ss.ap, in_=ss.ap), r=[ss], w=[ss])

        def transposes(src_ap_fn, n, src_bufs, ident, PT, pt_ap_fn, dst, dst_ap, cols, parts=128):
            for k in range(n):
                kb.op(pe, lambda e: e.transpose(out=pt_ap_fn(k), in_=src_ap_fn(k), identity=ident.ap), r=src_bufs + [ident],
                      w=[PT] if k == 0 else (), aw=() if k == 0 else [PT])
            kb.op(act, lambda e: e.copy(out=dst_ap, in_=PT.ap[0:parts, 0:cols]), r=[PT], w=[dst])

        ident = Buf(G.take(128), kb.dsem())
        iota = Buf(G.take(48), kb.dsem())
        ropeA = Buf(G.take(NT_L * 64), kb.dsem())
        ropeC = Buf(G.take(NT_L * 32), kb.dsem())
        FG = Buf(G.take(1024), kb.dsem())
        gq = Buf(G.take(64))
        gk = Buf(G.take(64))
        gcq = Buf(G.take(384))
        gckv = Buf(G.take(256))
        psc = Buf(G.take(2))
        PWbd = Buf(G.take(256))
        kb.dma(sp, ident.ap, ident_d, ident, w=[ident])
        kb.dma(sp, iota.ap, iota_d, iota, w=[iota])
        kb.dma(sp, v3(ropeA.ap, NT_L, 64), ropeA_d.rearrange("(n p) c -> p n c", p=128), ropeA, w=[ropeA])
        kb.dma(sp, v3(ropeC.ap, NT_L, 32), ropeC_d.rearrange("(n p) c -> p n c", p=128), ropeC, w=[ropeC])
        kb.dma(sp, FG.ap, final_g.partition_broadcast(128), FG, w=[FG])
        NB1 = NB + 1
        cT = Buf(R2.take(KD * NB1), kb.dsem())
        cT3 = v3(cT.ap, KD, NB1)
        kb.dma(sp, cT3, cT_d.rearrange("(k p) n -> p k n", p=128), cT, w=[cT])
        kb.op(act, lambda e: e.activation(out=cT.ap, in_=cT.ap, func=AF.Silu), r=[cT], w=[cT])
        adab = Buf(R2.take(NMOD * D, NB1), kb.dsem())
        g1b = Buf(R2.take(D, NB1), kb.dsem())
        g2b = Buf(R2.take(D, NB1), kb.dsem())
        modsb = Buf(R2.take(NMOD * D, NB1), kb.dsem())
        wsl = [Buf(R1.take(KD * 512), kb.dsem()) for _ in range(2)]
        pps = [PSB(0, 512, NB1), PSB(512, 1024, NB1)]
        for l in range(DEPTH):
            kb.dma(sp, adab.ap, ada_b[l].partition_broadcast(NB1), adab, w=[adab])
            kb.dma(sp, g1b.ap, norm1_g[l].partition_broadcast(NB1), g1b, w=[g1b])
            kb.dma(sp, g2b.ap, norm2_g[l].partition_broadcast(NB1), g2b, w=[g2b])
            for pc in range(12):
                ws_ = wsl[pc % 2]
                pp = pps[pc % 2]
                kb.dma(sp, v3(ws_.ap, KD, 512), ada_w[l][:, pc * 512:(pc + 1) * 512].rearrange("(k p) n -> p k n", p=128), ws_, w=[ws_])
                for k in range(KD):
                    kb.op(pe, lambda e: e.matmul(pp.ap, lhsT=cT3[:, k, :], rhs=v3(ws_.ap, KD, 512)[:, k, :], start=(k == 0), stop=(k == KD - 1)),
                          r=[cT, ws_], w=[pp] if k == 0 else (), aw=() if k == 0 else [pp])
                kb.op(dve, lambda e: e.tensor_tensor(out=modsb.ap[:, pc * 512:(pc + 1) * 512], in0=pp.ap, in1=adab.ap[:, pc * 512:(pc + 1) * 512], op=ALU.add),
                      r=[pp, adab], w=[modsb] if pc == 0 else (), aw=() if pc == 0 else [modsb])
            kb.op(dve, lambda e: e.scalar_tensor_tensor(out=modsb.ap[:, D:2 * D], in0=modsb.ap[:, D:2 * D], scalar=1.0, in1=g1b.ap, op0=ALU.add, op1=ALU.mult),
                  r=[g1b], w=[modsb])
            kb.op(dve, lambda e: e.scalar_tensor_tensor(out=modsb.ap[:, 4 * D:5 * D], in0=modsb.ap[:, 4 * D:5 * D], scalar=1.0, in1=g2b.ap, op0=ALU.add, op1=ALU.mult),
                  r=[g2b], w=[modsb])
            kb.dma(sp, mod_d[l], modsb.ap, modsb, r=[modsb])

        def bc_load(dst, l, row, i):
            kb.dma(sp, dst.ap, mod_d[l, row, i * D:(i + 1) * D].partition_broadcast(128), dst, w=[dst])

        def tok_tiles(with_ctx):
            tl = []
            if with_ctx:
                tl += [(True, i, i * 128) for i in range(NT_C)]
            tl += [(False, i, CTX + i * 128) for i in range(NT_L)]
            return tl

        def x_src(l, b, is_ctx, ti):
            if l == 0:
                return (ctx_d if is_ctx else x_d)[b, ti * 128:(ti + 1) * 128, :]
            return (xc1_d if is_ctx else xl1_d)[b, ti * 128:(ti + 1) * 128, :]

        def gen_A(l, b):
            A = lambda n, parts=128, d=False: Buf(R2.take(n, parts), kb.dsem() if d else None)
            w_in_sb = Buf(R1.take(KD * INC), kb.dsem())
            w_uq_sb = Buf(R1.take(3 * 576), kb.dsem())
            w_ukv_sb = Buf(R1.take(2 * 768), kb.dsem())
            wi3 = v3(w_in_sb.ap, KD, INC)
            wq3 = v3(w_uq_sb.ap, 3, 576)
            wkv3 = v3(w_ukv_sb.ap, 2, 768)
            kb.dma(sp, wi3, w_in[l].rearrange("(k p) n -> p k n", p=128), w_in_sb, w=[w_in_sb])
            kb.dma(sp, wq3, mla_w_uq[l].rearrange("(k p) n -> p k n", p=128), w_uq_sb, w=[w_uq_sb])
            kb.dma(sp, wkv3, mla_w_ukv[l].rearrange("(k p) n -> p k n", p=128), w_ukv_sb, w=[w_ukv_sb])
            bcs = {}
            for is_ctx in (True, False):
                row = NB if is_ctx else b
                a1 = A(D, d=True)
                sh1 = A(D, d=True)
                bc_load(a1, l, row, 1)
                bc_load(sh1, l, row, 0)
                bcs[is_ctx] = (a1, sh1)
            xts = [A(D, d=True) for _ in range(2)]
            h = A(D)
            hT = A(D)
            pj = A(INC, d=True)
            sq = A(512)
            ss = A(1); ss8 = A(8); ssq = A(1); ssk = A(1)
            qkn = A(512)
            qkr = A(512)
            t1 = A(256); t2 = A(256); t3 = A(256); t4 = A(256)
            qkT = A(512, d=True)
            bTs = A(256, d=True)
            cqn = A(384)
            cqnT = A(384)
            qm = A(576)
            u1 = A(96); u2 = A(96); u3 = A(96); u4 = A(96)
            qcT = A(768, 96, d=True)
            ckvn = A(256)
            ckvnT = A(256)
            kfull = A(576)
            vc = A(384, d=True)
            kcT = A(768, 96, d=True)
            krr = A(32)
            k1 = A(16); k2 = A(16); k3 = A(16); k4 = A(16)
            PT = PSB(0, 1024)
            PJ = PSB(1024, 3072)
            PX = PSB(3072, 4096)
            tiles = tok_tiles(True)

            def load_x(i):
                is_ctx, ti, tk0 = tiles[i]
                xt = xts[i % 2]
                kb.dma(sp, xt.ap, x_src(l, b, is_ctx, ti), xt, w=[xt])

            load_x(0)
            for i, (is_ctx, ti, tk0) in enumerate(tiles):
                if i + 1 < len(tiles):
                    load_x(i + 1)
                xt = xts[i % 2]
                a1, sh1 = bcs[is_ctx]
                rms_a(xt.ap, [xt], h, ss, D)
                yield
                rms_b(ss)
                kb.op(dve, lambda e: e.scalar_tensor_tensor(out=h.ap, in0=xt.ap, scalar=ss.ap[:, 0:1], in1=a1.ap, op0=ALU.mult, op1=ALU.mult), r=[xt, ss, a1], w=[h])
                kb.op(dve, lambda e: e.tensor_tensor(out=h.ap, in0=h.ap, in1=sh1.ap, op=ALU.add), r=[sh1], w=[h])
                transposes(lambda k: h.ap[:, k * 128:(k + 1) * 128], KD, [h], ident, PT, lambda k: PT.ap[:, k * 128:(k + 1) * 128], hT, hT.ap, 1024)
                hT3 = v3(hT.ap, KD, 128)
                for pc, (c0, c1) in enumerate(((0, 512), (512, 1024), (1024, 1536), (1536, INC))):
                    for k in range(KD):
                        kb.op(pe, lambda e: e.matmul(PJ.ap[:, c0:c1], lhsT=hT3[:, k, :], rhs=wi3[:, k, c0:c1], start=(k == 0), stop=(k == KD - 1)),
                              r=[hT, w_in_sb], w=[PJ] if (pc == 0 and k == 0) else (), aw=() if (pc == 0 and k == 0) else [PJ])
                kb.op(act, lambda e: e.copy(out=pj.ap, in_=PJ.ap[:, 0:INC]), r=[PJ], w=[pj])
                yield
                kb.op(dve, lambda e: e.tensor_tensor(out=sq.ap, in0=pj.ap[:, 0:512], in1=pj.ap[:, 0:512], op=ALU.mult), r=[pj], w=[sq])
                kb.op(dve, lambda e: e.reduce_sum(out=ss8.ap, in_=v3(sq.ap, 8, 64), axis=AX.X), r=[sq], w=[ss8])
                kb.op(act, lambda e: e.activation(out=ss8.ap, in_=ss8.ap, func=AF.Sqrt, scale=1.0 / 64, bias=EPS), r=[ss8], w=[ss8])
                yield
                kb.op(dve, lambda e: e.reciprocal(out=ss8.ap, in_=ss8.ap), r=[ss8], w=[ss8])
                qkn3 = v3(qkn.ap, 8, 64)
                kb.op(dve, lambda e: e.tensor_tensor(out=qkn3, in0=v3(pj.ap[:, 0:512], 8, 64), in1=ss8.ap.unsqueeze(2).to_broadcast([128, 8, 64]), op=ALU.mult), r=[pj, ss8], w=[qkn])
                kb.op(dve, lambda e: e.tensor_tensor(out=qkn3[:, 0:6, :], in0=qkn3[:, 0:6, :], in1=gq.ap.unsqueeze(1).to_broadcast([128, 6, 64]), op=ALU.mult), r=[gq], w=[qkn])
                kb.op(dve, lambda e: e.tensor_tensor(out=qkn3[:, 6:8, :], in0=qkn3[:, 6:8, :], in1=gk.ap.unsqueeze(1).to_broadcast([128, 2, 64]), op=ALU.mult), r=[gk], w=[qkn])
                if is_ctx:
                    qsrc = qkn
                else:
                    qsrc = qkr
                    q4 = v4(qkn.ap, 8, 2, 32)
                    r4 = v4(qkr.ap, 8, 2, 32)
                    rA = v3(ropeA.ap, NT_L, 64)
                    cosb = rA[:, ti, 0:32].unsqueeze(1).to_broadcast([128, 8, 32])
                    sinb = rA[:, ti, 32:64].unsqueeze(1).to_broadcast([128, 8, 32])
                    T = lambda t: v3(t.ap, 8, 32)
                    kb.op(dve, lambda e: e.tensor_tensor(out=T(t1), in0=q4[:, :, 0, :], in1=cosb, op=ALU.mult), r=[qkn, ropeA], w=[t1])
                    kb.op(dve, lambda e: e.tensor_tensor(out=T(t2), in0=q4[:, :, 1, :], in1=sinb, op=ALU.mult), r=[qkn, ropeA], w=[t2])
                    kb.op(dve, lambda e: e.tensor_tensor(out=r4[:, :, 0, :], in0=T(t1), in1=T(t2), op=ALU.subtract), r=[t1, t2], w=[qkr])
                    kb.op(dve, lambda e: e.tensor_tensor(out=T(t3), in0=q4[:, :, 0, :], in1=sinb, op=ALU.mult), r=[qkn, ropeA], w=[t3])
                    kb.op(dve, lambda e: e.tensor_tensor(out=T(t4), in0=q4[:, :, 1, :], in1=cosb, op=ALU.mult), r=[qkn, ropeA], w=[t4])
                    kb.op(dve, lambda e: e.tensor_tensor(out=r4[:, :, 1, :], in0=T(t3), in1=T(t4), op=ALU.add), r=[t3, t4], aw=[qkr])
                transposes(lambda k: qsrc.ap[:, k * 128:(k + 1) * 128], 4, [qsrc], ident, PJ, lambda k: PJ.ap[:, k * 128:(k + 1) * 128], qkT, qkT.ap, 512)
                qkT3 = v3(qkT.ap, 4, 128)
                kb.dma(sp, QaT_d[:, :, tk0:tk0 + 128].rearrange("j p t -> p j t"), qkT3[:, 0:3, :], qkT, r=[qkT])
                kb.dma(sp, KaT_d[:, tk0:tk0 + 128], qkT3[:, 3, :], qkT, r=[qkT])
                kb.dma(sp, Va_d[tk0:tk0 + 128, :], pj.ap[:, 512:640], pj, r=[pj])
                transposes(lambda k: pj.ap[:, 640 + k * 128:640 + (k + 1) * 128], 2, [pj], ident, PX, lambda k: PX.ap[:, k * 128:(k + 1) * 128], bTs, bTs.ap, 256)
                kb.dma(sp, bT_d[:, :, tk0:tk0 + 128].rearrange("j p t -> p j t"), v3(bTs.ap, 2, 128), bTs, r=[bTs])
                rms_a(pj.ap[:, 896:1280], [pj], cqn, ssq, 384)
                yield
                rms_b(ssq)
                kb.op(dve, lambda e: e.scalar_tensor_tensor(out=cqn.ap, in0=pj.ap[:, 896:1280], scalar=ssq.ap[:, 0:1], in1=gcq.ap, op0=ALU.mult, op1=ALU.mult), r=[pj, ssq, gcq], w=[cqn])
                transposes(lambda k: cqn.ap[:, k * 128:(k + 1) * 128], 3, [cqn], ident, PT, lambda k: PT.ap[:, k * 128:(k + 1) * 128], cqnT, cqnT.ap, 384)
                cq3 = v3(cqnT.ap, 3, 128)
                for pc, (c0, c1) in enumerate(((0, 512), (512, 576))):
                    for k in range(3):
                        kb.op(pe, lambda e: e.matmul(PX.ap[:, c0:c1], lhsT=cq3[:, k, :], rhs=wq3[:, k, c0:c1], start=(k == 0), stop=(k == 2)),
                              r=[cqnT, w_uq_sb], w=[PX] if (pc == 0 and k == 0) else (), aw=() if (pc == 0 and k == 0) else [PX])
                kb.op(act, lambda e: e.copy(out=qm.ap, in_=PX.ap[:, 0:576]), r=[PX], w=[qm])
                qm3 = v3(qm.ap, 6, 96)
                if not is_ctx:
                    yield
                    rC = v3(ropeC.ap, NT_L, 32)
                    cosc = rC[:, ti, 0:16].unsqueeze(1).to_broadcast([128, 6, 16])
                    sinc = rC[:, ti, 16:32].unsqueeze(1).to_broadcast([128, 6, 16])
                    U = lambda t: v3(t.ap, 6, 16)
                    x1 = qm3[:, :, 64:80]
                    x2 = qm3[:, :, 80:96]
                    kb.op(dve, lambda e: e.tensor_tensor(out=U(u1), in0=x1, in1=cosc, op=ALU.mult), r=[qm, ropeC], w=[u1])
                    kb.op(dve, lambda e: e.tensor_tensor(out=U(u2), in0=x2, in1=sinc, op=ALU.mult), r=[qm, ropeC], w=[u2])
                    kb.op(dve, lambda e: e.tensor_tensor(out=U(u3), in0=x1, in1=sinc, op=ALU.mult), r=[qm, ropeC], w=[u3])
                    kb.op(dve, lambda e: e.tensor_tensor(out=U(u4), in0=x2, in1=cosc, op=ALU.mult), r=[qm, ropeC], w=[u4])
                    kb.op(dve, lambda e: e.tensor_tensor(out=x1, in0=U(u1), in1=U(u2), op=ALU.subtract), r=[u1, u2, u3, u4], w=[qm])
                    kb.op(dve, lambda e: e.tensor_tensor(out=x2, in0=U(u3), in1=U(u4), op=ALU.add), r=[u3, u4], w=[qm])
                transposes(lambda hh: qm3[:, hh, :], 6, [qm], ident, PT, lambda hh: PSALL[0:96, hh * 128:(hh + 1) * 128], qcT, qcT.ap, 768, parts=96)
                kb.dma(sp, QcT_d[:, :, tk0:tk0 + 128].rearrange("h p t -> p h t"), v3(qcT.ap, 6, 128), qcT, r=[qcT])
                rms_a(pj.ap[:, 1280:1536], [pj], ckvn, ssk, 256)
                yield
                rms_b(ssk)
                kb.op(dve, lambda e: e.scalar_tensor_tensor(out=ckvn.ap, in0=pj.ap[:, 1280:1536], scalar=ssk.ap[:, 0:1], in1=gckv.ap, op0=ALU.mult, op1=ALU.mult), r=[pj, ssk, gckv], w=[ckvn])
                transposes(lambda k: ckvn.ap[:, k * 128:(k + 1) * 128], 2, [ckvn], ident, PJ, lambda k: PJ.ap[:, k * 128:(k + 1) * 128], ckvnT, ckvnT.ap, 256)
                ck3 = v3(ckvnT.ap, 2, 128)
                for pc, (c0, c1) in enumerate(((0, 512), (512, 768))):
                    for k in range(2):
                        kb.op(pe, lambda e: e.matmul(PX.ap[:, c0:c1], lhsT=ck3[:, k, :], rhs=wkv3[:, k, c0:c1], start=(k == 0), stop=(k == 1)),
                              r=[ckvnT, w_ukv_sb], w=[PX] if (pc == 0 and k == 0) else (), aw=() if (pc == 0 and k == 0) else [PX])
                kf3 = v3(kfull.ap, 6, 96)
                px3 = v3(PX.ap[:, 0:768], 6, 128)
                yield
                kb.op(dve, lambda e: e.tensor_copy(out=kf3[:, :, 0:64], in_=px3[:, :, 0:64]), r=[PX], w=[kfull])
                kb.op(act, lambda e: e.copy(out=v3(vc.ap, 6, 64), in_=px3[:, :, 64:128]), r=[PX], w=[vc])
                kb.dma(sp, Vc_d[tk0:tk0 + 128, :], vc.ap, vc, r=[vc])
                if is_ctx:
                    krsrc_ap, krsrc = pj.ap[:, 1536:1568], pj
                else:
                    rC = v3(ropeC.ap, NT_L, 32)
                    cos1 = rC[:, ti, 0:16]
                    sin1 = rC[:, ti, 16:32]
                    y1 = pj.ap[:, 1536:1552]
                    y2 = pj.ap[:, 1552:1568]
                    kb.op(dve, lambda e: e.tensor_tensor(out=k1.ap, in0=y1, in1=cos1, op=ALU.mult), r=[pj, ropeC], w=[k1])
                    kb.op(dve, lambda e: e.tensor_tensor(out=k2.ap, in0=y2, in1=sin1, op=ALU.mult), r=[pj, ropeC], w=[k2])
                    kb.op(dve, lambda e: e.tensor_tensor(out=k3.ap, in0=y1, in1=sin1, op=ALU.mult), r=[pj, ropeC], w=[k3])
                    kb.op(dve, lambda e: e.tensor_tensor(out=k4.ap, in0=y2, in1=cos1, op=ALU.mult), r=[pj, ropeC], w=[k4])
                    kb.op(dve, lambda e: e.tensor_tensor(out=krr.ap[:, 0:16], in0=k1.ap, in1=k2.ap, op=ALU.subtract), r=[k1, k2], w=[krr])
                    kb.op(dve, lambda e: e.tensor_tensor(out=krr.ap[:, 16:32], in0=k3.ap, in1=k4.ap, op=ALU.add), r=[k3, k4], aw=[krr])
                    krsrc_ap, krsrc = krr.ap, krr
                kb.op(dve, lambda e: e.tensor_copy(out=kf3[:, :, 64:96], in_=krsrc_ap.unsqueeze(1).to_broadcast([128, 6, 32])), r=[krsrc], aw=[kfull])
                transposes(lambda hh: kf3[:, hh, :], 6, [kfull], ident, PT, lambda hh: PSALL[0:96, hh * 128:(hh + 1) * 128], kcT, kcT.ap, 768, parts=96)
                kb.dma(sp, KcT_d[:, :, tk0:tk0 + 128].rearrange("h p t -> p h t"), v3(kcT.ap, 6, 128), kcT, r=[kcT])
                yield

        def phase_P(l, b, with_ctx):
            kb.new_phase()
            R1.reset()
            R2.reset()
            Lmax = S
            Lp = Lmax + 16
            xb = Buf(R1.take(2 * Lp), kb.dsem())
            Wa = Buf(R1.take(2 * Lp))
            Wb = Buf(R1.take(2 * Lp))
            PTb = Buf(R1.take(2 * Lmax))
            rc = Buf(R1.take(4 * Lmax), kb.dsem())
            obs = [Buf(R2.take(512), kb.dsem()) for _ in range(2)]
            pbs = [PSB(i * 512, (i + 1) * 512) for i in range(4)]
            streams = ([(CTX, 0, rcC_d)] if with_ctx else []) + [(S, CTX, rcL_d)]
            nmm = 0
            for (L, tk0, rc_d) in streams:
                Lq = L + 16
                x3 = v3(xb.ap[:, 0:2 * Lq], 2, Lq)
                a3 = v3(Wa.ap[:, 0:2 * Lq], 2, Lq)
                b3 = v3(Wb.ap[:, 0:2 * Lq], 2, Lq)
                p3 = v3(PTb.ap[:, 0:2 * L], 2, L)
                r3 = v3(rc.ap[:, 0:4 * L], 4, L)
                kb.op(pool, lambda e: e.memset(xb.ap[:, 0:2 * Lq], 0.0), w=[xb])
                kb.dma(sp, x3[:, :, 8:8 + L], bT_d[:, :, tk0:tk0 + L].rearrange("j p t -> p j t"), xb, w=[xb])
                kb.dma(sp, rc.ap[:, 0:4 * L], rc_d.rearrange("w s -> (w s)").partition_broadcast(128), rc, w=[rc])
                kb.op(dve, lambda e: e.tensor_tensor(out=a3[:, :, 1:Lq - 1], in0=x3[:, :, 0:Lq - 2], in1=x3[:, :, 1:Lq - 1], op=ALU.add), r=[xb], w=[Wa])

                def grp(g, Wsrc, w3):
                    j, p0 = g // 2, (g % 2) * 64
                    kb.op(dve, lambda e: e.tensor_tensor(out=p3[p0:p0 + 64, j, :], in0=w3[p0:p0 + 64, j, 8:8 + L], in1=r3[p0:p0 + 64, g, :], op=ALU.mult), r=[Wsrc, rc], aw=[PTb])
                    kb.op(dve, lambda e: e.tensor_tensor(out=p3[p0:p0 + 64, j, :], in0=p3[p0:p0 + 64, j, :], in1=x3[p0:p0 + 64, j, 8:8 + L], op=ALU.subtract), r=[xb], aw=[PTb])

                grp(0, Wa, a3)
                kb.op(dve, lambda e: e.tensor_tensor(out=b3[:, :, 2:Lq - 2], in0=a3[:, :, 1:Lq - 3], in1=a3[:, :, 3:Lq - 1], op=ALU.add), r=[Wa], w=[Wb])
                grp(1, Wb, b3)
                kb.op(dve, lambda e: e.tensor_tensor(out=a3[:, :, 4:Lq - 4], in0=b3[:, :, 2:Lq - 6], in1=b3[:, :, 6:Lq - 2], op=ALU.add), r=[Wb], w=[Wa])
                grp(2, Wa, a3)
                kb.op(dve, lambda e: e.tensor_tensor(out=b3[:, :, 8:Lq - 8], in0=a3[:, :, 4:Lq - 12], in1=a3[:, :, 12:Lq - 4], op=ALU.add), r=[Wa], w=[Wb])
                grp(3, Wb, b3)
                for j in range(2):
                    for c0 in range(0, L, 512):
                        cn = min(512, L - c0)
                        pb_ = pbs[nmm % 4]
                        ob = obs[nmm % 2]
                        nmm += 1
                        kb.op(pe, lambda e: e.matmul(pb_.ap[:, 0:cn], lhsT=v3(PWbd.ap, 2, 128)[:, j, :], rhs=p3[:, j, c0:c0 + cn], start=True, stop=True), r=[PWbd, PTb], w=[pb_])
                        kb.op(act, lambda e: e.activation(out=ob.ap[:, 0:cn], in_=pb_.ap[:, 0:cn], func=AF.Identity, scale=psc.ap[:, j:j + 1]), r=[pb_, psc], w=[ob])
                        kb.dma(sp, obT_d[j, :, tk0 + c0:tk0 + c0 + cn], ob.ap[:, 0:cn], ob, r=[ob])

        def gen_B(l, b, with_ctx):
            A = lambda n, parts=128, d=False: Buf(R2.take(n, parts), kb.dsem() if d else None)
            kTs = [A(NK, 96, d=True) for _ in range(2)]
            qTs = [A(NK, 96, d=True) for _ in range(2)]
            vSs = [A(NKC * 65, d=True) for _ in range(2)]
            PTs = [A(512) for _ in range(3)]
            osts = [A(4 * 65, d=True) for _ in range(2)]
            for vS in vSs:
                kb.op(pool, lambda e: e.memset(vS.ap, 1.0), w=[vS])
            PS = [PSB(i * 512, (i + 1) * 512) for i in range(3)]
            PO = [PSB((3 + i) * 512, (3 + i) * 512 + 65) for i in range(4)]
            jobs = []
            for hh in range(6):
                g = hh // 3
                jobs.append((64, KaT_d[g * 64:(g + 1) * 64, :], Va_d[:, g * 64:(g + 1) * 64], QaT_d[hh // 2, (hh % 2) * 64:(hh % 2) * 64 + 64, :], 64 ** -0.5, hh))
            for hh in range(6):
                jobs.append((96, KcT_d[hh], Vc_d[:, hh * 64:(hh + 1) * 64], QcT_d[hh], 96 ** -0.5, 6 + hh))

            def load_job(j):
                dk, ksrc, vsrc, qsrc, sc, col = jobs[j]
                kT, qT, vS = kTs[j % 2], qTs[j % 2], vSs[j % 2]
                kb.dma(sp, kT.ap[0:dk, :], ksrc, kT, w=[kT])
                kb.dma(sp, qT.ap[0:dk, :], qsrc, qT, w=[qT])
                kb.dma(sp, v3(vS.ap, NKC, 65)[:, :, 0:64], vsrc.rearrange("(c p) d -> p c d", p=128), vS, w=[vS])

            blocks = []
            if with_ctx:
                blocks.append((0, CTX, list(range(NT_C))))
            for q0 in range(CTX, NK, 512):
                blocks.append((q0, min(512, NK - q0), list(range(NKC))))
            load_job(0)
            nblk = 0
            for j in range(len(jobs)):
                if j + 1 < len(jobs):
                    load_job(j + 1)
                dk, ksrc, vsrc, qsrc, sc, col = jobs[j]
                kT, qT, vS = kTs[j % 2], qTs[j % 2], vSs[j % 2]
                vS3 = v3(vS.ap, NKC, 65)
                for (q0, qn, kcs) in blocks:
                    nsub = qn // 128
                    ost = osts[nblk % 2]
                    nblk += 1

                    def emit_S(idx):
                        kc = kcs[idx]
                        ps_ = PS[idx % 3]
                        kb.op(pe, lambda e: e.matmul(ps_.ap[:, 0:qn], lhsT=kT.ap[0:dk, kc * 128:(kc + 1) * 128], rhs=qT.ap[0:dk, q0:q0 + qn], start=True, stop=True), r=[kT, qT], w=[ps_])
                        pt_ = PTs[idx % 3]
                        kb.op(act, lambda e: e.activation(out=pt_.ap[:, 0:qn], in_=ps_.ap[:, 0:qn], func=AF.Exp, scale=float(sc)), r=[ps_], w=[pt_])

                    def emit_PV(idx):
                        kc = kcs[idx]
                        pt_ = PTs[idx % 3]
                        for s_ in range(nsub):
                            kb.op(pe, lambda e: e.matmul(PO[s_].ap, lhsT=pt_.ap[:, s_ * 128:(s_ + 1) * 128], rhs=vS3[:, kc, :], start=(idx == 0), stop=(idx == len(kcs) - 1)),
                                  r=[pt_, vS], w=[PO[s_]] if idx == 0 else (), aw=() if idx == 0 else [PO[s_]])

                    emit_S(0)
                    for idx in range(len(kcs)):
                        if idx + 1 < len(kcs):
                            emit_S(idx + 1)
                        emit_PV(idx)
                    o3 = v3(ost.ap, 4, 65)
                    for s_ in range(nsub):
                        kb.op(act, lambda e: e.copy(out=o3[:, s_, :], in_=PO[s_].ap), r=[PO[s_]], w=[ost] if s_ == 0 else (), aw=() if s_ == 0 else [ost])
                    kb.dma(sp, mix_d[q0:q0 + qn, col, :].rearrange("(s p) d -> p s d", p=128), o3[:, 0:nsub, :], ost, r=[ost])
                    yield

        def layer_setup(l):
            kb.new_phase()
            for bf in (gq, gk, gcq, gckv, psc, PWbd):
                bf.dsem = kb.dsem()
            kb.dma(sp, gq.ap, gqa_qn_g[l].partition_broadcast(128), gq, w=[gq])
            kb.dma(sp, gk.ap, gqa_kn_g[l].partition_broadcast(128), gk, w=[gk])
            kb.dma(sp, gcq.ap, mla_qn_g[l].partition_broadcast(128), gcq, w=[gcq])
            kb.dma(sp, gckv.ap, mla_kvn_g[l].partition_broadcast(128), gckv, w=[gckv])
            for j in range(2):
                kb.dma(sp, psc.ap[:, j:j + 1], pool_scale[l, j * 128:(j + 1) * 128].rearrange("(p o) -> p o", o=1), psc, w=[psc] if j == 0 else (), aw=() if j == 0 else [psc])
            kb.op(pool, lambda e: e.memset(PWbd.ap, 0.0), w=[PWbd])
            pw3 = v3(PWbd.ap, 2, 128)
            for g in range(4):
                p0 = (g % 2) * 64
                kb.dma(sp, pw3[p0:p0 + 64, g // 2, p0:p0 + 64], pool_w[l, g], PWbd, aw=[PWbd])

        def phase_C1(l, b, with_ctx):
            kb.new_phase()
            R1.reset()
            R2.reset()
            A = lambda n, parts=128, d=False: Buf(R2.take(n, parts), kb.dsem() if d else None)
            wq_sb = Buf(R1.take(KD * 2048), kb.dsem())
            wo_sb = Buf(R1.take(KD * 1024), kb.dsem())
            wq3 = v3(wq_sb.ap, KD, 2048)
            wo3 = v3(wo_sb.ap, KD, 1024)
            kb.dma(sp, wo3, w_out[l].rearrange("(k p) n -> p k n", p=128), wo_sb, w=[wo_sb])
            kb.dma(sp, wq3, peer_wq[l].rearrange("(k p) n -> p k n", p=128), wq_sb, w=[wq_sb])
            skT = A(2048)
            sSs = [A(2048, d=True) for _ in range(2)]
            sS = sSs[0]
            G1 = A(D, d=True); A2 = A(D, d=True); SH2 = A(D, d=True)
            xts = [A(D, d=True) for _ in range(1)]
            mixins = [A(780, d=True) for _ in range(2)]
            obts = [A(256, d=True) for _ in range(1)]
            mixT = A(768)
            xn = A(D, d=True)
            h2 = A(D, d=True)
            h2T = A(D)
            qpT = A(2048)
            eid = A(128, d=True)
            gate = A(128, d=True)
            ss = A(1)
            PT = PSB(0, 1024)
            PW = PSB(1024, 2048)
            PQ = PSB(2048, 4096)
            s3 = v3(sS.ap, 16, 128)
            kb.dma(sp, s3, peer_subkeys[l].rearrange("h p n d -> n (h p) d"), sS, w=[sS])
            transposes(lambda k: s3[:, k, :], 16, [sS], ident, PQ, lambda k: PQ.ap[:, k * 128:(k + 1) * 128], skT, skT.ap, 2048)
            sk3 = v3(skT.ap, 16, 128)
            tiles = tok_tiles(with_ctx)
            rcache = {}
            rar = Arena(R2T, R2C)
            rar.off = R2.off

            def loads(i):
                is_ctx, ti, tk0 = tiles[i]
                xt = xts[i % len(xts)]
                kb.dma(sp, xt.ap, x_src(l, b, is_ctx, ti), xt, w=[xt])
                ob_ = obts[i % len(obts)]
                kb.dma(sp, v3(ob_.ap, 2, 128), obT_d[:, :, tk0:tk0 + 128].rearrange("j p t -> p j t"), ob_, w=[ob_])
                mi_ = mixins[i % 2]
                kb.dma(sp, v3(mi_.ap[:, 0:768], 12, 64), mix_d[tk0:tk0 + 128, :, 0:64], mi_, w=[mi_])
                kb.dma(sp, None, None, mi_, aw=[mi_], fn=lambda q: q.dma_start(out=mi_.ap[:, 768:780].unsqueeze(2), in_=mix_d[tk0:tk0 + 128, :, 64:65], allow_slow_non_contiguous=True))

            def do_routing(p):
                sS_, tk_ = p
                peer_routing(kb, rar, sS_, iota, eid, gate, eoff=l * NEXP, cache=rcache)
                kb.dma(sp, eid_d[tk_:tk_ + 128, :], eid.ap.bitcast(I32), eid, r=[eid])
                kb.dma(sp, gate_d[tk_:tk_ + 128, :], gate.ap, gate, r=[gate])

            loads(0)
            cur_kind = None
            pending = None
            for i, (is_ctx, ti, tk0) in enumerate(tiles):
                if cur_kind != is_ctx:
                    cur_kind = is_ctx
                    row = NB if is_ctx else b
                    bc_load(G1, l, row, 2)
                    bc_load(A2, l, row, 4)
                    bc_load(SH2, l, row, 3)
                xt = xts[i % len(xts)]
                obt = obts[i % len(obts)]
                mixin = mixins[i % 2]
                sS = sSs[i % 2]
                kb.op(dve, lambda e: e.reciprocal(out=mixin.ap[:, 768:780], in_=mixin.ap[:, 768:780]), r=[mixin], w=[mixin])
                kb.op(dve, lambda e: e.tensor_tensor(out=v3(mixin.ap[:, 0:768], 12, 64), in0=v3(mixin.ap[:, 0:768], 12, 64), in1=mixin.ap[:, 768:780].unsqueeze(2).to_broadcast([128, 12, 64]), op=ALU.mult), r=[mixin], w=[mixin])
                transposes(lambda k: mixin.ap[:, k * 128:(k + 1) * 128], 6, [mixin], ident, PT, lambda k: PT.ap[:, k * 128:(k + 1) * 128], mixT, mixT.ap, 768)
                mT3 = v3(mixT.ap, 6, 128)
                ob3 = v3(obt.ap, 2, 128)
                for half in range(2):
                    for kk in range(KD):
                        if kk < 3:
                            lt, lb = mT3[:, kk, :], mixT
                        elif kk < 5:
                            lt, lb = ob3[:, kk - 3, :], obt
                        else:
                            lt, lb = mT3[:, kk - 2, :], mixT
                        first = (half == 0 and kk == 0)
                        kb.op(pe, lambda e: e.matmul(PW.ap[:, half * 512:(half + 1) * 512], lhsT=lt, rhs=wo3[:, kk, half * 512:(half + 1) * 512], start=(kk == 0), stop=(kk == KD - 1)),
                              r=[lb, wo_sb], w=[PW] if first else (), aw=() if first else [PW])
                kb.op(dve, lambda e: e.tensor_tensor(out=xn.ap, in0=PW.ap, in1=G1.ap, op=ALU.mult), r=[PW, G1], w=[xn])
                kb.op(pool, lambda e: e.tensor_tensor(out=xn.ap, in0=xn.ap, in1=xt.ap, op=ALU.add), r=[xt], w=[xn])
                kb.dma(sp, xmid_d[tk0:tk0 + 128, :], xn.ap, xn, r=[xn])
                rms_rstd(xn.ap, [xn], h2, ss, D)
                kb.op(dve, lambda e: e.scalar_tensor_tensor(out=h2.ap, in0=xn.ap, scalar=ss.ap[:, 0:1], in1=A2.ap, op0=ALU.mult, op1=ALU.mult), r=[xn, ss, A2], w=[h2])
                kb.op(pool, lambda e: e.tensor_tensor(out=h2.ap, in0=h2.ap, in1=SH2.ap, op=ALU.add), r=[SH2], w=[h2])
                kb.dma(sp, h2_d[tk0:tk0 + 128, :], h2.ap, h2, r=[h2])
                if pending is not None:
                    do_routing(pending)
                    pending = None
                if i + 1 < len(tiles):
                    loads(i + 1)
                transposes(lambda k: h2.ap[:, k * 128:(k + 1) * 128], KD, [h2], ident, PT, lambda k: PT.ap[:, k * 128:(k + 1) * 128], h2T, h2T.ap, 1024)
                hT3 = v3(h2T.ap, KD, 128)
                for hp in range(16):
                    for k in range(KD):
                        first = (hp == 0 and k == 0)
                        kb.op(pe, lambda e: e.matmul(PQ.ap[:, hp * 128:(hp + 1) * 128], lhsT=wq3[:, k, hp * 128:(hp + 1) * 128], rhs=hT3[:, k, :], start=(k == 0), stop=(k == KD - 1)),
                              r=[wq_sb, h2T], w=[PQ] if first else (), aw=() if first else [PQ])
                kb.op(act, lambda e: e.copy(out=qpT.ap, in_=PQ.ap), r=[PQ], w=[qpT])
                qp3 = v3(qpT.ap, 16, 128)
                for hp in range(16):
                    kb.op(pe, lambda e: e.matmul(PSALL[:, hp * 128:(hp + 1) * 128], lhsT=qp3[:, hp, :], rhs=sk3[:, hp, :], start=True, stop=True),
                          r=[qpT, skT], w=[PT, PW] if hp == 0 else (), aw=() if hp == 0 else [PT, PW])
                kb.op(act, lambda e: e.copy(out=sS.ap[:, 0:1024], in_=PT.ap), r=[PT], w=[sS])
                kb.op(act, lambda e: e.copy(out=sS.ap[:, 1024:2048], in_=PW.ap), r=[PW], aw=[sS])
                pending = (sS, tk0)
            do_routing(pending)

        NS = 16

        def gen_C2(l, b, with_ctx, last, sub=None, nslots=NS, nacc=4):
            A = lambda n, parts=128, d=False: Buf(R2.take(n, parts), kb.dsem() if d else None)
            A1_ = lambda n, d=False: Buf(R1.take(n), kb.dsem() if d else None)
            G2 = A(D, d=True)
            h2ts = [A(D, d=True) for _ in range(2)]
            xnts = [A(D, d=True) for _ in range(2)]
            eids = [A(128, d=True) for _ in range(2)]
            gates = [A(128, d=True) for _ in range(2)]
            a = A(128); wgt = A(128); g1 = A(128); g2 = A(128)
            ss = A(1)
            accs = [A1_(D, d=True) for _ in range(nacc)]
            xo = A1_(D, d=True)
            junk = A1_(D)
            slots = [Buf(R1.take(D), kb.gsems[k_]) for k_ in range(nslots)]
            tiles = tok_tiles(with_ctx)
            if sub is not None:
                tiles = [tiles[k_] for k_ in sub]
            if not tiles:
                return

            def loads(i):
                is_ctx, ti, tk0 = tiles[i]
                kb.dma(sp, h2ts[i % 2].ap, h2_d[tk0:tk0 + 128, :], h2ts[i % 2], w=[h2ts[i % 2]])
                kb.dma(sp, xnts[i % 2].ap, xmid_d[tk0:tk0 + 128, :], xnts[i % 2], w=[xnts[i % 2]])
                kb.dma(sp, eids[i % 2].ap.bitcast(I32), eid_d[tk0:tk0 + 128, :], eids[i % 2], w=[eids[i % 2]])
                kb.dma(sp, gates[i % 2].ap, gate_d[tk0:tk0 + 128, :], gates[i % 2], w=[gates[i % 2]])

            def gather(slot, tab, eidb, e_):
                kb.dma(pool, None, None, slot, r=[eidb], w=[slot],
                       fn=lambda q: q.indirect_dma_start(out=slot.ap, out_offset=None, in_=tab,
                                                         in_offset=bass.IndirectOffsetOnAxis(ap=eidb.ap.bitcast(I32)[:, e_:e_ + 1], axis=0)))

            loads(0)
            cur_kind = None
            ng = 0
            for i, (is_ctx, ti, tk0) in enumerate(tiles):
                if cur_kind != is_ctx:
                    cur_kind = is_ctx
                    bc_load(G2, l, NB if is_ctx else b, 5)
                if i + 1 < len(tiles):
                    loads(i + 1)
                h2t, xnt, eidb, gateb = h2ts[i % 2], xnts[i % 2], eids[i % 2], gates[i % 2]
                kb.op(dve, lambda e: e.memset(a.ap, 0.0), w=[a])
                for e_ in range(128):
                    slot = slots[ng % nslots]
                    ng += 1
                    gather(slot, peer_u, eidb, e_)
                    kb.op(dve, lambda e: e.scalar_tensor_tensor(out=junk.ap, in0=slot.ap, scalar=1.0, in1=h2t.ap, op0=ALU.mult, op1=ALU.mult, accum_out=a.ap[:, e_:e_ + 1]), r=[slot, h2t], w=[junk], aw=[a])
                    if e_ % 8 == 7:
                        yield
                kb.op(dve, lambda e: e.tensor_tensor(out=g1.ap, in0=a.ap, in1=a.ap, op=ALU.mult), r=[a], w=[g1])
                kb.op(dve, lambda e: e.tensor_scalar(out=g1.ap, in0=g1.ap, scalar1=0.044715, scalar2=1.0, op0=ALU.mult, op1=ALU.add), r=[g1], w=[g1])
                kb.op(dve, lambda e: e.tensor_tensor(out=g1.ap, in0=g1.ap, in1=a.ap, op=ALU.mult), r=[a], w=[g1])
                kb.op(act, lambda e: e.activation(out=g2.ap, in_=g1.ap, func=AF.Tanh, scale=0.7978845608028654), r=[g1], w=[g2])
                kb.op(dve, lambda e: e.tensor_scalar(out=g2.ap, in0=g2.ap, scalar1=1.0, scalar2=0.5, op0=ALU.add, op1=ALU.mult), r=[g2], w=[g2])
                kb.op(dve, lambda e: e.tensor_tensor(out=g2.ap, in0=g2.ap, in1=a.ap, op=ALU.mult), r=[a], w=[g2])
                kb.op(dve, lambda e: e.tensor_tensor(out=wgt.ap, in0=g2.ap, in1=gateb.ap, op=ALU.mult), r=[g2, gateb], w=[wgt])
                for e_ in range(128):
                    slot = slots[ng % nslots]
                    ng += 1
                    gather(slot, peer_v, eidb, e_)
                    acc = accs[e_ % nacc]
                    if e_ < nacc:
                        kb.op(dve, lambda e: e.tensor_scalar_mul(out=acc.ap, in0=slot.ap, scalar1=wgt.ap[:, e_:e_ + 1]), r=[slot, wgt], w=[acc])
                    else:
                        kb.op(dve, lambda e: e.scalar_tensor_tensor(out=acc.ap, in0=slot.ap, scalar=wgt.ap[:, e_:e_ + 1], in1=acc.ap, op0=ALU.mult, op1=ALU.add), r=[slot, wgt], w=[acc])
                    if e_ % 8 == 7:
                        yield
                kb.op(dve, lambda e: e.tensor_tensor(out=accs[0].ap, in0=accs[0].ap, in1=accs[1].ap, op=ALU.add), r=[accs[1]], w=[accs[0]])
                if nacc == 4:
                    kb.op(dve, lambda e: e.tensor_tensor(out=accs[2].ap, in0=accs[2].ap, in1=accs[3].ap, op=ALU.add), r=[accs[3]], w=[accs[2]])
                    kb.op(dve, lambda e: e.tensor_tensor(out=accs[0].ap, in0=accs[0].ap, in1=accs[2].ap, op=ALU.add), r=[accs[2]], w=[accs[0]])
                kb.op(dve, lambda e: e.tensor_tensor(out=xo.ap, in0=accs[0].ap, in1=G2.ap, op=ALU.mult), r=[accs[0], G2], w=[xo])
                kb.op(dve, lambda e: e.tensor_tensor(out=xo.ap, in0=xo.ap, in1=xnt.ap, op=ALU.add), r=[xnt], w=[xo])
                if last:
                    if not is_ctx:
                        rms_rstd(xo.ap, [xo], accs[0], ss, D)
                        kb.op(dve, lambda e: e.scalar_tensor_tensor(out=accs[1].ap, in0=xo.ap, scalar=ss.ap[:, 0:1], in1=FG.ap, op0=ALU.mult, op1=ALU.mult), r=[xo, ss, FG], w=[accs[1]])
                        kb.dma(sp, out_d[b, ti * 128:(ti + 1) * 128, :], accs[1].ap, accs[1], r=[accs[1]])
                else:
                    dst = (xc1_d if is_ctx else xl1_d)[b, ti * 128:(ti + 1) * 128, :]
                    kb.dma(sp, dst, xo.ap, xo, r=[xo])

        def run_phase(gens, weights):
            kb.new_phase()
            R1.reset()
            R2.reset()
            done = [0] * len(gens)
            alive = [True] * len(gens)
            while any(alive):
                best = None
                for k_, g_ in enumerate(gens):
                    if alive[k_]:
                        fr = done[k_] / float(max(1, weights[k_]))
                        if best is None or fr < best[0]:
                            best = (fr, k_)
                k_ = best[1]
                try:
                    next(gens[k_])
                    done[k_] += 1
                except StopIteration:
                    alive[k_] = False

        def n_yields_B(with_ctx):
            return 12 * (len(range(CTX, NK, 512)) + (1 if with_ctx else 0))

        def n_yields_C2(with_ctx):
            return 32 * (NT_L + (NT_C if with_ctx else 0))

        def schedule():
            if stop == "pro":
                return
            seqs = [(l, b) for l in range(DEPTH) for b in range(NB)]
            n = len(seqs)
            upd_of = lambda q: q[0] < DEPTH - 1
            last_of = lambda q: q[0] == DEPTH - 1
            ntile = lambda q: NT_L + (NT_C if upd_of(q) else 0)
            setup_done = set()

            def ensure_setup(l):
                if l not in setup_done:
                    layer_setup(l)
                    setup_done.add(l)

            def C2g(q, sub=None, nslots=NS, nacc=4):
                return gen_C2(q[0], q[1], upd_of(q), last_of(q), sub=sub, nslots=nslots, nacc=nacc)

            if NB >= 3 and stop is None:
                TAIL = 4
                ensure_setup(seqs[0][0])
                run_phase([gen_A(*seqs[0])], [1])
                phase_P(seqs[0][0], seqs[0][1], upd_of(seqs[0]))
                run_phase([gen_B(seqs[0][0], seqs[0][1], upd_of(seqs[0]))], [1])
                phase_C1(seqs[0][0], seqs[0][1], upd_of(seqs[0]))
                ensure_setup(seqs[1][0])
                run_phase([gen_A(*seqs[1])], [1])
                phase_P(seqs[1][0], seqs[1][1], upd_of(seqs[1]))
                for k in range(n - 1):
                    q0, q1 = seqs[k], seqs[k + 1]
                    nt = ntile(q0)
                    split = max(0, nt - TAIL)
                    run_phase([C2g(q0, range(0, split)), gen_B(q1[0], q1[1], upd_of(q1))], [32 * split, n_yields_B(upd_of(q1))])
                    if k + 2 < n:
                        q2 = seqs[k + 2]
                        ensure_setup(q2[0])
                        run_phase([C2g(q0, range(split, nt), nslots=4, nacc=2), gen_A(*q2)], [32 * TAIL, 8 * (NT_C + NT_L)])
                    else:
                        run_phase([C2g(q0, range(split, nt))], [1])
                    phase_C1(q1[0], q1[1], upd_of(q1))
                    if k + 2 < n:
                        phase_P(q2[0], q2[1], upd_of(q2))
                run_phase([C2g(seqs[n - 1])], [1])
                return

            fuse = NB >= 2 and stop is None
            pend = None
            for (l, b) in seqs:
                ensure_setup(l)
                upd = l < DEPTH - 1
                if pend is not None and not fuse:
                    run_phase([C2g(pend)], [1])
                    pend = None
                    if stop == "C2":
                        return
                run_phase([gen_A(l, b)], [1])
                if stop == "A":
                    return
                phase_P(l, b, upd)
                if stop == "P":
                    return
                if pend is not None:
                    run_phase([C2g(pend), gen_B(l, b, upd)], [n_yields_C2(upd_of(pend)), n_yields_B(upd)])
                else:
                    run_phase([gen_B(l, b, upd)], [1])
                if stop == "B":
                    return
                phase_C1(l, b, upd)
                if stop == "C1":
                    return
                pend = (l, b)
            run_phase([C2g(pend)], [1])

        schedule()
        kb.barrier()
        build.stats = {e.name: e.n for e in kb.engs}
        build.stats['nops'] = kb.nops
    return nc


def _consts(S, CTX):
    f = np.float32
    c = {}
    c["c_ident"] = np.eye(128, dtype=f)
    i16 = np.arange(16, dtype=f)
    c["c_iota"] = np.tile(np.concatenate([i16, 16 * i16, 16 * i16 + 16]).astype(f), (128, 1))
    rows = S // GRID_W
    row = np.repeat(np.arange(rows), GRID_W).astype(f)
    col = np.tile(np.arange(GRID_W), rows).astype(f)

    def rope(rot_dim):
        n = rot_dim // 4
        inv = (f(10000.0) ** (-np.arange(n, dtype=f) / f(n))).astype(f)
        ang = np.concatenate([row[:, None] * inv, col[:, None] * inv], axis=-1).astype(f)
        return np.concatenate([np.cos(ang), np.sin(ang)], axis=-1).astype(f)

    c["c_ropeA"] = rope(64)
    c["c_ropeC"] = rope(32)

    def rc(L):
        out = np.zeros((4, L), f)
        t = np.arange(L)
        for i, w in enumerate((2, 4, 8, 16)):
            lo = np.clip(t - w // 2, 0, L)
            hi = np.clip(t - w // 2 + w, 0, L)
            out[i] = (1.0 / (hi - lo).astype(f)).astype(f)
        return out

    c["c_rcL"] = rc(S)
    c["c_rcC"] = rc(CTX)
    return c


def make_in_maps(inputs, n_cores, NB, S, CTX, DEPTH):
    f = np.float32
    shared = {}
    for k in ("ada_w", "ada_b", "norm1_g", "norm2_g", "w_in", "gqa_qn_g", "gqa_kn_g", "pool_w", "pool_scale", "mla_qn_g",
              "mla_kvn_g", "mla_w_uq", "mla_w_ukv", "w_out", "peer_wq", "peer_subkeys", "final_g"):
        shared[k] = np.ascontiguousarray(np.asarray(inputs[k], dtype=f))
    shared["peer_u"] = np.ascontiguousarray(np.asarray(inputs["peer_u"], dtype=f).reshape(DEPTH * NEXP, D))
    shared["peer_v"] = np.ascontiguousarray(np.asarray(inputs["peer_v"], dtype=f).reshape(DEPTH * NEXP, D))
    shared.update(_consts(S, CTX))
    x = np.asarray(inputs["x"], dtype=f)
    ctx = np.asarray(inputs["ctx"], dtype=f)
    c = np.asarray(inputs["c"], dtype=f)
    c_ctx = np.asarray(inputs["c_ctx"], dtype=f)
    maps = []
    for i in range(n_cores):
        m = dict(shared)
        m["x"] = np.ascontiguousarray(x[i * NB:(i + 1) * NB])
        m["ctx"] = np.ascontiguousarray(ctx[i * NB:(i + 1) * NB])
        m["cT"] = np.ascontiguousarray(np.concatenate([c[i * NB:(i + 1) * NB], c_ctx[None, :]], axis=0).T)
        maps.append(m)
    return maps


_NC_CACHE = {}


def kernel(**inputs):
    B, S, _ = inputs["x"].shape
    CTX = inputs["ctx"].shape[1]
    DEPTH = inputs["ada_w"].shape[0]
    NB = B // N_CORES
    key = (NB, S, CTX, DEPTH)
    if key not in _NC_CACHE:
        _NC_CACHE[key] = build(NB, S, CTX, DEPTH)
    nc = _NC_CACHE[key]
    maps = make_in_maps(inputs, N_CORES, NB, S, CTX, DEPTH)
    res = run_bass_kernel_spmd(nc, maps, core_ids=list(range(N_CORES)))
    return np.concatenate([np.asarray(r["out"], dtype=np.float32) for r in res.results], axis=0)
```

```python
import contextlib
import numpy as np
import concourse.bass as bass
import concourse.mybir as mybir
from concourse.bass_utils import run_bass_kernel_spmd

F32 = mybir.dt.float32
I32 = mybir.dt.int32
U32 = mybir.dt.uint32
ALU = mybir.AluOpType
AF = mybir.ActivationFunctionType
AX = mybir.AxisListType

D = 1024
KD = 8
NMOD = 6
EPS = 1e-6
GRID_W = 64
HD = 64
INC = 1568
NEXP = 16384
N_CORES = 8


class Buf:
    def __init__(self, ap, dsem=None, psum=False):
        self.ap = ap
        self.psum = psum
        self.ws = {}
        self.rs = {}
        self.dsem = dsem

    def __getitem__(self, k):
        return self.ap[k]


class DSem:
    def __init__(self, sem):
        self.sem = sem
        self.cnt = 0


class Eng:
    def __init__(self, name, e, sem, is_pe=False):
        self.name = name
        self.e = e
        self.sem = sem
        self.cnt = 0
        self.seen = {}
        self.is_pe = is_pe
        self.n = 0

    def wait(self, sem, val):
        if val <= 0 or self.seen.get(sem, 0) >= val:
            return
        self.e.wait_ge(sem, val)
        self.seen[sem] = val
        self.n += 1


class KB:
    def __init__(self, nc, es, n_dsem=64):
        self.nc = nc
        self.es = es
        mk = lambda n: es.enter_context(nc.semaphore(n))
        self.pe = Eng("pe", nc.tensor, mk("s_pe"), is_pe=True)
        self.act = Eng("act", nc.scalar, mk("s_act"))
        self.dve = Eng("dve", nc.vector, mk("s_dve"))
        self.pool = Eng("pool", nc.gpsimd, mk("s_pool"))
        self.sp = Eng("sp", nc.sync, None)
        self.engs = [self.pe, self.act, self.dve, self.pool, self.sp]
        self.dsems = [DSem(mk("s_d%d" % i)) for i in range(n_dsem)]
        self.dfree = 0
        self.budget = None
        self.nops = 0
        self.gsems = [DSem(mk("s_g%d" % i)) for i in range(16)]

    def new_phase(self):
        self.barrier()
        self.dfree = 0

    def dsem(self):
        d = self.dsems[self.dfree]
        self.dfree += 1
        return d

    def _deps(self, eng, r, w, aw):
        deps = {}
        def need(dd):
            for s, v in dd.items():
                if deps.get(s, 0) < v:
                    deps[s] = v
        for b in r:
            need(b.ws)
            if b.psum:
                need(b.rs)
        for b in w:
            need(b.ws)
            need(b.rs)
        for b in aw:
            need(b.rs)
            need(b.ws)
        for s, v in deps.items():
            if eng.is_pe and s is eng.sem:
                continue
            eng.wait(s, v)

    def op(self, eng, fn, r=(), w=(), aw=()):
        self.nops += 1
        if self.budget is not None and self.nops > self.budget:
            return None
        self._deps(eng, r, w, aw)
        inst = fn(eng.e)
        eng.cnt += 1
        eng.n += 1
        inst.then_inc(eng.sem, 1)
        s, c = eng.sem, eng.cnt
        for b in r:
            b.rs[s] = c
        for b in w:
            b.ws = {s: c}
            b.rs = {}
        for b in aw:
            b.ws[s] = c
        return inst

    def dma(self, q, out, in_, slot, r=(), w=(), aw=(), fn=None):
        self.nops += 1
        if self.budget is not None and self.nops > self.budget:
            return None
        self._deps(q, r, w, aw)
        if fn is None:
            inst = q.e.dma_start(out=out, in_=in_)
        else:
            inst = fn(q.e)
        q.n += 1
        d = slot.dsem
        d.cnt += 16
        inst.then_inc(d.sem, 16)
        s, c = d.sem, d.cnt
        for b in r:
            b.rs[s] = c
        for b in w:
            b.ws = {s: c}
            b.rs = {}
        for b in aw:
            b.ws[s] = c
        return inst

    def barrier(self):
        allv = {}
        for e in self.engs:
            if e.sem is not None and e.cnt > 0:
                allv[e.sem] = e.cnt
        for d in self.dsems + self.gsems:
            if d.cnt > 0:
                allv[d.sem] = d.cnt
        for e in self.engs:
            for s, v in allv.items():
                e.wait(s, v)


def v3(ap, a, b):
    return ap.rearrange("p (a b) -> p a b", a=a, b=b)


def v4(ap, a, b, c):
    return ap.rearrange("p (a b c) -> p a b c", a=a, b=b, c=c)


class Arena:
    def __init__(self, t, ncols):
        self.t = t
        self.n = ncols
        self.off = 0

    def reset(self):
        self.off = 0

    def take(self, ncols, parts=128):
        assert self.off + ncols <= self.n, ("arena overflow", self.off, ncols, self.n)
        ap = self.t[0:parts, self.off:self.off + ncols]
        self.off += ncols
        return ap


def peer_routing(kb, ar, sS, iota16, eid_i, gate, eoff=0, cache=None):
    dve, act, pool = kb.dve, kb.act, kb.pool
    if cache is None:
        cache = {}
    names = iter(range(1000))
    def B(n, parts=128):
        k = next(names)
        if k not in cache:
            cache[k] = Buf(ar.take(n, parts))
        return cache[k]
    sv = B(256)
    si = B(256)
    sif = B(256)
    wk = B(128)
    cs = B(2048)
    wk2 = B(256)
    ts = B(128)
    pos = B(128)
    posf = B(128)
    pbf = B(128)
    paf = B(128)
    oh = B(2048)
    i1s = B(128)
    i2s = B(128)
    eidf = B(128)
    ex = B(128)
    sm = B(8)
    s3 = v3(sS.ap, 16, 128)
    sv3 = v3(sv.ap, 16, 16)
    siu = si.ap.bitcast(U32)
    si3 = v3(siu, 16, 16)
    for hp in range(16):
        kb.op(dve, lambda e: e.max(out=sv3[:, hp, 0:8], in_=s3[:, hp, :]), r=[sS], aw=[sv])
        kb.op(dve, lambda e: e.max_index(out=si3[:, hp, 0:8], in_max=sv3[:, hp, 0:8], in_values=s3[:, hp, :]), r=[sS, sv], aw=[si])
        kb.op(dve, lambda e: e.match_replace(out=wk.ap, in_to_replace=sv3[:, hp, 0:8], in_values=s3[:, hp, :], imm_value=-1e30), r=[sS, sv], w=[wk])
        kb.op(dve, lambda e: e.max(out=sv3[:, hp, 8:16], in_=wk.ap), r=[wk], aw=[sv])
        kb.op(dve, lambda e: e.max_index(out=si3[:, hp, 8:16], in_max=sv3[:, hp, 8:16], in_values=wk.ap), r=[wk, sv], aw=[si])
    kb.op(dve, lambda e: e.tensor_copy(out=sif.ap, in_=siu), r=[si], w=[sif])
    sv4 = v4(sv.ap, 8, 2, 16)
    sif4 = v4(sif.ap, 8, 2, 16)
    cs4 = v4(cs.ap, 8, 16, 16)
    shp = [128, 8, 16, 16]
    kb.op(dve, lambda e: e.tensor_tensor(out=cs4, in0=sv4[:, :, 0, :].unsqueeze(3).to_broadcast(shp),
                                         in1=sv4[:, :, 1, :].unsqueeze(2).to_broadcast(shp), op=ALU.add), r=[sv], w=[cs])
    cs3 = v3(cs.ap, 8, 256)
    ts3 = v3(ts.ap, 8, 16)
    posu = pos.ap.bitcast(U32)
    pos3 = v3(posu, 8, 16)
    for h in range(8):
        kb.op(dve, lambda e: e.max(out=ts3[:, h, 0:8], in_=cs3[:, h, :]), r=[cs], aw=[ts])
        kb.op(dve, lambda e: e.max_index(out=pos3[:, h, 0:8], in_max=ts3[:, h, 0:8], in_values=cs3[:, h, :]), r=[cs, ts], aw=[pos])
        kb.op(dve, lambda e: e.match_replace(out=wk2.ap, in_to_replace=ts3[:, h, 0:8], in_values=cs3[:, h, :], imm_value=-1e30), r=[cs, ts], w=[wk2])
        kb.op(dve, lambda e: e.max(out=ts3[:, h, 8:16], in_=wk2.ap), r=[wk2], aw=[ts])
        kb.op(dve, lambda e: e.max_index(out=pos3[:, h, 8:16], in_max=ts3[:, h, 8:16], in_values=wk2.ap), r=[wk2, ts], aw=[pos])
    kb.op(dve, lambda e: e.tensor_copy(out=posf.ap, in_=posu), r=[pos], w=[posf])
    oh4 = v4(oh.ap, 8, 16, 16)
    oh2 = cs
    oh2_4 = v4(oh2.ap, 8, 16, 16)
    io_i = iota16.ap[:, 0:16].unsqueeze(1).unsqueeze(1).to_broadcast(shp)
    io_lo = iota16.ap[:, 16:32].unsqueeze(1).unsqueeze(1).to_broadcast(shp)
    io_hi = iota16.ap[:, 32:48].unsqueeze(1).unsqueeze(1).to_broadcast(shp)
    posb = v3(posf.ap, 8, 16).unsqueeze(3).to_broadcast(shp)
    kb.op(dve, lambda e: e.tensor_tensor(out=oh4, in0=posb, in1=io_lo, op=ALU.is_ge), r=[posf, iota16], w=[oh])
    kb.op(dve, lambda e: e.tensor_tensor(out=oh2_4, in0=posb, in1=io_hi, op=ALU.is_ge), r=[posf, iota16], w=[oh2])
    kb.op(dve, lambda e: e.tensor_tensor(out=oh4, in0=oh4, in1=oh2_4, op=ALU.subtract), r=[oh, oh2], w=[oh])
    kb.op(dve, lambda e: e.tensor_tensor(out=oh2_4, in0=oh4, in1=io_i, op=ALU.mult), r=[oh, iota16], w=[oh2])
    kb.op(dve, lambda e: e.reduce_sum(out=paf.ap, in_=v3(oh2.ap, 128, 16), axis=AX.X), r=[oh2], w=[paf])
    kb.op(dve, lambda e: e.tensor_tensor(out=oh2_4, in0=oh4, in1=sif4[:, :, 0, :].unsqueeze(2).to_broadcast(shp), op=ALU.mult), r=[oh, sif], w=[oh2])
    kb.op(dve, lambda e: e.reduce_sum(out=i1s.ap, in_=v3(oh2.ap, 128, 16), axis=AX.X), r=[oh2], w=[i1s])
    kb.op(dve, lambda e: e.scalar_tensor_tensor(out=pbf.ap, in0=paf.ap, scalar=-16.0, in1=posf.ap, op0=ALU.mult, op1=ALU.add), r=[paf, posf], w=[pbf])
    kb.op(dve, lambda e: e.tensor_tensor(out=oh4, in0=v3(pbf.ap, 8, 16).unsqueeze(3).to_broadcast(shp), in1=io_i, op=ALU.is_equal), r=[pbf, iota16], w=[oh])
    kb.op(dve, lambda e: e.tensor_tensor(out=oh4, in0=oh4, in1=sif4[:, :, 1, :].unsqueeze(2).to_broadcast(shp), op=ALU.mult), r=[oh, sif], w=[oh])
    kb.op(dve, lambda e: e.reduce_sum(out=i2s.ap, in_=v3(oh.ap, 128, 16), axis=AX.X), r=[oh], w=[i2s])
    kb.op(dve, lambda e: e.scalar_tensor_tensor(out=eidf.ap, in0=i1s.ap, scalar=128.0, in1=i2s.ap, op0=ALU.mult, op1=ALU.add), r=[i1s, i2s], w=[eidf])
    if eoff:
        kb.op(dve, lambda e: e.tensor_scalar_add(out=eidf.ap, in0=eidf.ap, scalar1=float(eoff)), r=[eidf], w=[eidf])
    kb.op(dve, lambda e: e.tensor_copy(out=eid_i.ap.bitcast(I32), in_=eidf.ap), r=[eidf], w=[eid_i])
    ex3 = v3(ex.ap, 8, 16)
    kb.op(dve, lambda e: e.tensor_tensor(out=ex3, in0=ts3, in1=ts3[:, :, 0:1].to_broadcast([128, 8, 16]), op=ALU.subtract), r=[ts], w=[ex])
    kb.op(act, lambda e: e.activation(out=ex.ap, in_=ex.ap, func=AF.Exp), r=[ex], w=[ex])
    kb.op(dve, lambda e: e.reduce_sum(out=sm.ap, in_=ex3, axis=AX.X), r=[ex], w=[sm])
    kb.op(dve, lambda e: e.reciprocal(out=sm.ap, in_=sm.ap), r=[sm], w=[sm])
    kb.op(dve, lambda e: e.tensor_tensor(out=v3(gate.ap, 8, 16), in0=ex3, in1=sm.ap.unsqueeze(2).to_broadcast([128, 8, 16]), op=ALU.mult), r=[ex, sm], w=[gate])


WNAMES = [("ada_w", None), ("ada_b", None), ("norm1_g", None), ("norm2_g", None), ("w_in", None),
          ("gqa_qn_g", None), ("gqa_kn_g", None), ("pool_w", None), ("pool_scale", None), ("mla_qn_g", None),
          ("mla_kvn_g", None), ("mla_w_uq", None), ("mla_w_ukv", None), ("w_out", None), ("peer_wq", None),
          ("peer_subkeys", None), ("final_g", None)]


def build(NB, S, CTX, DEPTH, stop=None, budget=None):
    NT_L = S // 128
    NT_C = CTX // 128
    NK = CTX + S
    NKC = NK // 128
    nc = bass.Bass("TRN2", target_bir_lowering=False)

    def din(name, shape, dtype=F32):
        return nc.dram_tensor(name, list(shape), dtype, kind="ExternalInput").ap()

    def dscr(name, shape, dtype=F32):
        return nc.dram_tensor(name, list(shape), dtype).ap()

    x_d = din("x", [NB, S, D])
    ctx_d = din("ctx", [NB, CTX, D])
    cT_d = din("cT", [D, NB + 1])
    ada_w = din("ada_w", [DEPTH, D, NMOD * D])
    ada_b = din("ada_b", [DEPTH, NMOD * D])
    norm1_g = din("norm1_g", [DEPTH, D])
    norm2_g = din("norm2_g", [DEPTH, D])
    w_in = din("w_in", [DEPTH, D, INC])
    gqa_qn_g = din("gqa_qn_g", [DEPTH, 64])
    gqa_kn_g = din("gqa_kn_g", [DEPTH, 64])
    pool_w = din("pool_w", [DEPTH, 4, 64, 64])
    pool_scale = din("pool_scale", [DEPTH, 256])
    mla_qn_g = din("mla_qn_g", [DEPTH, 384])
    mla_kvn_g = din("mla_kvn_g", [DEPTH, 256])
    mla_w_uq = din("mla_w_uq", [DEPTH, 384, 576])
    mla_w_ukv = din("mla_w_ukv", [DEPTH, 256, 768])
    w_out = din("w_out", [DEPTH, D, D])
    peer_wq = din("peer_wq", [DEPTH, D, 2048])
    peer_subkeys = din("peer_subkeys", [DEPTH, 8, 2, 128, 128])
    peer_u = din("peer_u", [DEPTH * NEXP, D])
    peer_v = din("peer_v", [DEPTH * NEXP, D])
    final_g = din("final_g", [D])
    ident_d = din("c_ident", [128, 128])
    iota_d = din("c_iota", [128, 48])
    ropeA_d = din("c_ropeA", [S, 64])
    ropeC_d = din("c_ropeC", [S, 32])
    rcL_d = din("c_rcL", [4, S])
    rcC_d = din("c_rcC", [4, CTX])
    out_d = nc.dram_tensor("out", [NB, S, D], F32, kind="ExternalOutput").ap()

    mod_d = dscr("mod_d", [DEPTH, NB + 1, NMOD * D])
    QaT_d = dscr("QaT_d", [3, 128, NK])
    KaT_d = dscr("KaT_d", [128, NK])
    Va_d = dscr("Va_d", [NK, 128])
    bT_d = dscr("bT_d", [2, 128, NK])
    obT_d = dscr("obT_d", [2, 128, NK])
    QcT_d = dscr("QcT_d", [6, 96, NK])
    KcT_d = dscr("KcT_d", [6, 96, NK])
    Vc_d = dscr("Vc_d", [NK, 384])
    mix_d = dscr("mix_d", [NK, 12, 65])
    xmid_d = dscr("xmid_d", [NK, D])
    h2_d = dscr("h2_d", [NK, D])
    eid_d = dscr("eid_d", [NK, 128], I32)
    gate_d = dscr("gate_d", [NK, 128])
    xl1_d = dscr("xl1_d", [NB, S, D])
    xc1_d = dscr("xc1_d", [NB, CTX, D])

    GC, R1C, R2C = 3840, 24704, 24616
    with contextlib.ExitStack() as es:
        kb = KB(nc, es, n_dsem=44)
        kb.budget = budget
        pe, act, dve, pool, sp = kb.pe, kb.act, kb.dve, kb.pool, kb.sp
        GT = es.enter_context(nc.sbuf_tensor("GT", [128, GC], F32))
        R1T = es.enter_context(nc.sbuf_tensor("R1T", [128, R1C], F32))
        R2T = es.enter_context(nc.sbuf_tensor("R2T", [128, R2C], F32))
        PSALL = es.enter_context(nc.psum_tensor("PSALL", [128, 4096], F32))
        G = Arena(GT, GC)
        R1 = Arena(R1T, R1C)
        R2 = Arena(R2T, R2C)

        def PSB(c0, c1, parts=128):
            return Buf(PSALL[0:parts, c0:c1], psum=True)

        def rms_a(src_ap, src_bufs, junk, ss, n):
            kb.op(act, lambda e: e.activation(out=junk.ap[:, 0:n], in_=src_ap, func=AF.Square, accum_out=ss.ap), r=src_bufs, w=[junk, ss])
            kb.op(act, lambda e: e.activation(out=ss.ap, in_=ss.ap, func=AF.Sqrt, scale=1.0 / n, bias=EPS), r=[ss], w=[ss])

        def rms_b(ss):
            kb.op(dve, lambda e: e.reciprocal(out=ss.ap, in_=ss.ap), r=[ss], w=[ss])

        def rms_rstd(src_ap, src_bufs, junk, ss, n):
            kb.op(act, lambda e: e.activation(out=junk.ap[:, 0:n], in_=src_ap, func=AF.Square, accum_out=ss.ap), r=src_bufs, w=[junk, ss])
            kb.op(act, lambda e: e.activation(out=ss.ap, in_=ss.ap, func=AF.Sqrt, scale=1.0 / n, bias=EPS), r=[ss], w=[ss])
            kb.op(dve, lambda e: e.reciprocal(out=ss.ap, in_=ss.ap), r=[ss], w=[ss])

        def transposes(src_ap_fn, n, src_bufs, ident, PT, pt_ap_fn, dst, dst_ap, cols, parts=128):
            for k in range(n):
                kb.op(pe, lambda e: e.transpose(out=pt_ap_fn(k), in_=src_ap_fn(k), identity=ident.ap), r=src_bufs + [ident],
                      w=[PT] if k == 0 else (), aw=() if k == 0 else [PT])
            kb.op(act, lambda e: e.copy(out=dst_ap, in_=PT.ap[0:parts, 0:cols]), r=[PT], w=[dst])

        ident = Buf(G.take(128), kb.dsem())
        iota = Buf(G.take(48), kb.dsem())
        ropeA = Buf(G.take(NT_L * 64), kb.dsem())
        ropeC = Buf(G.take(NT_L * 32), kb.dsem())
        FG = Buf(G.take(1024), kb.dsem())
        gq = Buf(G.take(64))
        gk = Buf(G.take(64))
        gcq = Buf(G.take(384))
        gckv = Buf(G.take(256))
        psc = Buf(G.take(2))
        PWbd = Buf(G.take(256))
        kb.dma(sp, ident.ap, ident_d, ident, w=[ident])
        kb.dma(sp, iota.ap, iota_d, iota, w=[iota])
        kb.dma(sp, v3(ropeA.ap, NT_L, 64), ropeA_d.rearrange("(n p) c -> p n c", p=128), ropeA, w=[ropeA])
        kb.dma(sp, v3(ropeC.ap, NT_L, 32), ropeC_d.rearrange("(n p) c -> p n c", p=128), ropeC, w=[ropeC])
        kb.dma(sp, FG.ap, final_g.partition_broadcast(128), FG, w=[FG])
        NB1 = NB + 1
        cT = Buf(R2.take(KD * NB1), kb.dsem())
        cT3 = v3(cT.ap, KD, NB1)
        kb.dma(sp, cT3, cT_d.rearrange("(k p) n -> p k n", p=128), cT, w=[cT])
        kb.op(act, lambda e: e.activation(out=cT.ap, in_=cT.ap, func=AF.Silu), r=[cT], w=[cT])
        adab = Buf(R2.take(NMOD * D, NB1), kb.dsem())
        g1b = Buf(R2.take(D, NB1), kb.dsem())
        g2b = Buf(R2.take(D, NB1), kb.dsem())
        modsb = Buf(R2.take(NMOD * D, NB1), kb.dsem())
        wsl = [Buf(R1.take(KD * 512), kb.dsem()) for _ in range(2)]
        pps = [PSB(0, 512, NB1), PSB(512, 1024, NB1)]
        for l in range(DEPTH):
            kb.dma(sp, adab.ap, ada_b[l].partition_broadcast(NB1), adab, w=[adab])
            kb.dma(sp, g1b.ap, norm1_g[l].partition_broadcast(NB1), g1b, w=[g1b])
            kb.dma(sp, g2b.ap, norm2_g[l].partition_broadcast(NB1), g2b, w=[g2b])
            for pc in range(12):
                ws_ = wsl[pc % 2]
                pp = pps[pc % 2]
                kb.dma(sp, v3(ws_.ap, KD, 512), ada_w[l][:, pc * 512:(pc + 1) * 512].rearrange("(k p) n -> p k n", p=128), ws_, w=[ws_])
                for k in range(KD):
                    kb.op(pe, lambda e: e.matmul(pp.ap, lhsT=cT3[:, k, :], rhs=v3(ws_.ap, KD, 512)[:, k, :], start=(k == 0), stop=(k == KD - 1)),
                          r=[cT, ws_], w=[pp] if k == 0 else (), aw=() if k == 0 else [pp])
                kb.op(dve, lambda e: e.tensor_tensor(out=modsb.ap[:, pc * 512:(pc + 1) * 512], in0=pp.ap, in1=adab.ap[:, pc * 512:(pc + 1) * 512], op=ALU.add),
                      r=[pp, adab], w=[modsb] if pc == 0 else (), aw=() if pc == 0 else [modsb])
            kb.op(dve, lambda e: e.scalar_tensor_tensor(out=modsb.ap[:, D:2 * D], in0=modsb.ap[:, D:2 * D], scalar=1.0, in1=g1b.ap, op0=ALU.add, op1=ALU.mult),
                  r=[g1b], w=[modsb])
            kb.op(dve, lambda e: e.scalar_tensor_tensor(out=modsb.ap[:, 4 * D:5 * D], in0=modsb.ap[:, 4 * D:5 * D], scalar=1.0, in1=g2b.ap, op0=ALU.add, op1=ALU.mult),
                  r=[g2b], w=[modsb])
            kb.dma(sp, mod_d[l], modsb.ap, modsb, r=[modsb])

        def bc_load(dst, l, row, i):
            kb.dma(sp, dst.ap, mod_d[l, row, i * D:(i + 1) * D].partition_broadcast(128), dst, w=[dst])

        def tok_tiles(with_ctx):
            tl = []
            if with_ctx:
                tl += [(True, i, i * 128) for i in range(NT_C)]
            tl += [(False, i, CTX + i * 128) for i in range(NT_L)]
            return tl

        def x_src(l, b, is_ctx, ti):
            if l == 0:
                return (ctx_d if is_ctx else x_d)[b, ti * 128:(ti + 1) * 128, :]
            return (xc1_d if is_ctx else xl1_d)[b, ti * 128:(ti + 1) * 128, :]

        def gen_A(l, b):
            A = lambda n, parts=128, d=False: Buf(R2.take(n, parts), kb.dsem() if d else None)
            w_in_sb = Buf(R1.take(KD * INC), kb.dsem())
            w_uq_sb = Buf(R1.take(3 * 576), kb.dsem())
            w_ukv_sb = Buf(R1.take(2 * 768), kb.dsem())
            wi3 = v3(w_in_sb.ap, KD, INC)
            wq3 = v3(w_uq_sb.ap, 3, 576)
            wkv3 = v3(w_ukv_sb.ap, 2, 768)
            kb.dma(sp, wi3, w_in[l].rearrange("(k p) n -> p k n", p=128), w_in_sb, w=[w_in_sb])
            kb.dma(sp, wq3, mla_w_uq[l].rearrange("(k p) n -> p k n", p=128), w_uq_sb, w=[w_uq_sb])
            kb.dma(sp, wkv3, mla_w_ukv[l].rearrange("(k p) n -> p k n", p=128), w_ukv_sb, w=[w_ukv_sb])
            bcs = {}
            for is_ctx in (True, False):
                row = NB if is_ctx else b
                a1 = A(D, d=True)
                sh1 = A(D, d=True)
                bc_load(a1, l, row, 1)
                bc_load(sh1, l, row, 0)
                bcs[is_ctx] = (a1, sh1)
            xts = [A(D, d=True) for _ in range(2)]
            h = A(D)
            hT = A(D)
            pj = A(INC, d=True)
            sq = A(512)
            ss = A(1); ss8 = A(8); ssq = A(1); ssk = A(1)
            qkn = A(512)
            qkr = A(512)
            t1 = A(256); t2 = A(256); t3 = A(256); t4 = A(256)
            qkT = A(512, d=True)
            bTs = A(256, d=True)
            cqn = A(384)
            cqnT = A(384)
            qm = A(576)
            u1 = A(96); u2 = A(96); u3 = A(96); u4 = A(96)
            qcT = A(768, 96, d=True)
            ckvn = A(256)
            ckvnT = A(256)
            kfull = A(576)
            vc = A(384, d=True)
            kcT = A(768, 96, d=True)
            krr = A(32)
            k1 = A(16); k2 = A(16); k3 = A(16); k4 = A(16)
            PT = PSB(0, 1024)
            PJ = PSB(1024, 3072)
            PX = PSB(3072, 4096)
            tiles = tok_tiles(True)

            def load_x(i):
                is_ctx, ti, tk0 = tiles[i]
                xt = xts[i % 2]
                kb.dma(sp, xt.ap, x_src(l, b, is_ctx, ti), xt, w=[xt])

            load_x(0)
            for i, (is_ctx, ti, tk0) in enumerate(tiles):
                if i + 1 < len(tiles):
                    load_x(i + 1)
                xt = xts[i % 2]
                a1, sh1 = bcs[is_ctx]
                rms_a(xt.ap, [xt], h, ss, D)
                yield
                rms_b(ss)
                kb.op(dve, lambda e: e.scalar_tensor_tensor(out=h.ap, in0=xt.ap, scalar=ss.ap[:, 0:1], in1=a1.ap, op0=ALU.mult, op1=ALU.mult), r=[xt, ss, a1], w=[h])
                kb.op(dve, lambda e: e.tensor_tensor(out=h.ap, in0=h.ap, in1=sh1.ap, op=ALU.add), r=[sh1], w=[h])
                transposes(lambda k: h.ap[:, k * 128:(k + 1) * 128], KD, [h], ident, PT, lambda k: PT.ap[:, k * 128:(k + 1) * 128], hT, hT.ap, 1024)
                hT3 = v3(hT.ap, KD, 128)
                for pc, (c0, c1) in enumerate(((0, 512), (512, 1024), (1024, 1536), (1536, INC))):
                    for k in range(KD):
                        kb.op(pe, lambda e: e.matmul(PJ.ap[:, c0:c1], lhsT=hT3[:, k, :], rhs=wi3[:, k, c0:c1], start=(k == 0), stop=(k == KD - 1)),
                              r=[hT, w_in_sb], w=[PJ] if (pc == 0 and k == 0) else (), aw=() if (pc == 0 and k == 0) else [PJ])
                kb.op(act, lambda e: e.copy(out=pj.ap, in_=PJ.ap[:, 0:INC]), r=[PJ], w=[pj])
                yield
                kb.op(dve, lambda e: e.tensor_tensor(out=sq.ap, in0=pj.ap[:, 0:512], in1=pj.ap[:, 0:512], op=ALU.mult), r=[pj], w=[sq])
                kb.op(dve, lambda e: e.reduce_sum(out=ss8.ap, in_=v3(sq.ap, 8, 64), axis=AX.X), r=[sq], w=[ss8])
                kb.op(act, lambda e: e.activation(out=ss8.ap, in_=ss8.ap, func=AF.Sqrt, scale=1.0 / 64, bias=EPS), r=[ss8], w=[ss8])
                yield
                kb.op(dve, lambda e: e.reciprocal(out=ss8.ap, in_=ss8.ap), r=[ss8], w=[ss8])
                qkn3 = v3(qkn.ap, 8, 64)
                kb.op(dve, lambda e: e.tensor_tensor(out=qkn3, in0=v3(pj.ap[:, 0:512], 8, 64), in1=ss8.ap.unsqueeze(2).to_broadcast([128, 8, 64]), op=ALU.mult), r=[pj, ss8], w=[qkn])
                kb.op(dve, lambda e: e.tensor_tensor(out=qkn3[:, 0:6, :], in0=qkn3[:, 0:6, :], in1=gq.ap.unsqueeze(1).to_broadcast([128, 6, 64]), op=ALU.mult), r=[gq], w=[qkn])
                kb.op(dve, lambda e: e.tensor_tensor(out=qkn3[:, 6:8, :], in0=qkn3[:, 6:8, :], in1=gk.ap.unsqueeze(1).to_broadcast([128, 2, 64]), op=ALU.mult), r=[gk], w=[qkn])
                if is_ctx:
                    qsrc = qkn
                else:
                    qsrc = qkr
                    q4 = v4(qkn.ap, 8, 2, 32)
                    r4 = v4(qkr.ap, 8, 2, 32)
                    rA = v3(ropeA.ap, NT_L, 64)
                    cosb = rA[:, ti, 0:32].unsqueeze(1).to_broadcast([128, 8, 32])
                    sinb = rA[:, ti, 32:64].unsqueeze(1).to_broadcast([128, 8, 32])
                    T = lambda t: v3(t.ap, 8, 32)
                    kb.op(dve, lambda e: e.tensor_tensor(out=T(t1), in0=q4[:, :, 0, :], in1=cosb, op=ALU.mult), r=[qkn, ropeA], w=[t1])
                    kb.op(dve, lambda e: e.tensor_tensor(out=T(t2), in0=q4[:, :, 1, :], in1=sinb, op=ALU.mult), r=[qkn, ropeA], w=[t2])
                    kb.op(dve, lambda e: e.tensor_tensor(out=r4[:, :, 0, :], in0=T(t1), in1=T(t2), op=ALU.subtract), r=[t1, t2], w=[qkr])
                    kb.op(dve, lambda e: e.tensor_tensor(out=T(t3), in0=q4[:, :, 0, :], in1=sinb, op=ALU.mult), r=[qkn, ropeA], w=[t3])
                    kb.op(dve, lambda e: e.tensor_tensor(out=T(t4), in0=q4[:, :, 1, :], in1=cosb, op=ALU.mult), r=[qkn, ropeA], w=[t4])
                    kb.op(dve, lambda e: e.tensor_tensor(out=r4[:, :, 1, :], in0=T(t3), in1=T(t4), op=ALU.add), r=[t3, t4], aw=[qkr])
                transposes(lambda k: qsrc.ap[:, k * 128:(k + 1) * 128], 4, [qsrc], ident, PJ, lambda k: PJ.ap[:, k * 128:(k + 1) * 128], qkT, qkT.ap, 512)
                qkT3 = v3(qkT.ap, 4, 128)
                kb.dma(sp, QaT_d[:, :, tk0:tk0 + 128].rearrange("j p t -> p j t"), qkT3[:, 0:3, :], qkT, r=[qkT])
                kb.dma(sp, KaT_d[:, tk0:tk0 + 128], qkT3[:, 3, :], qkT, r=[qkT])
                kb.dma(sp, Va_d[tk0:tk0 + 128, :], pj.ap[:, 512:640], pj, r=[pj])
                transposes(lambda k: pj.ap[:, 640 + k * 128:640 + (k + 1) * 128], 2, [pj], ident, PX, lambda k: PX.ap[:, k * 128:(k + 1) * 128], bTs, bTs.ap, 256)
                kb.dma(sp, bT_d[:, :, tk0:tk0 + 128].rearrange("j p t -> p j t"), v3(bTs.ap, 2, 128), bTs, r=[bTs])
                rms_a(pj.ap[:, 896:1280], [pj], cqn, ssq, 384)
                yield
                rms_b(ssq)
                kb.op(dve, lambda e: e.scalar_tensor_tensor(out=cqn.ap, in0=pj.ap[:, 896:1280], scalar=ssq.ap[:, 0:1], in1=gcq.ap, op0=ALU.mult, op1=ALU.mult), r=[pj, ssq, gcq], w=[cqn])
                transposes(lambda k: cqn.ap[:, k * 128:(k + 1) * 128], 3, [cqn], ident, PT, lambda k: PT.ap[:, k * 128:(k + 1) * 128], cqnT, cqnT.ap, 384)
                cq3 = v3(cqnT.ap, 3, 128)
                for pc, (c0, c1) in enumerate(((0, 512), (512, 576))):
                    for k in range(3):
                        kb.op(pe, lambda e: e.matmul(PX.ap[:, c0:c1], lhsT=cq3[:, k, :], rhs=wq3[:, k, c0:c1], start=(k == 0), stop=(k == 2)),
                              r=[cqnT, w_uq_sb], w=[PX] if (pc == 0 and k == 0) else (), aw=() if (pc == 0 and k == 0) else [PX])
                kb.op(act, lambda e: e.copy(out=qm.ap, in_=PX.ap[:, 0:576]), r=[PX], w=[qm])
                qm3 = v3(qm.ap, 6, 96)
                if not is_ctx:
                    yield
                    rC = v3(ropeC.ap, NT_L, 32)
                    cosc = rC[:, ti, 0:16].unsqueeze(1).to_broadcast([128, 6, 16])
                    sinc = rC[:, ti, 16:32].unsqueeze(1).to_broadcast([128, 6, 16])
                    U = lambda t: v3(t.ap, 6, 16)
                    x1 = qm3[:, :, 64:80]
                    x2 = qm3[:, :, 80:96]
                    kb.op(dve, lambda e: e.tensor_tensor(out=U(u1), in0=x1, in1=cosc, op=ALU.mult), r=[qm, ropeC], w=[u1])
                    kb.op(dve, lambda e: e.tensor_tensor(out=U(u2), in0=x2, in1=sinc, op=ALU.mult), r=[qm, ropeC], w=[u2])
                    kb.op(dve, lambda e: e.tensor_tensor(out=U(u3), in0=x1, in1=sinc, op=ALU.mult), r=[qm, ropeC], w=[u3])
                    kb.op(dve, lambda e: e.tensor_tensor(out=U(u4), in0=x2, in1=cosc, op=ALU.mult), r=[qm, ropeC], w=[u4])
                    kb.op(dve, lambda e: e.tensor_tensor(out=x1, in0=U(u1), in1=U(u2), op=ALU.subtract), r=[u1, u2, u3, u4], w=[qm])
                    kb.op(dve, lambda e: e.tensor_tensor(out=x2, in0=U(u3), in1=U(u4), op=ALU.add), r=[u3, u4], w=[qm])
                transposes(lambda hh: qm3[:, hh, :], 6, [qm], ident, PT, lambda hh: PSALL[0:96, hh * 128:(hh + 1) * 128], qcT, qcT.ap, 768, parts=96)
                kb.dma(sp, QcT_d[:, :, tk0:tk0 + 128].rearrange("h p t -> p h t"), v3(qcT.ap, 6, 128), qcT, r=[qcT])
                rms_a(pj.ap[:, 1280:1536], [pj], ckvn, ssk, 256)
                yield
                rms_b(ssk)
                kb.op(dve, lambda e: e.scalar_tensor_tensor(out=ckvn.ap, in0=pj.ap[:, 1280:1536], scalar=ssk.ap[:, 0:1], in1=gckv.ap, op0=ALU.mult, op1=ALU.mult), r=[pj, ssk, gckv], w=[ckvn])
                transposes(lambda k: ckvn.ap[:, k * 128:(k + 1) * 128], 2, [ckvn], ident, PJ, lambda k: PJ.ap[:, k * 128:(k + 1) * 128], ckvnT, ckvnT.ap, 256)
                ck3 = v3(ckvnT.ap, 2, 128)
                for pc, (c0, c1) in enumerate(((0, 512), (512, 768))):
                    for k in range(2):
                        kb.op(pe, lambda e: e.matmul(PX.ap[:, c0:c1], lhsT=ck3[:, k, :], rhs=wkv3[:, k, c0:c1], start=(k == 0), stop=(k == 1)),
                              r=[ckvnT, w_ukv_sb], w=[PX] if (pc == 0 and k == 0) else (), aw=() if (pc == 0 and k == 0) else [PX])
                kf3 = v3(kfull.ap, 6, 96)
                px3 = v3(PX.ap[:, 0:768], 6, 128)
                yield
                kb.op(dve, lambda e: e.tensor_copy(out=kf3[:, :, 0:64], in_=px3[:, :, 0:64]), r=[PX], w=[kfull])
                kb.op(act, lambda e: e.copy(out=v3(vc.ap, 6, 64), in_=px3[:, :, 64:128]), r=[PX], w=[vc])
                kb.dma(sp, Vc_d[tk0:tk0 + 128, :], vc.ap, vc, r=[vc])
                if is_ctx:
                    krsrc_ap, krsrc = pj.ap[:, 1536:1568], pj
                else:
                    rC = v3(ropeC.ap, NT_L, 32)
                    cos1 = rC[:, ti, 0:16]
                    sin1 = rC[:, ti, 16:32]
                    y1 = pj.ap[:, 1536:1552]
                    y2 = pj.ap[:, 1552:1568]
                    kb.op(dve, lambda e: e.tensor_tensor(out=k1.ap, in0=y1, in1=cos1, op=ALU.mult), r=[pj, ropeC], w=[k1])
                    kb.op(dve, lambda e: e.tensor_tensor(out=k2.ap, in0=y2, in1=sin1, op=ALU.mult), r=[pj, ropeC], w=[k2])
                    kb.op(dve, lambda e: e.tensor_tensor(out=k3.ap, in0=y1, in1=sin1, op=ALU.mult), r=[pj, ropeC], w=[k3])
                    kb.op(dve, lambda e: e.tensor_tensor(out=k4.ap, in0=y2, in1=cos1, op=ALU.mult), r=[pj, ropeC], w=[k4])
                    kb.op(dve, lambda e: e.tensor_tensor(out=krr.ap[:, 0:16], in0=k1.ap, in1=k2.ap, op=ALU.subtract), r=[k1, k2], w=[krr])
                    kb.op(dve, lambda e: e.tensor_tensor(out=krr.ap[:, 16:32], in0=k3.ap, in1=k4.ap, op=ALU.add), r=[k3, k4], aw=[krr])
                    krsrc_ap, krsrc = krr.ap, krr
                kb.op(dve, lambda e: e.tensor_copy(out=kf3[:, :, 64:96], in_=krsrc_ap.unsqueeze(1).to_broadcast([128, 6, 32])), r=[krsrc], aw=[kfull])
                transposes(lambda hh: kf3[:, hh, :], 6, [kfull], ident, PT, lambda hh: PSALL[0:96, hh * 128:(hh + 1) * 128], kcT, kcT.ap, 768, parts=96)
                kb.dma(sp, KcT_d[:, :, tk0:tk0 + 128].rearrange("h p t -> p h t"), v3(kcT.ap, 6, 128), kcT, r=[kcT])
                yield

        def phase_P(l, b, with_ctx):
            kb.new_phase()
            R1.reset()
            R2.reset()
            Lmax = S
            Lp = Lmax + 16
            xb = Buf(R1.take(2 * Lp), kb.dsem())
            Wa = Buf(R1.take(2 * Lp))
            Wb = Buf(R1.take(2 * Lp))
            PTb = Buf(R1.take(2 * Lmax))
            rc = Buf(R1.take(4 * Lmax), kb.dsem())
            obs = [Buf(R2.take(512), kb.dsem()) for _ in range(2)]
            pbs = [PSB(i * 512, (i + 1) * 512) for i in range(4)]
            streams = ([(CTX, 0, rcC_d)] if with_ctx else []) + [(S, CTX, rcL_d)]
            nmm = 0
            for (L, tk0, rc_d) in streams:
                Lq = L + 16
                x3 = v3(xb.ap[:, 0:2 * Lq], 2, Lq)
                a3 = v3(Wa.ap[:, 0:2 * Lq], 2, Lq)
                b3 = v3(Wb.ap[:, 0:2 * Lq], 2, Lq)
                p3 = v3(PTb.ap[:, 0:2 * L], 2, L)
                r3 = v3(rc.ap[:, 0:4 * L], 4, L)
                kb.op(pool, lambda e: e.memset(xb.ap[:, 0:2 * Lq], 0.0), w=[xb])
                kb.dma(sp, x3[:, :, 8:8 + L], bT_d[:, :, tk0:tk0 + L].rearrange("j p t -> p j t"), xb, w=[xb])
                kb.dma(sp, rc.ap[:, 0:4 * L], rc_d.rearrange("w s -> (w s)").partition_broadcast(128), rc, w=[rc])
                kb.op(dve, lambda e: e.tensor_tensor(out=a3[:, :, 1:Lq - 1], in0=x3[:, :, 0:Lq - 2], in1=x3[:, :, 1:Lq - 1], op=ALU.add), r=[xb], w=[Wa])

                def grp(g, Wsrc, w3):
                    j, p0 = g // 2, (g % 2) * 64
                    kb.op(dve, lambda e: e.tensor_tensor(out=p3[p0:p0 + 64, j, :], in0=w3[p0:p0 + 64, j, 8:8 + L], in1=r3[p0:p0 + 64, g, :], op=ALU.mult), r=[Wsrc, rc], aw=[PTb])
                    kb.op(dve, lambda e: e.tensor_tensor(out=p3[p0:p0 + 64, j, :], in0=p3[p0:p0 + 64, j, :], in1=x3[p0:p0 + 64, j, 8:8 + L], op=ALU.subtract), r=[xb], aw=[PTb])

                grp(0, Wa, a3)
                kb.op(dve, lambda e: e.tensor_tensor(out=b3[:, :, 2:Lq - 2], in0=a3[:, :, 1:Lq - 3], in1=a3[:, :, 3:Lq - 1], op=ALU.add), r=[Wa], w=[Wb])
                grp(1, Wb, b3)
                kb.op(dve, lambda e: e.tensor_tensor(out=a3[:, :, 4:Lq - 4], in0=b3[:, :, 2:Lq - 6], in1=b3[:, :, 6:Lq - 2], op=ALU.add), r=[Wb], w=[Wa])
                grp(2, Wa, a3)
                kb.op(dve, lambda e: e.tensor_tensor(out=b3[:, :, 8:Lq - 8], in0=a3[:, :, 4:Lq - 12], in1=a3[:, :, 12:Lq - 4], op=ALU.add), r=[Wa], w=[Wb])
                grp(3, Wb, b3)
                for j in range(2):
                    for c0 in range(0, L, 512):
                        cn = min(512, L - c0)
                        pb_ = pbs[nmm % 4]
                        ob = obs[nmm % 2]
                        nmm += 1
                        kb.op(pe, lambda e: e.matmul(pb_.ap[:, 0:cn], lhsT=v3(PWbd.ap, 2, 128)[:, j, :], rhs=p3[:, j, c0:c0 + cn], start=True, stop=True), r=[PWbd, PTb], w=[pb_])
                        kb.op(act, lambda e: e.activation(out=ob.ap[:, 0:cn], in_=pb_.ap[:, 0:cn], func=AF.Identity, scale=psc.ap[:, j:j + 1]), r=[pb_, psc], w=[ob])
                        kb.dma(sp, obT_d[j, :, tk0 + c0:tk0 + c0 + cn], ob.ap[:, 0:cn], ob, r=[ob])

        def gen_B(l, b, with_ctx):
            A = lambda n, parts=128, d=False: Buf(R2.take(n, parts), kb.dsem() if d else None)
            kTs = [A(NK, 96, d=True) for _ in range(2)]
            qTs = [A(NK, 96, d=True) for _ in range(2)]
            vSs = [A(NKC * 65, d=True) for _ in range(2)]
            PTs = [A(512) for _ in range(3)]
            osts = [A(4 * 65, d=True) for _ in range(2)]
            for vS in vSs:
                kb.op(pool, lambda e: e.memset(vS.ap, 1.0), w=[vS])
            PS = [PSB(i * 512, (i + 1) * 512) for i in range(3)]
            PO = [PSB((3 + i) * 512, (3 + i) * 512 + 65) for i in range(4)]
            jobs = []
            for hh in range(6):
                g = hh // 3
                jobs.append((64, KaT_d[g * 64:(g + 1) * 64, :], Va_d[:, g * 64:(g + 1) * 64], QaT_d[hh // 2, (hh % 2) * 64:(hh % 2) * 64 + 64, :], 64 ** -0.5, hh))
            for hh in range(6):
                jobs.append((96, KcT_d[hh], Vc_d[:, hh * 64:(hh + 1) * 64], QcT_d[hh], 96 ** -0.5, 6 + hh))

            def load_job(j):
                dk, ksrc, vsrc, qsrc, sc, col = jobs[j]
                kT, qT, vS = kTs[j % 2], qTs[j % 2], vSs[j % 2]
                kb.dma(sp, kT.ap[0:dk, :], ksrc, kT, w=[kT])
                kb.dma(sp, qT.ap[0:dk, :], qsrc, qT, w=[qT])
                kb.dma(sp, v3(vS.ap, NKC, 65)[:, :, 0:64], vsrc.rearrange("(c p) d -> p c d", p=128), vS, w=[vS])

            blocks = []
            if with_ctx:
                blocks.append((0, CTX, list(range(NT_C))))
            for q0 in range(CTX, NK, 512):
                blocks.append((q0, min(512, NK - q0), list(range(NKC))))
            load_job(0)
            nblk = 0
            for j in range(len(jobs)):
                if j + 1 < len(jobs):
                    load_job(j + 1)
                dk, ksrc, vsrc, qsrc, sc, col = jobs[j]
                kT, qT, vS = kTs[j % 2], qTs[j % 2], vSs[j % 2]
                vS3 = v3(vS.ap, NKC, 65)
                for (q0, qn, kcs) in blocks:
                    nsub = qn // 128
                    ost = osts[nblk % 2]
                    nblk += 1

                    def emit_S(idx):
                        kc = kcs[idx]
                        ps_ = PS[idx % 3]
                        kb.op(pe, lambda e: e.matmul(ps_.ap[:, 0:qn], lhsT=kT.ap[0:dk, kc * 128:(kc + 1) * 128], rhs=qT.ap[0:dk, q0:q0 + qn], start=True, stop=True), r=[kT, qT], w=[ps_])
                        pt_ = PTs[idx % 3]
                        kb.op(act, lambda e: e.activation(out=pt_.ap[:, 0:qn], in_=ps_.ap[:, 0:qn], func=AF.Exp, scale=float(sc)), r=[ps_], w=[pt_])

                    def emit_PV(idx):
                        kc = kcs[idx]
                        pt_ = PTs[idx % 3]
                        for s_ in range(nsub):
                            kb.op(pe, lambda e: e.matmul(PO[s_].ap, lhsT=pt_.ap[:, s_ * 128:(s_ + 1) * 128], rhs=vS3[:, kc, :], start=(idx == 0), stop=(idx == len(kcs) - 1)),
                                  r=[pt_, vS], w=[PO[s_]] if idx == 0 else (), aw=() if idx == 0 else [PO[s_]])

                    emit_S(0)
                    for idx in range(len(kcs)):
                        if idx + 1 < len(kcs):
                            emit_S(idx + 1)
                        emit_PV(idx)
                    o3 = v3(ost.ap, 4, 65)
                    for s_ in range(nsub):
                        kb.op(act, lambda e: e.copy(out=o3[:, s_, :], in_=PO[s_].ap), r=[PO[s_]], w=[ost] if s_ == 0 else (), aw=() if s_ == 0 else [ost])
                    kb.dma(sp, mix_d[q0:q0 + qn, col, :].rearrange("(s p) d -> p s d", p=128), o3[:, 0:nsub, :], ost, r=[ost])
                    yield

        def layer_setup(l):
            kb.new_phase()
            for bf in (gq, gk, gcq, gckv, psc, PWbd):
                bf.dsem = kb.dsem()
            kb.dma(sp, gq.ap, gqa_qn_g[l].partition_broadcast(128), gq, w=[gq])
            kb.dma(sp, gk.ap, gqa_kn_g[l].partition_broadcast(128), gk, w=[gk])
            kb.dma(sp, gcq.ap, mla_qn_g[l].partition_broadcast(128), gcq, w=[gcq])
            kb.dma(sp, gckv.ap, mla_kvn_g[l].partition_broadcast(128), gckv, w=[gckv])
            for j in range(2):
                kb.dma(sp, psc.ap[:, j:j + 1], pool_scale[l, j * 128:(j + 1) * 128].rearrange("(p o) -> p o", o=1), psc, w=[psc] if j == 0 else (), aw=() if j == 0 else [psc])
            kb.op(pool, lambda e: e.memset(PWbd.ap, 0.0), w=[PWbd])
            pw3 = v3(PWbd.ap, 2, 128)
            for g in range(4):
                p0 = (g % 2) * 64
                kb.dma(sp, pw3[p0:p0 + 64, g // 2, p0:p0 + 64], pool_w[l, g], PWbd, aw=[PWbd])

        def phase_C1(l, b, with_ctx):
            kb.new_phase()
            R1.reset()
            R2.reset()
            A = lambda n, parts=128, d=False: Buf(R2.take(n, parts), kb.dsem() if d else None)
            wq_sb = Buf(R1.take(KD * 2048), kb.dsem())
            wo_sb = Buf(R1.take(KD * 1024), kb.dsem())
            wq3 = v3(wq_sb.ap, KD, 2048)
            wo3 = v3(wo_sb.ap, KD, 1024)
            kb.dma(sp, wo3, w_out[l].rearrange("(k p) n -> p k n", p=128), wo_sb, w=[wo_sb])
            kb.dma(sp, wq3, peer_wq[l].rearrange("(k p) n -> p k n", p=128), wq_sb, w=[wq_sb])
            skT = A(2048)
            sSs = [A(2048, d=True) for _ in range(2)]
            sS = sSs[0]
            G1 = A(D, d=True); A2 = A(D, d=True); SH2 = A(D, d=True)
            xts = [A(D, d=True) for _ in range(1)]
            mixins = [A(780, d=True) for _ in range(2)]
            obts = [A(256, d=True) for _ in range(1)]
            mixT = A(768)
            xn = A(D, d=True)
            h2 = A(D, d=True)
            h2T = A(D)
            qpT = A(2048)
            eid = A(128, d=True)
            gate = A(128, d=True)
            ss = A(1)
            PT = PSB(0, 1024)
            PW = PSB(1024, 2048)
            PQ = PSB(2048, 4096)
            s3 = v3(sS.ap, 16, 128)
            kb.dma(sp, s3, peer_subkeys[l].rearrange("h p n d -> n (h p) d"), sS, w=[sS])
            transposes(lambda k: s3[:, k, :], 16, [sS], ident, PQ, lambda k: PQ.ap[:, k * 128:(k + 1) * 128], skT, skT.ap, 2048)
            sk3 = v3(skT.ap, 16, 128)
            tiles = tok_tiles(with_ctx)
            rcache = {}
            rar = Arena(R2T, R2C)
            rar.off = R2.off

            def loads(i):
                is_ctx, ti, tk0 = tiles[i]
                xt = xts[i % len(xts)]
                kb.dma(sp, xt.ap, x_src(l, b, is_ctx, ti), xt, w=[xt])
                ob_ = obts[i % len(obts)]
                kb.dma(sp, v3(ob_.ap, 2, 128), obT_d[:, :, tk0:tk0 + 128].rearrange("j p t -> p j t"), ob_, w=[ob_])
                mi_ = mixins[i % 2]
                kb.dma(sp, v3(mi_.ap[:, 0:768], 12, 64), mix_d[tk0:tk0 + 128, :, 0:64], mi_, w=[mi_])
                kb.dma(sp, None, None, mi_, aw=[mi_], fn=lambda q: q.dma_start(out=mi_.ap[:, 768:780].unsqueeze(2), in_=mix_d[tk0:tk0 + 128, :, 64:65], allow_slow_non_contiguous=True))

            def do_routing(p):
                sS_, tk_ = p
                peer_routing(kb, rar, sS_, iota, eid, gate, eoff=l * NEXP, cache=rcache)
                kb.dma(sp, eid_d[tk_:tk_ + 128, :], eid.ap.bitcast(I32), eid, r=[eid])
                kb.dma(sp, gate_d[tk_:tk_ + 128, :], gate.ap, gate, r=[gate])

            loads(0)
            cur_kind = None
            pending = None
            for i, (is_ctx, ti, tk0) in enumerate(tiles):
                if cur_kind != is_ctx:
                    cur_kind = is_ctx
                    row = NB if is_ctx else b
                    bc_load(G1, l, row, 2)
                    bc_load(A2, l, row, 4)
                    bc_load(SH2, l, row, 3)
                xt = xts[i % len(xts)]
                obt = obts[i % len(obts)]
                mixin = mixins[i % 2]
                sS = sSs[i % 2]
                kb.op(dve, lambda e: e.reciprocal(out=mixin.ap[:, 768:780], in_=mixin.ap[:, 768:780]), r=[mixin], w=[mixin])
                kb.op(dve, lambda e: e.tensor_tensor(out=v3(mixin.ap[:, 0:768], 12, 64), in0=v3(mixin.ap[:, 0:768], 12, 64), in1=mixin.ap[:, 768:780].unsqueeze(2).to_broadcast([128, 12, 64]), op=ALU.mult), r=[mixin], w=[mixin])
                transposes(lambda k: mixin.ap[:, k * 128:(k + 1) * 128], 6, [mixin], ident, PT, lambda k: PT.ap[:, k * 128:(k + 1) * 128], mixT, mixT.ap, 768)
                mT3 = v3(mixT.ap, 6, 128)
                ob3 = v3(obt.ap, 2, 128)
                for half in range(2):
                    for kk in range(KD):
                        if kk < 3:
                            lt, lb = mT3[:, kk, :], mixT
                        elif kk < 5:
                            lt, lb = ob3[:, kk - 3, :], obt
                        else:
                            lt, lb = mT3[:, kk - 2, :], mixT
                        first = (half == 0 and kk == 0)
                        kb.op(pe, lambda e: e.matmul(PW.ap[:, half * 512:(half + 1) * 512], lhsT=lt, rhs=wo3[:, kk, half * 512:(half + 1) * 512], start=(kk == 0), stop=(kk == KD - 1)),
                              r=[lb, wo_sb], w=[PW] if first else (), aw=() if first else [PW])
                kb.op(dve, lambda e: e.tensor_tensor(out=xn.ap, in0=PW.ap, in1=G1.ap, op=ALU.mult), r=[PW, G1], w=[xn])
                kb.op(pool, lambda e: e.tensor_tensor(out=xn.ap, in0=xn.ap, in1=xt.ap, op=ALU.add), r=[xt], w=[xn])
                kb.dma(sp, xmid_d[tk0:tk0 + 128, :], xn.ap, xn, r=[xn])
                rms_rstd(xn.ap, [xn], h2, ss, D)
                kb.op(dve, lambda e: e.scalar_tensor_tensor(out=h2.ap, in0=xn.ap, scalar=ss.ap[:, 0:1], in1=A2.ap, op0=ALU.mult, op1=ALU.mult), r=[xn, ss, A2], w=[h2])
                kb.op(pool, lambda e: e.tensor_tensor(out=h2.ap, in0=h2.ap, in1=SH2.ap, op=ALU.add), r=[SH2], w=[h2])
                kb.dma(sp, h2_d[tk0:tk0 + 128, :], h2.ap, h2, r=[h2])
                if pending is not None:
                    do_routing(pending)
                    pending = None
                if i + 1 < len(tiles):
                    loads(i + 1)
                transposes(lambda k: h2.ap[:, k * 128:(k + 1) * 128], KD, [h2], ident, PT, lambda k: PT.ap[:, k * 128:(k + 1) * 128], h2T, h2T.ap, 1024)
                hT3 = v3(h2T.ap, KD, 128)
                for hp in range(16):
                    for k in range(KD):
                        first = (hp == 0 and k == 0)
                        kb.op(pe, lambda e: e.matmul(PQ.ap[:, hp * 128:(hp + 1) * 128], lhsT=wq3[:, k, hp * 128:(hp + 1) * 128], rhs=hT3[:, k, :], start=(k == 0), stop=(k == KD - 1)),
                              r=[wq_sb, h2T], w=[PQ] if first else (), aw=() if first else [PQ])
                kb.op(act, lambda e: e.copy(out=qpT.ap, in_=PQ.ap), r=[PQ], w=[qpT])
                qp3 = v3(qpT.ap, 16, 128)
                for hp in range(16):
                    kb.op(pe, lambda e: e.matmul(PSALL[:, hp * 128:(hp + 1) * 128], lhsT=qp3[:, hp, :], rhs=sk3[:, hp, :], start=True, stop=True),
                          r=[qpT, skT], w=[PT, PW] if hp == 0 else (), aw=() if hp == 0 else [PT, PW])
                kb.op(act, lambda e: e.copy(out=sS.ap[:, 0:1024], in_=PT.ap), r=[PT], w=[sS])
                kb.op(act, lambda e: e.copy(out=sS.ap[:, 1024:2048], in_=PW.ap), r=[PW], aw=[sS])
                pending = (sS, tk0)
            do_routing(pending)

        NS = 16

        def gen_C2(l, b, with_ctx, last, sub=None, nslots=NS, nacc=4):
            A = lambda n, parts=128, d=False: Buf(R2.take(n, parts), kb.dsem() if d else None)
            A1_ = lambda n, d=False: Buf(R1.take(n), kb.dsem() if d else None)
            G2 = A(D, d=True)
            h2ts = [A(D, d=True) for _ in range(2)]
            xnts = [A(D, d=True) for _ in range(2)]
            eids = [A(128, d=True) for _ in range(2)]
            gates = [A(128, d=True) for _ in range(2)]
            a = A(128); wgt = A(128); g1 = A(128); g2 = A(128)
            ss = A(1)
            accs = [A1_(D, d=True) for _ in range(nacc)]
            xo = A1_(D, d=True)
            junk = A1_(D)
            slots = [Buf(R1.take(D), kb.gsems[k_]) for k_ in range(nslots)]
            tiles = tok_tiles(with_ctx)
            if sub is not None:
                tiles = [tiles[k_] for k_ in sub]
            if not tiles:
                return

            def loads(i):
                is_ctx, ti, tk0 = tiles[i]
                kb.dma(sp, h2ts[i % 2].ap, h2_d[tk0:tk0 + 128, :], h2ts[i % 2], w=[h2ts[i % 2]])
                kb.dma(sp, xnts[i % 2].ap, xmid_d[tk0:tk0 + 128, :], xnts[i % 2], w=[xnts[i % 2]])
                kb.dma(sp, eids[i % 2].ap.bitcast(I32), eid_d[tk0:tk0 + 128, :], eids[i % 2], w=[eids[i % 2]])
                kb.dma(sp, gates[i % 2].ap, gate_d[tk0:tk0 + 128, :], gates[i % 2], w=[gates[i % 2]])

            def gather(slot, tab, eidb, e_):
                kb.dma(pool, None, None, slot, r=[eidb], w=[slot],
                       fn=lambda q: q.indirect_dma_start(out=slot.ap, out_offset=None, in_=tab,
                                                         in_offset=bass.IndirectOffsetOnAxis(ap=eidb.ap.bitcast(I32)[:, e_:e_ + 1], axis=0)))

            loads(0)
            cur_kind = None
            ng = 0
            for i, (is_ctx, ti, tk0) in enumerate(tiles):
                if cur_kind != is_ctx:
                    cur_kind = is_ctx
                    bc_load(G2, l, NB if is_ctx else b, 5)
                if i + 1 < len(tiles):
                    loads(i + 1)
                h2t, xnt, eidb, gateb = h2ts[i % 2], xnts[i % 2], eids[i % 2], gates[i % 2]
                kb.op(dve, lambda e: e.memset(a.ap, 0.0), w=[a])
                for e_ in range(128):
                    slot = slots[ng % nslots]
                    ng += 1
                    gather(slot, peer_u, eidb, e_)
                    kb.op(dve, lambda e: e.scalar_tensor_tensor(out=junk.ap, in0=slot.ap, scalar=1.0, in1=h2t.ap, op0=ALU.mult, op1=ALU.mult, accum_out=a.ap[:, e_:e_ + 1]), r=[slot, h2t], w=[junk], aw=[a])
                    if e_ % 8 == 7:
                        yield
                kb.op(dve, lambda e: e.tensor_tensor(out=g1.ap, in0=a.ap, in1=a.ap, op=ALU.mult), r=[a], w=[g1])
                kb.op(dve, lambda e: e.tensor_scalar(out=g1.ap, in0=g1.ap, scalar1=0.044715, scalar2=1.0, op0=ALU.mult, op1=ALU.add), r=[g1], w=[g1])
                kb.op(dve, lambda e: e.tensor_tensor(out=g1.ap, in0=g1.ap, in1=a.ap, op=ALU.mult), r=[a], w=[g1])
                kb.op(act, lambda e: e.activation(out=g2.ap, in_=g1.ap, func=AF.Tanh, scale=0.7978845608028654), r=[g1], w=[g2])
                kb.op(dve, lambda e: e.tensor_scalar(out=g2.ap, in0=g2.ap, scalar1=1.0, scalar2=0.5, op0=ALU.add, op1=ALU.mult), r=[g2], w=[g2])
                kb.op(dve, lambda e: e.tensor_tensor(out=g2.ap, in0=g2.ap, in1=a.ap, op=ALU.mult), r=[a], w=[g2])
                kb.op(dve, lambda e: e.tensor_tensor(out=wgt.ap, in0=g2.ap, in1=gateb.ap, op=ALU.mult), r=[g2, gateb], w=[wgt])
                for e_ in range(128):
                    slot = slots[ng % nslots]
                    ng += 1
                    gather(slot, peer_v, eidb, e_)
                    acc = accs[e_ % nacc]
                    if e_ < nacc:
                        kb.op(dve, lambda e: e.tensor_scalar_mul(out=acc.ap, in0=slot.ap, scalar1=wgt.ap[:, e_:e_ + 1]), r=[slot, wgt], w=[acc])
                    else:
                        kb.op(dve, lambda e: e.scalar_tensor_tensor(out=acc.ap, in0=slot.ap, scalar=wgt.ap[:, e_:e_ + 1], in1=acc.ap, op0=ALU.mult, op1=ALU.add), r=[slot, wgt], w=[acc])
                    if e_ % 8 == 7:
                        yield
                kb.op(dve, lambda e: e.tensor_tensor(out=accs[0].ap, in0=accs[0].ap, in1=accs[1].ap, op=ALU.add), r=[accs[1]], w=[accs[0]])
                if nacc == 4:
                    kb.op(dve, lambda e: e.tensor_tensor(out=accs[2].ap, in0=accs[2].ap, in1=accs[3].ap, op=ALU.add), r=[accs[3]], w=[accs[2]])
                    kb.op(dve, lambda e: e.tensor_tensor(out=accs[0].ap, in0=accs[0].ap, in1=accs[2].ap, op=ALU.add), r=[accs[2]], w=[accs[0]])
                kb.op(dve, lambda e: e.tensor_tensor(out=xo.ap, in0=accs[0].ap, in1=G2.ap, op=ALU.mult), r=[accs[0], G2], w=[xo])
                kb.op(dve, lambda e: e.tensor_tensor(out=xo.ap, in0=xo.ap, in1=xnt.ap, op=ALU.add), r=[xnt], w=[xo])
                if last:
                    if not is_ctx:
                        rms_rstd(xo.ap, [xo], accs[0], ss, D)
                        kb.op(dve, lambda e: e.scalar_tensor_tensor(out=accs[1].ap, in0=xo.ap, scalar=ss.ap[:, 0:1], in1=FG.ap, op0=ALU.mult, op1=ALU.mult), r=[xo, ss, FG], w=[accs[1]])
                        kb.dma(sp, out_d[b, ti * 128:(ti + 1) * 128, :], accs[1].ap, accs[1], r=[accs[1]])
                else:
                    dst = (xc1_d if is_ctx else xl1_d)[b, ti * 128:(ti + 1) * 128, :]
                    kb.dma(sp, dst, xo.ap, xo, r=[xo])

        def run_phase(gens, weights):
            kb.new_phase()
            R1.reset()
            R2.reset()
            done = [0] * len(gens)
            alive = [True] * len(gens)
            while any(alive):
                best = None
                for k_, g_ in enumerate(gens):
                    if alive[k_]:
                        fr = done[k_] / float(max(1, weights[k_]))
                        if best is None or fr < best[0]:
                            best = (fr, k_)
                k_ = best[1]
                try:
                    next(gens[k_])
                    done[k_] += 1
                except StopIteration:
                    alive[k_] = False

        def n_yields_B(with_ctx):
            return 12 * (len(range(CTX, NK, 512)) + (1 if with_ctx else 0))

        def n_yields_C2(with_ctx):
            return 32 * (NT_L + (NT_C if with_ctx else 0))

        def schedule():
            if stop == "pro":
                return
            seqs = [(l, b) for l in range(DEPTH) for b in range(NB)]
            n = len(seqs)
            upd_of = lambda q: q[0] < DEPTH - 1
            last_of = lambda q: q[0] == DEPTH - 1
            ntile = lambda q: NT_L + (NT_C if upd_of(q) else 0)
            setup_done = set()

            def ensure_setup(l):
                if l not in setup_done:
                    layer_setup(l)
                    setup_done.add(l)

            def C2g(q, sub=None, nslots=NS, nacc=4):
                return gen_C2(q[0], q[1], upd_of(q), last_of(q), sub=sub, nslots=nslots, nacc=nacc)

            if NB >= 3 and stop is None:
                TAIL = 5
                ensure_setup(seqs[0][0])
                run_phase([gen_A(*seqs[0])], [1])
                phase_P(seqs[0][0], seqs[0][1], upd_of(seqs[0]))
                run_phase([gen_B(seqs[0][0], seqs[0][1], upd_of(seqs[0]))], [1])
                phase_C1(seqs[0][0], seqs[0][1], upd_of(seqs[0]))
                ensure_setup(seqs[1][0])
                run_phase([gen_A(*seqs[1])], [1])
                phase_P(seqs[1][0], seqs[1][1], upd_of(seqs[1]))
                for k in range(n - 1):
                    q0, q1 = seqs[k], seqs[k + 1]
                    nt = ntile(q0)
                    split = max(0, nt - TAIL)
                    run_phase([C2g(q0, range(0, split)), gen_B(q1[0], q1[1], upd_of(q1))], [32 * split, n_yields_B(upd_of(q1))])
                    if k + 2 < n:
                        q2 = seqs[k + 2]
                        ensure_setup(q2[0])
                        run_phase([C2g(q0, range(split, nt), nslots=4, nacc=2), gen_A(*q2)], [32 * TAIL, 8 * (NT_C + NT_L)])
                    else:
                        run_phase([C2g(q0, range(split, nt))], [1])
                    phase_C1(q1[0], q1[1], upd_of(q1))
                    if k + 2 < n:
                        phase_P(q2[0], q2[1], upd_of(q2))
                run_phase([C2g(seqs[n - 1])], [1])
                return

            fuse = NB >= 2 and stop is None
            pend = None
            for (l, b) in seqs:
                ensure_setup(l)
                upd = l < DEPTH - 1
                if pend is not None and not fuse:
                    run_phase([C2g(pend)], [1])
                    pend = None
                    if stop == "C2":
                        return
                run_phase([gen_A(l, b)], [1])
                if stop == "A":
                    return
                phase_P(l, b, upd)
                if stop == "P":
                    return
                if pend is not None:
                    run_phase([C2g(pend), gen_B(l, b, upd)], [n_yields_C2(upd_of(pend)), n_yields_B(upd)])
                else:
                    run_phase([gen_B(l, b, upd)], [1])
                if stop == "B":
                    return
                phase_C1(l, b, upd)
                if stop == "C1":
                    return
                pend = (l, b)
            run_phase([C2g(pend)], [1])

        schedule()
        kb.barrier()
        build.stats = {e.name: e.n for e in kb.engs}
        build.stats['nops'] = kb.nops
    return nc


def _consts(S, CTX):
    f = np.float32
    c = {}
    c["c_ident"] = np.eye(128, dtype=f)
    i16 = np.arange(16, dtype=f)
    c["c_iota"] = np.tile(np.concatenate([i16, 16 * i16, 16 * i16 + 16]).astype(f), (128, 1))
    rows = S // GRID_W
    row = np.repeat(np.arange(rows), GRID_W).astype(f)
    col = np.tile(np.arange(GRID_W), rows).astype(f)

    def rope(rot_dim):
        n = rot_dim // 4
        inv = (f(10000.0) ** (-np.arange(n, dtype=f) / f(n))).astype(f)
        ang = np.concatenate([row[:, None] * inv, col[:, None] * inv], axis=-1).astype(f)
        return np.concatenate([np.cos(ang), np.sin(ang)], axis=-1).astype(f)

    c["c_ropeA"] = rope(64)
    c["c_ropeC"] = rope(32)

    def rc(L):
        out = np.zeros((4, L), f)
        t = np.arange(L)
        for i, w in enumerate((2, 4, 8, 16)):
            lo = np.clip(t - w // 2, 0, L)
            hi = np.clip(t - w // 2 + w, 0, L)
            out[i] = (1.0 / (hi - lo).astype(f)).astype(f)
        return out

    c["c_rcL"] = rc(S)
    c["c_rcC"] = rc(CTX)
    return c


def make_in_maps(inputs, n_cores, NB, S, CTX, DEPTH):
    f = np.float32
    shared = {}
    for k in ("ada_w", "ada_b", "norm1_g", "norm2_g", "w_in", "gqa_qn_g", "gqa_kn_g", "pool_w", "pool_scale", "mla_qn_g",
              "mla_kvn_g", "mla_w_uq", "mla_w_ukv", "w_out", "peer_wq", "peer_subkeys", "final_g"):
        shared[k] = np.ascontiguousarray(np.asarray(inputs[k], dtype=f))
    shared["peer_u"] = np.ascontiguousarray(np.asarray(inputs["peer_u"], dtype=f).reshape(DEPTH * NEXP, D))
    shared["peer_v"] = np.ascontiguousarray(np.asarray(inputs["peer_v"], dtype=f).reshape(DEPTH * NEXP, D))
    shared.update(_consts(S, CTX))
    x = np.asarray(inputs["x"], dtype=f)
    ctx = np.asarray(inputs["ctx"], dtype=f)
    c = np.asarray(inputs["c"], dtype=f)
    c_ctx = np.asarray(inputs["c_ctx"], dtype=f)
    maps = []
    for i in range(n_cores):
        m = dict(shared)
        m["x"] = np.ascontiguousarray(x[i * NB:(i + 1) * NB])
        m["ctx"] = np.ascontiguousarray(ctx[i * NB:(i + 1) * NB])
        m["cT"] = np.ascontiguousarray(np.concatenate([c[i * NB:(i + 1) * NB], c_ctx[None, :]], axis=0).T)
        maps.append(m)
    return maps


_NC_CACHE = {}


def kernel(**inputs):
    B, S, _ = inputs["x"].shape
    CTX = inputs["ctx"].shape[1]
    DEPTH = inputs["ada_w"].shape[0]
    NB = B // N_CORES
    key = (NB, S, CTX, DEPTH)
    if key not in _NC_CACHE:
        _NC_CACHE[key] = build(NB, S, CTX, DEPTH)
    nc = _NC_CACHE[key]
    maps = make_in_maps(inputs, N_CORES, NB, S, CTX, DEPTH)
    res = run_bass_kernel_spmd(nc, maps, core_ids=list(range(N_CORES)))
    return np.concatenate([np.asarray(r["out"], dtype=np.float32) for r in res.results], axis=0)
```
